# Optimizing a Trainium2 kernel written in Bass

```python
import jax, jax.numpy as jnp
from jax import lax
import numpy as np

D_MODEL = 1024
BATCH = 4
SEQ = 4096
DEPTH = 1

CHUNK = 64
EPS = 1e-6
D_MIX = D_MODEL
MLSTM_HEADS = 4
MLSTM_HEAD_DIM = (D_MIX // 2) // MLSTM_HEADS
MLSTM_WIDTH = MLSTM_HEADS * MLSTM_HEAD_DIM
CONV_WIDTH = 4
GMLP_GROUPS = 4
GMLP_WIDTH = D_MIX - MLSTM_WIDTH
GMLP_GROUP_DIM = GMLP_WIDTH // GMLP_GROUPS
GMLP_CHUNK = 128
D_IN_PROJ = 4 * MLSTM_WIDTH + 2 * MLSTM_HEADS + 2 * GMLP_WIDTH
PEER_HEADS = 8
PEER_KEYS = 128
PEER_EXPERTS = PEER_KEYS * PEER_KEYS
PEER_QUERY_DIM = 256
PEER_HALF = PEER_QUERY_DIM // 2
PEER_TOPK = 16
PEER_BLOCK = 128

kernel_name = "hymba_mlstm_gmlp_peer_block"


def rmsnorm(x, g):
    xf = x.astype(jnp.float32)
    y = xf * lax.rsqrt(jnp.mean(xf * xf, axis=-1, keepdims=True) + EPS)
    return (y * g.astype(jnp.float32)).astype(x.dtype)


def causal_conv(x, w, b):
    S = x.shape[1]
    xp = jnp.pad(x, ((0, 0), (CONV_WIDTH - 1, 0), (0, 0)))
    out = b
    for j in range(CONV_WIDTH):
        out = out + xp[:, j:j + S] * w[j]
    return out


def mlstm_chunkwise(q, k, v, ig, lf):
    B, H, S, Dh = q.shape
    nc = S // CHUNK

    def to_chunks(a):
        a = a.reshape(B, H, nc, CHUNK, *a.shape[3:])
        return jnp.moveaxis(a, 2, 0)

    causal = jnp.tril(jnp.ones((CHUNK, CHUNK), dtype=bool))

    def step(carry, xs):
        C, n, m = carry
        qc, kc, vc, igc, lfc = xs
        b = jnp.cumsum(lfc, axis=-1)
        D = b[..., :, None] - b[..., None, :] + igc[..., None, :]
        D = jnp.where(causal, D, -jnp.inf)
        inter = b + m[..., None]
        m_t = jnp.maximum(inter, jnp.max(D, axis=-1))
        w = jnp.exp(D - m_t[..., None])
        s = jnp.einsum('bhtd,bhsd->bhts', qc, kc) * w
        sc = jnp.exp(inter - m_t)
        num = jnp.einsum('bhts,bhsd->bhtd', s, vc) + sc[..., None] * jnp.einsum('bhvk,bhtk->bhtv', C, qc)
        den = jnp.sum(s, axis=-1) + sc * jnp.einsum('bhk,bhtk->bht', n, qc)
        h = num / jnp.maximum(jnp.abs(den), jnp.exp(-m_t))[..., None]
        bL = b[..., -1]
        g = bL[..., None] - b + igc
        m_new = jnp.maximum(bL + m, jnp.max(g, axis=-1))
        decay = jnp.exp(bL + m - m_new)
        wg = jnp.exp(g - m_new[..., None])
        C = decay[..., None, None] * C + jnp.einsum('bhs,bhsv,bhsk->bhvk', wg, vc, kc)
        n = decay[..., None] * n + jnp.einsum('bhs,bhsk->bhk', wg, kc)
        return (C, n, m_new), h

    init = (jnp.zeros((B, H, Dh, Dh), jnp.float32),
            jnp.zeros((B, H, Dh), jnp.float32),
            jnp.zeros((B, H), jnp.float32))
    _, hs = lax.scan(step, init, (to_chunks(q), to_chunks(k), to_chunks(v), to_chunks(ig), to_chunks(lf)))
    return jnp.moveaxis(hs, 0, 2).reshape(B, H, S, Dh)


def mlstm_group(q_pre, k_pre, v, o_pre, i_pre, f_pre, conv_w, conv_b, b_igate, b_fgate, norm_g):
    B, S, _ = q_pre.shape
    qk = jax.nn.silu(causal_conv(jnp.concatenate([q_pre, k_pre], axis=-1), conv_w, conv_b))
    q, k = qk[..., :MLSTM_WIDTH], qk[..., MLSTM_WIDTH:]

    def heads(a):
        return a.reshape(B, S, MLSTM_HEADS, MLSTM_HEAD_DIM).transpose(0, 2, 1, 3).astype(jnp.float32)

    ig = (i_pre + b_igate).astype(jnp.float32).transpose(0, 2, 1)
    lf = jax.nn.log_sigmoid((f_pre + b_fgate).astype(jnp.float32)).transpose(0, 2, 1)
    h = mlstm_chunkwise(heads(q), heads(k) * (MLSTM_HEAD_DIM ** -0.5), heads(v), ig, lf)
    h = h * lax.rsqrt(jnp.mean(h * h, axis=-1, keepdims=True) + EPS)
    h = h.transpose(0, 2, 1, 3).reshape(B, S, MLSTM_WIDTH) * norm_g.astype(jnp.float32)
    return (h * jax.nn.sigmoid(o_pre.astype(jnp.float32))).astype(q_pre.dtype)


def gmlp_group(u_pre, v_pre, ln_g, ln_b, w_s, b_s):
    B, S, _ = u_pre.shape
    u = jax.nn.gelu(u_pre)
    vv = jax.nn.gelu(v_pre).reshape(B, S, GMLP_GROUPS, GMLP_GROUP_DIM).astype(jnp.float32)
    mu = jnp.mean(vv, axis=-1, keepdims=True)
    var = jnp.mean(jnp.square(vv - mu), axis=-1, keepdims=True)
    vv = (vv - mu) * lax.rsqrt(var + EPS)
    vv = vv * ln_g.reshape(GMLP_GROUPS, GMLP_GROUP_DIM) + ln_b.reshape(GMLP_GROUPS, GMLP_GROUP_DIM)
    vv = vv.astype(u_pre.dtype).reshape(B, S // GMLP_CHUNK, GMLP_CHUNK, GMLP_GROUPS, GMLP_GROUP_DIM)
    pos = jnp.arange(GMLP_CHUNK) // CHUNK
    mask = (pos[:, None] >= pos[None, :]).astype(w_s.dtype)
    mix = jnp.einsum('gts,bcsgd->bctgd', w_s * mask, vv) + b_s.T[:, :, None]
    return u * mix.reshape(B, S, GMLP_WIDTH)


def peer(x, w_query, sub_keys, expert_u, expert_v):
    B, S, D = x.shape
    T = B * S
    xt = x.reshape(T, D)
    q = (xt @ w_query).reshape(T, PEER_HEADS, 2, PEER_HALF)
    s = jnp.einsum('thpd,hpkd->thpk', q, sub_keys).astype(jnp.float32)
    s1, i1 = lax.top_k(s[:, :, 0], PEER_TOPK)
    s2, i2 = lax.top_k(s[:, :, 1], PEER_TOPK)
    cand = (s1[..., :, None] + s2[..., None, :]).reshape(T, PEER_HEADS, PEER_TOPK * PEER_TOPK)
    cand_idx = (i1[..., :, None] * PEER_KEYS + i2[..., None, :]).reshape(T, PEER_HEADS, PEER_TOPK * PEER_TOPK)
    top_s, pos = lax.top_k(cand, PEER_TOPK)
    idx = jnp.take_along_axis(cand_idx, pos, axis=-1)
    gate = jax.nn.softmax(top_s, axis=-1).astype(x.dtype)
    nb = T // PEER_BLOCK

    def block(args):
        xb, ib, gb = args
        ub = jnp.take(expert_u, ib, axis=0)
        vb = jnp.take(expert_v, ib, axis=0)
        a = jax.nn.gelu(jnp.einsum('td,thkd->thk', xb, ub))
        return jnp.einsum('thk,thkd->td', a * gb, vb)

    out = lax.map(block, (xt.reshape(nb, PEER_BLOCK, D),
                          idx.reshape(nb, PEER_BLOCK, PEER_HEADS, PEER_TOPK),
                          gate.reshape(nb, PEER_BLOCK, PEER_HEADS, PEER_TOPK)))
    return out.reshape(B, S, D)


def setup_inputs(seed: int = 0) -> dict:
    key = jax.random.key(seed)
    ks = jax.random.split(key, 20)
    f32 = jnp.float32
    L = DEPTH
    nrm = lambda k, shape, scale: jax.random.normal(k, shape, f32) * scale
    return {
        "x": nrm(ks[0], (BATCH, SEQ, D_MODEL), 1.0),
        "norm1_g": 1.0 + nrm(ks[1], (L, D_MODEL), 0.02),
        "w_in": nrm(ks[2], (L, D_MODEL, D_IN_PROJ), D_MODEL ** -0.5),
        "conv_w": nrm(ks[3], (L, CONV_WIDTH, 2 * MLSTM_WIDTH), CONV_WIDTH ** -0.5),
        "conv_b": nrm(ks[4], (L, 2 * MLSTM_WIDTH), 0.02),
        "b_igate": nrm(ks[5], (L, MLSTM_HEADS), 0.1),
        "b_fgate": jnp.broadcast_to(jnp.linspace(3.0, 6.0, MLSTM_HEADS, dtype=f32), (L, MLSTM_HEADS)) + nrm(ks[6], (L, MLSTM_HEADS), 0.1),
        "mlstm_norm_g": 1.0 + nrm(ks[7], (L, MLSTM_WIDTH), 0.02),
        "gmlp_ln_g": 1.0 + nrm(ks[8], (L, GMLP_WIDTH), 0.02),
        "gmlp_ln_b": nrm(ks[9], (L, GMLP_WIDTH), 0.02),
        "gmlp_w_s": nrm(ks[10], (L, GMLP_GROUPS, GMLP_CHUNK, GMLP_CHUNK), GMLP_CHUNK ** -0.5),
        "gmlp_b_s": 1.0 + nrm(ks[11], (L, GMLP_GROUPS, GMLP_CHUNK), 0.02),
        "w_out": nrm(ks[12], (L, D_MIX, D_MODEL), D_MIX ** -0.5),
        "norm2_g": 1.0 + nrm(ks[13], (L, D_MODEL), 0.02),
        "peer_w_query": nrm(ks[14], (L, D_MODEL, PEER_HEADS * PEER_QUERY_DIM), D_MODEL ** -0.5),
        "peer_sub_keys": nrm(ks[15], (L, PEER_HEADS, 2, PEER_KEYS, PEER_HALF), PEER_HALF ** -0.5),
        "peer_u": nrm(ks[16], (L, PEER_EXPERTS, D_MODEL), D_MODEL ** -0.5),
        "peer_v": nrm(ks[17], (L, PEER_EXPERTS, D_MODEL), PEER_HEADS ** -0.5),
        "final_g": 1.0 + nrm(ks[18], (D_MODEL,), 0.02),
    }


def reference(x, norm1_g, w_in, conv_w, conv_b, b_igate, b_fgate, mlstm_norm_g,
              gmlp_ln_g, gmlp_ln_b, gmlp_w_s, gmlp_b_s, w_out, norm2_g,
              peer_w_query, peer_sub_keys, peer_u, peer_v, final_g):
    W = MLSTM_WIDTH
    for l in range(DEPTH):
        h = rmsnorm(x, norm1_g[l])
        z = h @ w_in[l]
        q_pre = z[..., 0:W]
        k_pre = z[..., W:2 * W]
        v = z[..., 2 * W:3 * W]
        o_pre = z[..., 3 * W:4 * W]
        g0 = 4 * W
        i_pre = z[..., g0:g0 + MLSTM_HEADS]
        f_pre = z[..., g0 + MLSTM_HEADS:g0 + 2 * MLSTM_HEADS]
        u0 = g0 + 2 * MLSTM_HEADS
        u_pre = z[..., u0:u0 + GMLP_WIDTH]
        vg_pre = z[..., u0 + GMLP_WIDTH:u0 + 2 * GMLP_WIDTH]
        y_a = mlstm_group(q_pre, k_pre, v, o_pre, i_pre, f_pre, conv_w[l], conv_b[l],
                          b_igate[l], b_fgate[l], mlstm_norm_g[l])
        y_b = gmlp_group(u_pre, vg_pre, gmlp_ln_g[l], gmlp_ln_b[l], gmlp_w_s[l], gmlp_b_s[l])
        x = x + jnp.concatenate([y_a, y_b], axis=-1) @ w_out[l]
        x = x + peer(rmsnorm(x, norm2_g[l]), peer_w_query[l], peer_sub_keys[l], peer_u[l], peer_v[l])
    return rmsnorm(x, final_g)
```

```python
import contextlib
import os
import numpy as np
import concourse.bass as bass
import concourse.mybir as mybir
from concourse.bass_utils import run_bass_kernel_spmd

F32 = mybir.dt.float32; BF16 = mybir.dt.bfloat16; U32 = mybir.dt.uint32; I32 = mybir.dt.int32
AF = mybir.ActivationFunctionType; ALU = mybir.AluOpType

NCORES = 8
NT = 2048
D = 1024
EPS = 1e-6
C_CW = 0; C_CB = 32; C_G1 = 40; C_G2 = 48; C_NG = 56; C_GB = 60; C_BS = 68; C_FLAG = 72
C_EPS = 73; C_ONE = 74; C_LNS = 75; C_SMALL = 80
C_LNG = 80; C_LNB = C_LNG + 512; C_FGB = C_LNB + 512; C_CST = C_FGB + 1024; C_WS = C_CST + 512
C_TOT = C_WS + 512


class Prog:
    ENG = ("pe", "act", "dve", "pool", "sp")

    def __init__(self, nc):
        self.nc = nc; self.ops = []; self.lastw = {}; self.readers = {}; self.dma_cnt = {}

    BANK = {}

    def op(self, eng, fn, reads=(), writes=(), dma=None):
        i = len(self.ops)
        reads = list(reads) + [self.BANK[k] for k in reads if k in self.BANK]
        writes = list(writes) + [self.BANK[k] for k in writes if k in self.BANK]
        deps = set()
        for k in list(reads) + list(writes):
            if k in self.lastw: deps.add(self.lastw[k])
        for k in writes:
            lastr = {}
            for r in self.readers.get(k, ()):
                ro = self.ops[r]
                if ro["dma"] is not None: deps.add(r)
                else: lastr[ro["eng"]] = r
            deps.update(lastr.values())
        o = dict(eng=eng, fn=fn, deps=deps, dma=dma, sig=False, dcount=None, sidx=None)
        if dma is not None:
            self.dma_cnt[dma] = self.dma_cnt.get(dma, 0) + 1
            o["dcount"] = self.dma_cnt[dma]
        self.ops.append(o)
        for k in writes:
            self.lastw[k] = i; self.readers[k] = []
        for k in reads:
            self.readers.setdefault(k, []).append(i)
        return i

    def fence(self):
        last = {}
        for i, o in enumerate(self.ops):
            if o["fn"] is None: continue
            if o["dma"] is not None: last[("d", o["dma"])] = i
            else: last[("e", o["eng"])] = i
        for e in self.ENG:
            i = self.op(e, None)
            self.ops[i]["deps"] = set(last.values())

    def emit(self, final_keys=()):
        nc = self.nc; ops = self.ops
        if not os.environ.get("NOFENCE"):
            self.fence()
        self.op("sp", None, reads=list(final_keys), writes=["__final"])
        pos = {e: 0 for e in self.ENG}
        for o in ops:
            if o["fn"] is not None:
                pos[o["eng"]] += 1
            o["epos"] = pos[o["eng"]]

        def needs(o, od):
            if od["dma"] is not None: return True
            if od["eng"] == o["eng"] and o["fn"] is not None and o["dma"] is None:
                if o["eng"] == "pe": return False
                return o["epos"] - od["epos"] < int(os.environ.get("NEEDS_DIST", "4"))
            return True
        self.needs = needs
        for o in ops:
            for d in o["deps"]:
                od = ops[d]
                if od["dma"] is None and needs(o, od):
                    od["sig"] = True
        cnt = {e: 0 for e in self.ENG}
        for o in ops:
            if o["dma"] is None and o["sig"] and o["fn"] is not None:
                cnt[o["eng"]] += 1; o["sidx"] = cnt[o["eng"]]
        self.stats = dict(nops=len(ops), sig=cnt, dma=dict(self.dma_cnt))
        with contextlib.ExitStack() as st:
            esem = {e: st.enter_context(nc.semaphore("s_" + e)) for e in self.ENG}
            dsem = {k: st.enter_context(nc.semaphore("d_" + str(k))) for k in self.dma_cnt}
            block = st.enter_context(nc.Block())

            def expand(o, acc, seen):
                for d in o["deps"]:
                    if d in seen: continue
                    seen.add(d)
                    od = ops[d]
                    if od["fn"] is None:
                        expand(od, acc, seen)
                    elif od["dma"] is not None:
                        acc.append((("d", od["dma"]), 16 * od["dcount"]))
                    elif self.needs(o, od):
                        acc.append((("e", od["eng"]), od["sidx"]))

            def run(ename, eng):
                waited = {}
                for o in ops:
                    if o["eng"] != ename: continue
                    acc = []
                    expand(o, acc, set())
                    best = {}
                    for k, v in acc:
                        if v is not None and v > best.get(k, 0): best[k] = v
                    for k, v in best.items():
                        if waited.get(k, 0) >= v: continue
                        waited[k] = v
                        eng.wait_ge(esem[k[1]] if k[0] == "e" else dsem[k[1]], v)
                    if o["fn"] is None: continue
                    ins = o["fn"](eng)
                    if o["dma"] is not None:
                        ins.then_inc(dsem[o["dma"]], 16)
                    elif o["sig"]:
                        ins.then_inc(esem[ename], 1)

            @block.tensor
            def _(e): run("pe", e)

            @block.scalar
            def _(e): run("act", e)

            @block.vector
            def _(e): run("dve", e)

            @block.gpsimd
            def _(e): run("pool", e)

            @block.sync
            def _(e): run("sp", e)


class Bld:
    def __init__(self, nc):
        self.P = Prog(nc)

    def mm(self, out, lhsT, rhs, start, stop, r, w):
        self.P.op("pe", lambda e: e.matmul(out, lhsT=lhsT, rhs=rhs, start=start, stop=stop), reads=r, writes=w)

    def tr(self, out, in_, ident, r, w):
        self.P.op("pe", lambda e: e.transpose(out=out, in_=in_, identity=ident), reads=r, writes=w)

    def act(self, out, in_, func, r, w, scale=None, bias=None, accum=None):
        kw = {}
        if scale is not None: kw["scale"] = scale
        if bias is not None: kw["bias"] = bias
        if accum is not None: kw["accum_out"] = accum
        self.P.op("act", lambda e: e.activation(out=out, in_=in_, func=func, **kw), reads=r, writes=w)

    def tt(self, eng, out, in0, in1, op, r, w):
        self.P.op(eng, lambda e: e.tensor_tensor(out=out, in0=in0, in1=in1, op=op), reads=r, writes=w)

    def ts(self, eng, out, in0, s1, s2, op0, op1, r, w):
        if s2 is None:
            self.P.op(eng, lambda e: e.tensor_scalar(out=out, in0=in0, scalar1=s1, scalar2=None, op0=op0), reads=r, writes=w)
        else:
            self.P.op(eng, lambda e: e.tensor_scalar(out=out, in0=in0, scalar1=s1, scalar2=s2, op0=op0, op1=op1), reads=r, writes=w)

    def stt(self, eng, out, in0, scalar, in1, op0, op1, r, w, accum=None):
        kw = {} if accum is None else {"accum_out": accum}
        self.P.op(eng, lambda e: e.scalar_tensor_tensor(out=out, in0=in0, scalar=scalar, in1=in1, op0=op0, op1=op1, **kw), reads=r, writes=w)

    def cp(self, eng, out, in_, r, w):
        self.P.op(eng, lambda e: e.tensor_copy(out=out, in_=in_), reads=r, writes=w)

    def ms(self, eng, out, val, w):
        self.P.op(eng, lambda e: e.memset(out, val), writes=w)

    def dma(self, q, out, in_, r, w, name):
        self.P.op(q, lambda e: e.dma_start(out=out, in_=in_), reads=r, writes=w, dma=name)


def build(stage="full"):
    nc = bass.Bass("TRN2", target_bir_lowering=False)
    dt = lambda name, shape, kind="ExternalInput", dty=F32: nc.dram_tensor(name, shape, dty, kind=kind).ap()
    xT_d = dt("xT", [128, 8, 4096]); xm_d = dt("xm", [NT, D]); win_d = dt("win", [128, 8, 3080])
    wout_d = dt("wout", [128, 8, 1024]); cst_d = dt("consts", [128, C_TOT])
    if stage not in ("A", "A2"):
        wq_d = dt("wq", [128, 8, 2048]); keys_d = dt("keysT", [128, 16, 128])
        ut_d = dt("UT", [128, 128, 8, 128]); v_d = dt("V", [16384, 1024])
    y_d = dt("y", [NT, D], kind="ExternalOutput")
    x1_d = dt("x1s", [NT, D], kind="Internal") if stage not in ("A", "A2") else y_d
    if stage not in ("A", "A2"):
        ub_d = dt("ubs", [128, 128, 1024], kind="Internal", dty=BF16)
        vb_d = dt("vbs", [16384, 1024], kind="Internal", dty=BF16)

    b = Bld(nc); P = b.P
    with contextlib.ExitStack() as st:
        NW = 41400
        cs = st.enter_context(nc.sbuf_tensor("cs", [128, C_TOT], F32))
        xn2T = st.enter_context(nc.sbuf_tensor("xn2T", [128, 8, NT], BF16))
        cb16 = st.enter_context(nc.sbuf_tensor("cb16", [128, 512], BF16))
        ar = st.enter_context(nc.sbuf_tensor("arena", [128, NW], F32))
        psb = [st.enter_context(nc.psum_tensor("psb%d" % i, [128, 512], F32))[:, :] for i in range(8)]
        identb = cb16[:, 0:128]; onesb = cb16[:, 128:256]; trib = cb16[:, 256:384]; iotab = cb16[:, 384:512]
        ident = cs[:, C_CST:C_CST + 128]; tri = cs[:, C_CST + 128:C_CST + 256]
        onesf = cs[:, C_CST + 256:C_CST + 384]; iota = cs[:, C_CST + 384:C_CST + 512]
        col = lambda c, n=1: cs[:, c:c + n]
        eps_c = col(C_EPS); one_c = col(C_ONE); lns_c = col(C_LNS); flag_c = col(C_FLAG)

        off = [0]
        def carve(words, dty=F32, shape=None):
            a = ar[:, off[0]:off[0] + words]; off[0] += words
            assert off[0] <= NW, off[0]
            if dty != F32: a = a.bitcast(dty)
            if shape is not None:
                names = " ".join("d%d" % i for i in range(len(shape)))
                a = a.rearrange("p (%s) -> p %s" % (names, names), **{"d%d" % i: s for i, s in enumerate(shape)})
            return a

        b.dma("sp", cs[:, :], cst_d[:, :], [], ["cs"], "cs")
        b.cp("dve", identb, ident, ["cs"], ["cb16"])
        b.cp("dve", onesb, onesf, ["cs"], ["cb16"])
        b.cp("dve", trib, tri, ["cs"], ["cb16"])
        b.cp("dve", iotab, iota, ["cs"], ["cb16"])

        Winb = carve(12320, BF16, [8, 3080]); Woutb = carve(4096, BF16, [8, 1024])
        xTs = carve(4096, F32, [8, 512]); raw = carve(4120, F32, [8, 515])
        sqb = carve(512, BF16, [2, 512]); lnv = carve(512); rstdb = carve(512)
        hT = carve(2048, BF16, [8, 512]); accs = carve(1024, F32, [2, 512]); qkT = carve(2048, BF16, [8, 512])
        WsTb = carve(256, BF16, [4, 128])
        sm = carve(128)
        sg = carve(192)
        vext = carve(264, BF16, [4, 132]); sig = carve(256, BF16); u_sb = carve(256, BF16)
        vgs = carve(512); vn32 = carve(512); vnb = carve(256, BF16); ktok = carve(256, BF16)
        PT = carve(256, BF16, [4, 128]); ybf = carve(512, BF16); yT = carve(512, BF16, [8, 128])
        xmt = carve(1024); x1 = carve(1024); xs = carve(512, BF16)
        Gst = carve(528, F32, [4, 132]); Cb = carve(264, BF16, [4, 132])
        offA = off[0]

        gsb = sg[:, 0:32]; e1 = sg[:, 32:48]; nlf = sg[:, 48:64]; t16 = sg[:, 64:80]; EK = sg[:, 80:96]; EB = sg[:, 96:112]
        A_t = [sg[:, 112:128], sg[:, 128:144]]; nhl16 = sg[:, 144:160].bitcast(BF16); nhi = nhl16[:, 0:16]; nlo = nhl16[:, 16:32]
        gsb3 = gsb.rearrange("p (j g) -> p j g", j=4)
        den = sm[:, 36:40]; dab = sm[:, 40:44]
        rr = sm[:, 44:48]; ssq = sm[:, 48:52]; mm4 = sm[:, 52:56]; scl = sm[:, 56:60]; st8 = sm[:, 60:68]
        mean = sm[:, 68:72]; msq = sm[:, 72:76]; var = sm[:, 76:80]; rs4 = sm[:, 80:84]
        ss2 = sm[:, 84:85]; l2 = sm[:, 85:86]; r2 = sm[:, 86:87]
        nhl = sm[:, 88:92].bitcast(BF16)
        psP = [psb[0], psb[1]]; psS = psb[3]; psO = [psb[4], psb[5]]; psU = [psb[6], psb[7]]
        psM = psb[2]
        psT = psM[:, 256:512].bitcast(BF16)
        Prog.BANK.clear()
        Prog.BANK.update({"psP0": "B0", "psP1": "B1", "psMg": "B2", "psMb": "B2", "psMl": "B2", "psT": "B2", "psS": "B3",
                          "psO0": "B4", "psO1": "B5", "psU0": "B6", "psU1": "B7",
                          "pq0": "B0", "pq1": "B1", "psc0": "B2", "psc1": "B3", "psc2": "B4", "psc3": "B5",
                          "pt6a": "B6", "pt6b": "B6", "pt6c": "B6",
                          "psOut0": "B0", "psOut1": "B1", "psOut2": "B2", "psOut3": "B3", "psA0": "B4", "psA1": "B5",
                          "psG0": "B6", "psG1": "B7"})
        pcnt = [0]
        def nextP():
            pcnt[0] += 1
            return psP[pcnt[0] % 2], "psP%d" % (pcnt[0] % 2)

        stg = [xTs.rearrange("p a b -> p (a b)"), raw.rearrange("p a b -> p (a b)")]
        for c in range(8):
            s = stg[c % 2][:, 0:3080]; sk = "stg%d" % (c % 2)
            b.dma("sp", s, win_d[:, c, :], [], [sk], sk)
            eng = "dve"
            b.cp(eng, Winb[:, c, 0:1540], s[:, 0:1540], [sk], ["Winb"])
            b.act(Winb[:, c, 1540:3080], s[:, 1540:3080], AF.Copy, [sk], ["Winb"])
        for hf in range(2):
            s = stg[hf][:, 0:4096]; sk = "stg%d" % hf
            b.dma("sp", s, wout_d[:, 4 * hf:4 * hf + 4, :], [], [sk], sk)
            for c4 in range(4):
                c = 4 * hf + c4
                if hf == 0:
                    b.ts("dve", Woutb[:, c, :], s[:, c4 * 1024:(c4 + 1) * 1024], col(C_NG + c), None, ALU.mult, None, [sk, "cs"], ["Woutb"])
                else:
                    b.cp("dve", Woutb[:, c, :], s[:, c4 * 1024:(c4 + 1) * 1024], [sk], ["Woutb"])
        b.cp("dve", WsTb.rearrange("p a b -> p (a b)"), cs[:, C_WS:C_WS + 512], ["cs"], ["WsTb"])
        for g in range(4):
            b.ms("dve", WsTb[64:128, g, 0:64], 0.0, ["WsTb"])
        b.ms("dve", raw.rearrange("p a b -> p (a b)"), 0.0, ["stg1"] + ["raw%d" % c for c in range(8)])
        b.ms("dve", Gst.rearrange("p a b -> p (a b)"), 0.0, ["G"])
        b.ms("dve", Cb.rearrange("p a b -> p (a b)"), 0.0, ["Cb"])
        b.ms("dve", A_t[1], 1.0, ["A1"])
        b.ms("dve", vext.rearrange("p a b -> p (a b)"), 0.0, ["vext"])

        for blk in range(int(os.environ.get("TRUNC", "8"))):
            main = blk >= 4
            tok0 = blk * 512
            for c in range(8):
                b.dma("sp", xTs[:, c, :], xT_d[:, c, tok0:tok0 + 512], [], ["xTs0", "stg0"] if c == 0 else ["xTs%d" % c], "xTs%d" % c)
            for c in range(8):
                b.act(sqb[:, c % 2, :], xTs[:, c, :], AF.Square, ["xTs%d" % c], ["sqb%d" % (c % 2)])
                b.mm(psS, onesb, sqb[:, c % 2, :], c == 0, c == 7, ["cb16", "sqb%d" % (c % 2)], ["psS"])
            b.act(lnv, psS, AF.Ln, ["psS", "cs"], ["lnv"], scale=1.0 / 1024, bias=eps_c)
            b.act(rstdb, lnv, AF.Exp, ["lnv"], ["rstdb"], scale=-0.5)
            for c in range(8):
                b.stt("dve", hT[:, c, :], xTs[:, c, :], col(C_G1 + c), rstdb, ALU.mult, ALU.mult, ["xTs%d" % c, "cs", "rstdb"], ["hT"])
            par = blk % 2
            for j in range(4):
                for c in range(8):
                    b.mm(psM[:, j * 8:(j + 1) * 8], hT[:, c, j * 128:(j + 1) * 128], Winb[:, c, 2048:2056], c == 0, c == 7, ["hT", "Winb"], ["psMg"])
            b.tt("dve", gsb3, psM[:, 0:32].rearrange("p (j g) -> p j g", j=4), col(C_GB, 8).unsqueeze(1).to_broadcast([128, 4, 8]), ALU.add,
                 ["psMg", "cs"], ["gsb"])
            b.act(e1.rearrange("p (j h) -> p j h", j=4), gsb3[:, :, 4:8], AF.Exp, ["gsb"], ["e1"], scale=-1.0)
            b.act(nlf, e1, AF.Ln, ["e1", "cs"], ["nlf"], scale=1.0, bias=one_c)
            b.cp("dve", nhi, nlf, ["nlf"], ["nhi"])
            b.tt("dve", nlo, nlf, nhi, ALU.subtract, ["nlf", "nhi"], ["nlo"])
            for j in range(4):
                js = slice(j * 4, (j + 1) * 4)
                b.mm(psM[:, 64 + j * 4:68 + j * 4], trib, nhi[:, js], True, False, ["cb16", "nhi"], ["psMb"])
                b.mm(psM[:, 64 + j * 4:68 + j * 4], trib, nlo[:, js], False, True, ["cb16", "nlo"], ["psMb"])
                b.mm(psM[:, 96 + j * 4:100 + j * 4], onesb, nhi[:, js], True, False, ["cb16", "nhi"], ["psMl"])
                b.mm(psM[:, 96 + j * 4:100 + j * 4], onesb, nlo[:, js], False, True, ["cb16", "nlo"], ["psMl"])
            b.tt("dve", t16.rearrange("p (j h) -> p j h", j=4), gsb3[:, :, 0:4], psM[:, 64:80].rearrange("p (j h) -> p j h", j=4), ALU.add,
                 ["gsb", "psMb"], ["t4"])
            b.act(EK, t16, AF.Exp, ["t4", "cs"], ["ek"], scale=1.0, bias=lns_c)
            b.act(EB, psM[:, 64:80], AF.Exp, ["psMb"], ["eb"], scale=-1.0)
            b.act(A_t[par], psM[:, 96:112], AF.Exp, ["psMl"], ["A%d" % par], scale=-1.0)
            if blk == 3:
                b.ts("dve", A_t[1][:, 12:16], A_t[1][:, 12:16], flag_c, None, ALU.mult, None, ["A1", "cs"], ["A1"])
            cc_list = list(range(8)) if blk >= 3 else list(range(4, 8))
            for cc in cc_list:
                ps, pk = nextP()
                for c in range(8):
                    b.mm(ps, Winb[:, c, cc * 128:(cc + 1) * 128], hT[:, c, :], c == 0, c == 7, ["Winb", "hT"], [pk])
                rk = "raw%d" % cc
                if blk == 4:
                    b.ts("dve", raw[:, cc, 0:3], raw[:, cc, 512:515], flag_c, None, ALU.mult, None, [rk, "cs"], [rk])
                elif blk > 0:
                    b.cp("dve", raw[:, cc, 0:3], raw[:, cc, 512:515], [rk], [rk])
                b.act(raw[:, cc, 3:515], ps, AF.Copy, [pk], [rk])
                if cc >= 4 or main:
                    ac = accs[:, cc % 2, :]; ak = "acc%d" % (cc % 2)
                    b.act(ac, raw[:, cc, 3:515], AF.Identity, [rk, "cs"], [ak], scale=col(C_CW + 3 * 8 + cc), bias=col(C_CB + cc))
                    for j in (2, 1, 0):
                        b.stt("dve", ac, raw[:, cc, j:j + 512], col(C_CW + j * 8 + cc), ac, ALU.mult, ALU.add, [rk, "cs", ak], [ak])
                    b.act(qkT[:, cc, :], ac, AF.Silu, [ak], ["qkT%d" % cc])
            for j in range(4):
                tsl = slice(j * 128, (j + 1) * 128)
                cidx = blk * 4 + j
                js = slice(j * 4, (j + 1) * 4)
                ek = EK[:, js]; eb = EB[:, js]
                acur = A_t[par][:, js]; ack = "A%d" % par
                if j > 0:
                    aprev = A_t[par][:, (j - 1) * 4:j * 4]; apk = ack
                else:
                    aprev = A_t[1 - par][:, 12:16]; apk = "A%d" % (1 - par)
                ps, pk = nextP()
                for c in range(8):
                    b.mm(ps, hT[:, c, tsl], Winb[:, c, 1024:1536], c == 0, c == 7, ["hT", "Winb"], [pk])
                for h in range(4):
                    b.act(vext[:, h, 0:128], ps[:, h * 128:(h + 1) * 128], AF.Copy, [pk, "ek"], ["vext"], scale=ek[:, h:h + 1])
                b.cp("dve", vext[:, :, 128], ek, ["ek"], ["vext"])
                for h in range(4):
                    b.tr(psT[:, h * 128:(h + 1) * 128], qkT[:, 4 + h, tsl], identb, ["qkT%d" % (4 + h), "cb16"], ["psT"])
                b.cp("dve", ktok, psT, ["psT"], ["ktok"])
                if main:
                    for h in range(4):
                        b.mm(psS[:, h * 128:(h + 1) * 128], qkT[:, 4 + h, tsl], qkT[:, h, tsl], True, True,
                             ["qkT%d" % (4 + h), "qkT%d" % h], ["psS"])
                    b.tt("dve", PT, psS.rearrange("p (a b) -> p a b", a=4), tri.unsqueeze(1).to_broadcast([128, 4, 128]),
                         ALU.mult, ["psS", "cs"], ["PT"])
                    for h in range(4):
                        o = psO[h // 2][:, (h % 2) * 256:(h % 2) * 256 + 129]; ok = "psO%d" % (h // 2)
                        b.mm(o, PT[:, h, :], vext[:, h, 0:129], True, False, ["PT", "vext"], [ok])
                        b.mm(o, qkT[:, h, tsl], Cb[:, h, 0:129], False, True, ["qkT%d" % h, "Cb"], [ok])
                for h in range(4):
                    u = psU[h // 2][:, (h % 2) * 256:(h % 2) * 256 + 129]; uk = "psU%d" % (h // 2)
                    b.mm(u, ktok[:, h * 128:(h + 1) * 128], vext[:, h, 0:129], True, True, ["ktok", "vext"], [uk])
                for h in range(4):
                    u = psU[h // 2][:, (h % 2) * 256:(h % 2) * 256 + 129]; uk = "psU%d" % (h // 2)
                    b.stt("dve", Gst[:, h, 0:129], Gst[:, h, 0:129], aprev[:, h:h + 1], u, ALU.mult, ALU.add, ["G", apk, uk], ["G"])
                    if cidx >= 15:
                        b.ts("dve", Cb[:, h, 0:129], Gst[:, h, 0:129], acur[:, h:h + 1], None, ALU.mult, None, ["G", ack], ["Cb"])
                if not main:
                    continue
                for p2 in range(2):
                    dv = psO[p2].rearrange("p (h w) -> p h w", h=2)[:, :, 128]
                    b.tt("dve", den[:, 2 * p2:2 * p2 + 2], dv, eb[:, 2 * p2:2 * p2 + 2], ALU.mult, ["psO%d" % p2, "eb"], ["den"])
                b.act(dab, den, AF.Abs, ["den"], ["dab"])
                b.ts("dve", dab, dab, 1.0, None, ALU.max, None, ["dab"], ["dab"])
                b.P.op("dve", lambda e: e.reciprocal(out=dab, in_=dab), reads=["dab"], writes=["dab"])
                b.tt("dve", rr, eb, dab, ALU.mult, ["eb", "dab"], ["rr"])
                b.ms("dve", ssq, 0.0, ["ssq"])
                for h in range(4):
                    o = psO[h // 2][:, (h % 2) * 256:(h % 2) * 256 + 128]
                    b.act(vn32[:, 0:128], o, AF.Square, ["psO%d" % (h // 2), "ssq"], ["vn32", "ssq"], accum=ssq[:, h:h + 1])
                b.tt("dve", mm4, ssq, rr, ALU.mult, ["ssq", "rr"], ["mm4"])
                b.tt("dve", mm4, mm4, rr, ALU.mult, ["mm4", "rr"], ["mm4"])
                b.act(mm4, mm4, AF.Ln, ["mm4", "cs"], ["mm4"], scale=1.0 / 128, bias=eps_c)
                b.act(mm4, mm4, AF.Exp, ["mm4"], ["mm4"], scale=-0.5)
                b.tt("dve", scl, rr, mm4, ALU.mult, ["rr", "mm4"], ["scl"])
                ps, pk = nextP()
                for c in range(8):
                    b.mm(ps, hT[:, c, tsl], Winb[:, c, 1536:2048], c == 0, c == 7, ["hT", "Winb"], [pk])
                b.act(sig, ps, AF.Sigmoid, [pk], ["sig"])
                for h in range(4):
                    o = psO[h // 2][:, (h % 2) * 256:(h % 2) * 256 + 128]
                    b.stt("dve", ybf[:, h * 128:(h + 1) * 128], o, scl[:, h:h + 1], sig[:, h * 128:(h + 1) * 128], ALU.mult, ALU.mult,
                          ["psO%d" % (h // 2), "scl", "sig"], ["ybf"])
                ps, pk = nextP()
                for c in range(8):
                    b.mm(ps, hT[:, c, tsl], Winb[:, c, 2056:2568], c == 0, c == 7, ["hT", "Winb"], [pk])
                b.act(u_sb, ps, AF.Gelu_apprx_tanh, [pk], ["u_sb"])
                ps, pk = nextP()
                for c in range(8):
                    b.mm(ps, hT[:, c, tsl], Winb[:, c, 2568:3080], c == 0, c == 7, ["hT", "Winb"], [pk])
                b.ms("dve", st8, 0.0, ["st8"])
                for g in range(4):
                    b.act(vgs[:, g * 128:(g + 1) * 128], ps[:, g * 128:(g + 1) * 128], AF.Gelu_apprx_tanh, [pk, "st8"], ["vgs", "st8"],
                          accum=st8[:, g:g + 1])
                for g in range(4):
                    b.act(vn32[:, g * 128:(g + 1) * 128], vgs[:, g * 128:(g + 1) * 128], AF.Square, ["vgs", "st8"], ["vn32", "st8"],
                          accum=st8[:, 4 + g:5 + g])
                b.ts("dve", mean, st8[:, 0:4], 1.0 / 128, None, ALU.mult, None, ["st8"], ["mean"])
                b.tt("dve", msq, mean, mean, ALU.mult, ["mean"], ["msq"])
                b.stt("dve", var, st8[:, 4:8], 1.0 / 128, msq, ALU.mult, ALU.subtract, ["st8", "msq"], ["var"])
                b.act(var, var, AF.Ln, ["var", "cs"], ["var"], scale=1.0, bias=eps_c)
                b.act(rs4, var, AF.Exp, ["var"], ["rs4"], scale=-0.5)
                for g in range(4):
                    b.ts("dve", vn32[:, g * 128:(g + 1) * 128], vgs[:, g * 128:(g + 1) * 128], mean[:, g:g + 1], rs4[:, g:g + 1],
                         ALU.subtract, ALU.mult, ["vgs", "mean", "rs4"], ["vn32"])
                b.tt("dve", vn32, vn32, cs[:, C_LNG:C_LNG + 512], ALU.mult, ["vn32", "cs"], ["vn32"])
                b.tt("dve", vnb, vn32, cs[:, C_LNB:C_LNB + 512], ALU.add, ["vn32", "cs"], ["vnb"])
                for g in range(4):
                    b.mm(psS[:, g * 128:(g + 1) * 128], WsTb[:, g, :], vnb[:, g * 128:(g + 1) * 128], True, True, ["WsTb", "vnb"], ["psS"])
                for g in range(4):
                    b.stt("dve", ybf[:, 512 + g * 128:512 + (g + 1) * 128], psS[:, g * 128:(g + 1) * 128], col(C_BS + g),
                          u_sb[:, g * 128:(g + 1) * 128], ALU.add, ALU.mult, ["psS", "cs", "u_sb"], ["ybf"])
                for hf in range(2):
                    for q in range(4):
                        b.tr(psT[:, q * 128:(q + 1) * 128], ybf[:, (hf * 4 + q) * 128:(hf * 4 + q + 1) * 128], identb, ["ybf", "cb16"], ["psT"])
                    b.cp("dve", yT[:, hf * 4:hf * 4 + 4, :].rearrange("p a b -> p (a b)"), psT, ["psT"], ["yT"])
                for hf in range(2):
                    for c in range(8):
                        b.mm(psO[hf], yT[:, c, :], Woutb[:, c, hf * 512:(hf + 1) * 512], c == 0, c == 7, ["yT", "Woutb"], ["psO%d" % hf])
                mt0 = (blk - 4) * 512 + j * 128
                b.dma("sp", xmt, xm_d[mt0:mt0 + 128, :], [], ["xmt"], "xmt")
                for hf in range(2):
                    b.tt("dve", x1[:, hf * 512:(hf + 1) * 512], psO[hf], xmt[:, hf * 512:(hf + 1) * 512], ALU.add, ["psO%d" % hf, "xmt"], ["x1"])
                b.dma("sp", x1_d[mt0:mt0 + 128, :], x1, ["x1"], ["x1d%d" % (mt0 // 128)], "x1st")
                if stage == "A":
                    continue
                b.ms("dve", ss2, 0.0, ["ss2"])
                b.act(xs, x1, AF.Square, ["x1", "ss2"], ["xs", "ss2"], accum=ss2)
                b.act(l2, ss2, AF.Ln, ["ss2", "cs"], ["l2"], scale=1.0 / 1024, bias=eps_c)
                b.act(r2, l2, AF.Exp, ["l2"], ["r2"], scale=-0.5)
                b.act(xs, x1, AF.Copy, ["x1", "r2"], ["xs"], scale=r2)
                for hf in range(2):
                    for q in range(4):
                        b.tr(psT[:, q * 128:(q + 1) * 128], xs[:, (hf * 4 + q) * 128:(hf * 4 + q + 1) * 128], identb, ["xs", "cb16"], ["psT"])
                    for q in range(4):
                        b.ts("dve", xn2T[:, hf * 4 + q, mt0:mt0 + 128], psT[:, q * 128:(q + 1) * 128], col(C_G2 + hf * 4 + q), None,
                             ALU.mult, None, ["psT", "cs"], ["xn2T"])
        fkeys = ["x1d%d" % i for i in range(16)]
        if stage not in ("A", "A2"):
            fkeys = build_peer(nc, b, carve, off, cs, xn2T, identb, ident, iotab, psb, wq_d, keys_d, ut_d, v_d, x1_d, y_d, stage, ub_d, vb_d)
        P.emit(final_keys=fkeys)
    return nc


import os


def build_peer(nc, b, carve, off, cs, xn2T, identb, ident, iota, psb, wq_d, keys_d, ut_d, v_d, x1_d, y_d, stage='full', ub_d=None, vb_d=None):
    P = b.P
    P.fence()
    off[0] = 0
    col = lambda c, n=1: cs[:, c:c + n]
    eps_c = col(C_EPS)
    I1T = carve(2048); I2T = carve(2048); GTs = carve(2048)
    offBC = off[0]
    Wqb = carve(8192, BF16, [8, 2048]); keysTb = carve(1024, BF16, [16, 128]); stg = carve(2048)
    qTb = carve(1024, BF16, [16, 128]); sc2 = carve(4096, F32, [2, 16, 128]); wk = carve(2048, F32, [16, 128])
    m16 = carve(256, F32, [16, 16]); i16 = carve(256, U32, [16, 16]); i16f = carve(256, F32, [16, 16])
    NC = 80; RECTS = [(0, 2, 16, 0), (2, 2, 8, 32), (4, 4, 4, 48), (8, 8, 2, 64)]
    cand = carve(8 * NC, F32, [8, NC]); cidx = carve(8 * NC, F32, [8, NC]); junk = carve(4 * NC, F32, [4, NC])
    ts16 = carve(128, F32, [8, 16]); eidx = carve(128); ex = carve(128, F32, [8, 16]); gate = carve(128, F32, [8, 16])
    ei = carve(128, I32); i1i = carve(128, I32); i2i = carve(128, I32); i1f = carve(128); i2f = carve(128)
    tb3 = carve(192, BF16)
    smb = carve(32); negm = smb[:, 0:8]; Z = smb[:, 8:16]; rz = smb[:, 16:24]
    cin = carve(6144, F32, [3, 2, 1024]); cout = carve(3072, BF16, [3, 2, 1024])
    jobs = {"in": 0, "cv": 0}

    def conv_in():
        j = jobs["in"]
        if j >= 128: return
        jobs["in"] += 1; s3 = j % 3
        b.dma("sp", cin[:, s3, 0, :], ut_d[j].rearrange("p c e -> p (c e)"), [], ["cinu%d" % s3], "cinu%d" % s3)
        b.dma("sp", cin[:, s3, 1, :], v_d[j * 128:(j + 1) * 128, :], [], ["cinv%d" % s3], "cinv%d" % s3)

    def conv_cv():
        j = jobs["cv"]
        if j >= 128: return
        jobs["cv"] += 1; s3 = j % 3
        b.act(cout[:, s3, 0, :], cin[:, s3, 0, :], AF.Copy, ["cinu%d" % s3], ["coutu%d" % s3])
        b.act(cout[:, s3, 1, :], cin[:, s3, 1, :], AF.Copy, ["cinv%d" % s3], ["coutv%d" % s3])
        b.dma("sp", ub_d[j], cout[:, s3, 0, :], ["coutu%d" % s3], ["ubd"], "cou%d" % s3)
        b.dma("sp", vb_d[j * 128:(j + 1) * 128, :], cout[:, s3, 1, :], ["coutv%d" % s3], ["vbd"], "cov%d" % s3)

    conv_in(); conv_in()
    for c in range(8):
        b.dma("sp", stg, wq_d[:, c, :], [], ["stgB"], "stgB")
        b.cp("dve", Wqb[:, c, 0:1024], stg[:, 0:1024], ["stgB"], ["Wqb"])
        b.act(Wqb[:, c, 1024:2048], stg[:, 1024:2048], AF.Copy, ["stgB"], ["Wqb"])
    b.dma("sp", stg, keys_d[:, :, :].rearrange("p a b -> p (a b)"), [], ["stgB"], "stgB")
    b.cp("dve", keysTb.rearrange("p a b -> p (a b)"), stg, ["stgB"], ["keysTb"])
    PB = int(os.environ.get("PB_STOP", "99"))
    if PB <= 0: return ["x1d%d" % q for q in range(16)]
    def part1(i):
        t0 = i * 128; par = i % 2; sc = sc2[:, par]
        for hp in range(16):
            ps = psb[hp % 2]; pk = "pq%d" % (hp % 2)
            for c in range(8):
                b.mm(ps[:, 0:128], Wqb[:, c, hp * 128:(hp + 1) * 128], xn2T[:, c, t0:t0 + 128], c == 0, c == 7, ["Wqb", "xn2T"], [pk])
            b.act(qTb[:, hp, :], ps[:, 0:128], AF.Copy, [pk], ["qTb%d" % hp])
            if hp % 2 == 0:
                conv_in(); conv_cv()
        for hp in range(16):
            b.mm(psb[2 + hp // 4][:, (hp % 4) * 128:(hp % 4 + 1) * 128], qTb[:, hp, :], keysTb[:, hp, :], True, True,
                 ["qTb%d" % hp, "keysTb"], ["psc%d" % (hp // 4)])
        for q4 in range(4):
            b.act(sc[:, q4 * 4:q4 * 4 + 4, :].rearrange("p a b -> p (a b)"), psb[2 + q4], AF.Copy, ["psc%d" % q4], ["sc%d_%d" % (q4, par)])

    def part2(i):
        t0 = i * 128; par = i % 2; sc = sc2[:, par]
        for hp in range(16):
            b.P.op("dve", (lambda hp: lambda e: e.max(out=m16[:, hp, 0:8], in_=sc[:, hp, :]))(hp), reads=["sc%d_%d" % (hp // 4, par)], writes=["m16_%d" % hp])
        for hp in range(16):
            b.P.op("dve", (lambda hp: lambda e: e.max_index(out=i16[:, hp, 0:8], in_max=m16[:, hp, 0:8], in_values=sc[:, hp, :]))(hp),
                   reads=["sc%d_%d" % (hp // 4, par), "m16_%d" % hp], writes=["i16_%d" % hp])
        for hp in range(16):
            b.P.op("dve", (lambda hp: lambda e: e.match_replace(out=wk[:, hp, :], in_to_replace=m16[:, hp, 0:8], in_values=sc[:, hp, :], imm_value=-1e30))(hp),
                   reads=["sc%d_%d" % (hp // 4, par), "m16_%d" % hp], writes=["wk%d" % hp])
        for hp in range(16):
            b.P.op("dve", (lambda hp: lambda e: e.max(out=m16[:, hp, 8:16], in_=wk[:, hp, :]))(hp), reads=["wk%d" % hp], writes=["m16_%d" % hp])
        for hp in range(16):
            b.P.op("dve", (lambda hp: lambda e: e.max_index(out=i16[:, hp, 8:16], in_max=m16[:, hp, 8:16], in_values=wk[:, hp, :]))(hp),
                   reads=["wk%d" % hp, "m16_%d" % hp], writes=["i16_%d" % hp])
        allm = ["m16_%d" % hp for hp in range(16)]; alli = ["i16_%d" % hp for hp in range(16)]
        b.cp("dve", i16f.rearrange("p a b -> p (a b)"), i16.rearrange("p a b -> p (a b)"), alli, ["i16f"])
        m16h = m16.rearrange("p (h two) a -> p h two a", two=2)
        i16h = i16f.rearrange("p (h two) a -> p h two a", two=2)
        allc = ["cand%d" % h for h in range(8)]; allx = ["cidx%d" % h for h in range(8)]
        b.ts("dve", i16h[:, :, 0, :], i16h[:, :, 0, :], 128.0, None, ALU.mult, None, ["i16f"], ["i16f"])
        for (a0, na, nb, o0) in RECTS:
            shp = [128, 8, na, nb]
            b.tt("dve", cand[:, :, o0:o0 + na * nb].rearrange("p h (a c) -> p h a c", a=na),
                 m16h[:, :, 0, a0:a0 + na].unsqueeze(3).to_broadcast(shp), m16h[:, :, 1, 0:nb].unsqueeze(2).to_broadcast(shp),
                 ALU.add, allm, allc)
            b.tt("dve", cidx[:, :, o0:o0 + na * nb].rearrange("p h (a c) -> p h a c", a=na),
                 i16h[:, :, 0, a0:a0 + na].unsqueeze(3).to_broadcast(shp), i16h[:, :, 1, 0:nb].unsqueeze(2).to_broadcast(shp),
                 ALU.add, ["i16f"], allx)
        wk2 = wk.rearrange("p a b -> p (a b)").rearrange("p (a b) -> p a b", a=8)[:, :, 0:NC]
        for h in range(8):
            b.P.op("dve", (lambda h: lambda e: e.max(out=ts16[:, h, 0:8], in_=cand[:, h, :]))(h), reads=["cand%d" % h], writes=["ts16_%d" % h])
        for h in range(8):
            b.P.op("dve", (lambda h: lambda e: e.match_replace(out=wk2[:, h, :], in_to_replace=ts16[:, h, 0:8], in_values=cand[:, h, :], imm_value=-1e30))(h),
                   reads=["cand%d" % h, "ts16_%d" % h] + ["wk%d" % (2 * h), "wk%d" % (2 * h + 1)], writes=["wk%d" % (2 * h), "wk%d" % (2 * h + 1)])
        for h in range(8):
            b.P.op("dve", (lambda h: lambda e: e.max(out=ts16[:, h, 8:16], in_=wk2[:, h, :]))(h), reads=["wk%d" % (2 * h), "wk%d" % (2 * h + 1)], writes=["ts16_%d" % h])
        b.ms("dve", eidx, 0.0, ["eidx"])
        if os.environ.get("TRIV"):
            for q in range(int(os.environ["TRIV"])):
                b.ms("dve", junk[:, q % 4, :], 0.0, ["junk%d" % (q % 4)])
            return ["x1d%d" % q for q in range(16)]
        for j in range(16):
            for h in range(8):
                jk = "junk%d" % ((j * 8 + h) % 4)
                b.stt("dve", junk[:, (j * 8 + h) % 4, :], cand[:, h, :], ts16[:, h, j:j + 1], cidx[:, h, :], ALU.is_equal, ALU.mult,
                      ["cand%d" % h, "ts16_%d" % h, "cidx%d" % h, "eidx"], [jk, "eidx%d" % (h * 16 + j)], accum=(None if os.environ.get("NOACC") else eidx[:, h * 16 + j:h * 16 + j + 1]))
        alle = ["eidx"] + ["eidx%d" % q for q in range(128)]
        allts = ["ts16_%d" % h for h in range(8)]
        b.ts("dve", negm, ts16[:, :, 0], -1.0, None, ALU.mult, None, allts, ["negm"])
        b.ms("dve", Z, 0.0, ["Z"])
        for h in range(8):
            b.act(ex[:, h, :], ts16[:, h, :], AF.Exp, allts + ["negm", "Z"], ["ex", "Z"], scale=1.0, bias=negm[:, h:h + 1], accum=Z[:, h:h + 1])
        b.P.op("dve", lambda e: e.reciprocal(out=rz, in_=Z), reads=["Z"], writes=["rz"])
        b.tt("dve", gate, ex, rz.unsqueeze(2).to_broadcast([128, 8, 16]), ALU.mult, ["ex", "rz"], ["gate"])
        b.cp("dve", ei, eidx, alle, ["ei"])
        b.ts("dve", i1i, ei, 7, None, ALU.arith_shift_right, None, ["ei"], ["i1i"])
        b.ts("dve", i2i, ei, 127, None, ALU.bitwise_and, None, ["ei"], ["i2i"])
        b.cp("dve", tb3[:, 0:128], i1i, ["i1i"], ["tb3a"])
        b.cp("dve", tb3[:, 128:256], i2i, ["i2i"], ["tb3b"])
        b.cp("dve", tb3[:, 256:384], gate.rearrange("p a b -> p (a b)"), ["gate"], ["tb3c"])
        pT6 = psb[6].bitcast(BF16)
        b.tr(pT6[:, 0:128], tb3[:, 0:128], identb, ["tb3a"], ["pt6a"])
        b.tr(pT6[:, 128:256], tb3[:, 128:256], identb, ["tb3b"], ["pt6b"])
        b.tr(pT6[:, 256:384], tb3[:, 256:384], identb, ["tb3c"], ["pt6c"])
        b.act(I1T[:, t0:t0 + 128], pT6[:, 0:128], AF.Copy, ["pt6a"], ["I1T"])
        b.act(I2T[:, t0:t0 + 128], pT6[:, 128:256], AF.Copy, ["pt6b"], ["I2T"])
        b.act(GTs[:, t0:t0 + 128], pT6[:, 256:384], AF.Copy, ["pt6c"], ["GTs"])
    part1(0)
    for i in range(16):
        if i + 1 < 16: part1(i + 1)
        part2(i)
    while jobs["cv"] < 128:
        conv_in(); conv_cv()
    if stage == "AB":
        return ["x1d%d" % i for i in range(16)]
    P.fence()
    off[0] = offBC
    GT = carve(16384, BF16, [128, 256]); ohA = carve(256, BF16, [4, 128]); ohB = carve(256, BF16, [4, 128])
    NSL = 4
    Ub = carve(NSL * 512, BF16, [NSL, 8, 128]); Vb = carve(NSL * 512, BF16, [NSL, 1024])
    Ab = carve(256, BF16, [2, 256]); AG = carve(256, BF16, [2, 256])
    x1t = carve(1024); x2 = carve(1024); ot = carve(1024); smc = carve(16)
    ssf = smc[:, 0:1]; lf_ = smc[:, 1:2]; rf = smc[:, 2:3]
    psOut = psb[0:4]; psA = psb[4:6]; psG = psb[6:8]
    fkeys = []
    def gbuild(T, half, grp):
        h0 = 64 * half; g = grp % 2
        for s4 in range(4):
            t = T * 256 + grp * 4 + s4
            b.ts("dve", ohA[:, s4, 0:64], iota[:, h0:h0 + 64], I1T[:, t:t + 1], GTs[:, t:t + 1], ALU.is_equal, ALU.mult,
                 ["cb16", "I1T", "GTs"], ["ohA%d" % s4])
            b.ts("dve", ohB[:, s4, :], iota, I2T[:, t:t + 1], None, ALU.is_equal, None, ["cb16", "I2T"], ["ohB%d" % s4])
            b.mm(psG[g][:, s4 * 64:(s4 + 1) * 64], ohB[:, s4, :], ohA[:, s4, 0:64], True, True, ["ohA%d" % s4, "ohB%d" % s4], ["psG%d" % g])
        b.act(GT[:, h0:h0 + 64, grp * 4:grp * 4 + 4], psG[g][:, 0:256].rearrange("p (t i) -> p i t", t=4), AF.Copy,
              ["psG%d" % g], ["GT%d" % half])

    for grp in range(64):
        gbuild(0, 0, grp)
    for T in range(8):
        tb = T * 256

        def stage1(blk):
            s4 = blk % NSL; s2 = blk % 2
            b.dma("sp", Ub[:, s4, :, :].rearrange("p a b -> p (a b)"), ub_d[blk], ["ubd"], ["Ub%d" % s4], "Ub%d" % s4)
            b.dma("sp", Vb[:, s4, :], vb_d[blk * 128:(blk + 1) * 128, :], ["vbd"], ["Vb%d" % s4], "Vb%d" % s4)
            for c in range(8):
                b.mm(psA[s2][:, 0:256], Ub[:, s4, c, :], xn2T[:, c, tb:tb + 256], c == 0, c == 7, ["Ub%d" % s4, "xn2T"], ["psA%d" % s2])

        def stage2(blk):
            s2 = blk % 2; s4 = blk % NSL
            b.act(Ab[:, s2, :], psA[s2][:, 0:256], AF.Gelu_apprx_tanh, ["psA%d" % s2], ["Ab%d" % s2])
            b.tt("dve", AG[:, s2, :], Ab[:, s2, :], GT[:, blk, :], ALU.mult, ["Ab%d" % s2, "GT%d" % (blk // 64)], ["AG%d" % s2])
            for sub in range(2):
                for hf in range(2):
                    b.mm(psOut[sub * 2 + hf], AG[:, s2, sub * 128:(sub + 1) * 128], Vb[:, s4, hf * 512:(hf + 1) * 512], blk == 0, blk == 127,
                         ["AG%d" % s2, "Vb%d" % s4], ["psOut%d" % (sub * 2 + hf)])

        for blk in range(129):
            if blk < 128: stage1(blk)
            if blk >= 1: stage2(blk - 1)
            if blk < 64:
                gbuild(T, 1, blk)
            elif blk < 128 and T + 1 < 8:
                gbuild(T + 1, 0, blk - 64)
        for sub in range(2):
            r0 = tb + sub * 128
            b.dma("sp", x1t, x1_d[r0:r0 + 128, :], ["x1d%d" % (r0 // 128)], ["x1t"], "x1t")
            for hf in range(2):
                b.tt("dve", x2[:, hf * 512:(hf + 1) * 512], psOut[sub * 2 + hf], x1t[:, hf * 512:(hf + 1) * 512], ALU.add,
                     ["psOut%d" % (sub * 2 + hf), "x1t"], ["x2"])
            b.ms("dve", ssf, 0.0, ["ssf"])
            b.act(ot, x2, AF.Square, ["x2", "ssf"], ["ot", "ssf"], accum=ssf)
            b.act(lf_, ssf, AF.Ln, ["ssf", "cs"], ["lf_"], scale=1.0 / 1024, bias=eps_c)
            b.act(rf, lf_, AF.Exp, ["lf_"], ["rf"], scale=-0.5)
            b.stt("dve", ot, x2, rf, cs[:, C_FGB:C_FGB + 1024], ALU.mult, ALU.mult, ["x2", "rf", "cs", "ot"], ["ot"])
            k = "yd%d" % (r0 // 128)
            b.dma("sp", y_d[r0:r0 + 128, :], ot, ["ot"], [k], "yst")
            fkeys.append(k)
    return fkeys


def host_inputs(inp):
    f = lambda a: np.ascontiguousarray(a, dtype=np.float32)
    x = inp["x"]
    consts = np.zeros((128, C_TOT), np.float32)
    cw = inp["conv_w"][0]
    consts[:, C_CW:C_CW + 32] = cw.reshape(4, 8, 128).transpose(2, 0, 1).reshape(128, 32)
    consts[:, C_CB:C_CB + 8] = inp["conv_b"][0].reshape(8, 128).T
    consts[:, C_G1:C_G1 + 8] = inp["norm1_g"][0].reshape(8, 128).T
    consts[:, C_G2:C_G2 + 8] = inp["norm2_g"][0].reshape(8, 128).T
    consts[:, C_NG:C_NG + 4] = inp["mlstm_norm_g"][0].reshape(4, 128).T
    consts[:, C_GB:C_GB + 4] = inp["b_igate"][0][None, :]
    consts[:, C_GB + 4:C_GB + 8] = inp["b_fgate"][0][None, :]
    consts[:, C_BS:C_BS + 4] = inp["gmlp_b_s"][0].T
    consts[:, C_EPS] = EPS; consts[:, C_ONE] = 1.0; consts[:, C_LNS] = np.float32(np.log(128.0 ** -0.5))
    consts[:, C_LNG:C_LNG + 512] = inp["gmlp_ln_g"][0][None, :]
    consts[:, C_LNB:C_LNB + 512] = inp["gmlp_ln_b"][0][None, :]
    consts[:, C_FGB:C_FGB + 1024] = inp["final_g"][None, :]
    consts[:, C_CST:C_CST + 128] = np.eye(128)
    consts[:, C_CST + 128:C_CST + 256] = np.triu(np.ones((128, 128)))
    consts[:, C_CST + 256:C_CST + 384] = 1.0
    consts[:, C_CST + 384:C_CST + 512] = np.arange(128)[None, :]
    consts[:, C_WS:C_WS + 512] = inp["gmlp_w_s"][0].transpose(2, 0, 1).reshape(128, 512)
    shared = {
        "win": f(inp["w_in"][0].reshape(8, 128, 3080).transpose(1, 0, 2)),
        "wout": f(inp["w_out"][0].reshape(8, 128, 1024).transpose(1, 0, 2)),
        "wq": f(inp["peer_w_query"][0].reshape(8, 128, 2048).transpose(1, 0, 2)),
        "keysT": f(inp["peer_sub_keys"][0].reshape(16, 128, 128).transpose(2, 0, 1)),
        "UT": f(inp["peer_u"][0].reshape(128, 128, 8, 128).transpose(0, 3, 2, 1)),
        "V": f(inp["peer_v"][0]),
    }
    maps = []
    for core in range(NCORES):
        bi, s = core // 2, core % 2
        xm = x[bi, s * NT:(s + 1) * NT]
        pre = x[bi, 0:NT]
        toks = np.concatenate([pre, xm], axis=0)
        xT = f(toks.T.reshape(8, 128, 4096).transpose(1, 0, 2))
        c = consts.copy(); c[:, C_FLAG] = float(s)
        m = dict(shared); m.update({"xT": xT, "xm": f(xm), "consts": c})
        maps.append(m)
    return maps


_NC_CACHE = {}


def kernel(**inputs):
    stage = "full"
    if stage not in _NC_CACHE:
        _NC_CACHE[stage] = build(stage)
    nc = _NC_CACHE[stage]
    maps = host_inputs({k: np.asarray(v) for k, v in inputs.items()})
    res = run_bass_kernel_spmd(nc, maps, core_ids=list(range(NCORES)))
    out = np.empty((4, 4096, 1024), np.float32)
    for core in range(NCORES):
        bi, s = core // 2, core % 2
        out[bi, s * NT:(s + 1) * NT] = res.results[core]["y"]
    return out
```

```python
import contextlib
import os
import numpy as np
import concourse.bass as bass
import concourse.mybir as mybir
from concourse.bass_utils import run_bass_kernel_spmd

F32 = mybir.dt.float32; BF16 = mybir.dt.bfloat16; U32 = mybir.dt.uint32; I32 = mybir.dt.int32
AF = mybir.ActivationFunctionType; ALU = mybir.AluOpType

NCORES = 8
NT = 2048
D = 1024
EPS = 1e-6
C_CW = 0; C_CB = 32; C_G1 = 40; C_G2 = 48; C_NG = 56; C_GB = 60; C_BS = 68; C_FLAG = 72
C_EPS = 73; C_ONE = 74; C_LNS = 75; C_SMALL = 80
C_LNG = 80; C_LNB = C_LNG + 512; C_FGB = C_LNB + 512; C_CST = C_FGB + 1024; C_WS = C_CST + 512
C_TOT = C_WS + 512


class Prog:
    ENG = ("pe", "act", "dve", "pool", "sp")

    def __init__(self, nc):
        self.nc = nc; self.ops = []; self.lastw = {}; self.readers = {}; self.dma_cnt = {}

    BANK = {}

    def op(self, eng, fn, reads=(), writes=(), dma=None):
        i = len(self.ops)
        reads = list(reads) + [self.BANK[k] for k in reads if k in self.BANK]
        writes = list(writes) + [self.BANK[k] for k in writes if k in self.BANK]
        deps = set()
        for k in list(reads) + list(writes):
            if k in self.lastw: deps.add(self.lastw[k])
        for k in writes:
            lastr = {}
            for r in self.readers.get(k, ()):
                ro = self.ops[r]
                if ro["dma"] is not None: deps.add(r)
                else: lastr[ro["eng"]] = r
            deps.update(lastr.values())
        o = dict(eng=eng, fn=fn, deps=deps, dma=dma, sig=False, dcount=None, sidx=None)
        if dma is not None:
            self.dma_cnt[dma] = self.dma_cnt.get(dma, 0) + 1
            o["dcount"] = self.dma_cnt[dma]
        self.ops.append(o)
        for k in writes:
            self.lastw[k] = i; self.readers[k] = []
        for k in reads:
            self.readers.setdefault(k, []).append(i)
        return i

    def fence(self):
        last = {}
        for i, o in enumerate(self.ops):
            if o["fn"] is None: continue
            if o["dma"] is not None: last[("d", o["dma"])] = i
            else: last[("e", o["eng"])] = i
        for e in self.ENG:
            i = self.op(e, None)
            self.ops[i]["deps"] = set(last.values())

    def emit(self, final_keys=()):
        nc = self.nc; ops = self.ops
        if not os.environ.get("NOFENCE"):
            self.fence()
        self.op("sp", None, reads=list(final_keys), writes=["__final"])
        pos = {e: 0 for e in self.ENG}
        for o in ops:
            if o["fn"] is not None:
                pos[o["eng"]] += 1
            o["epos"] = pos[o["eng"]]

        def needs(o, od):
            if od["dma"] is not None: return True
            if od["eng"] == o["eng"] and o["fn"] is not None and o["dma"] is None:
                if o["eng"] == "pe": return False
                return o["epos"] - od["epos"] < int(os.environ.get("NEEDS_DIST", "4"))
            return True
        self.needs = needs
        for o in ops:
            for d in o["deps"]:
                od = ops[d]
                if od["dma"] is None and needs(o, od):
                    od["sig"] = True
        cnt = {e: 0 for e in self.ENG}
        for o in ops:
            if o["dma"] is None and o["sig"] and o["fn"] is not None:
                cnt[o["eng"]] += 1; o["sidx"] = cnt[o["eng"]]
        self.stats = dict(nops=len(ops), sig=cnt, dma=dict(self.dma_cnt))
        with contextlib.ExitStack() as st:
            esem = {e: st.enter_context(nc.semaphore("s_" + e)) for e in self.ENG}
            dsem = {k: st.enter_context(nc.semaphore("d_" + str(k))) for k in self.dma_cnt}
            block = st.enter_context(nc.Block())

            def expand(o, acc, seen):
                for d in o["deps"]:
                    if d in seen: continue
                    seen.add(d)
                    od = ops[d]
                    if od["fn"] is None:
                        expand(od, acc, seen)
                    elif od["dma"] is not None:
                        acc.append((("d", od["dma"]), 16 * od["dcount"]))
                    elif self.needs(o, od):
                        acc.append((("e", od["eng"]), od["sidx"]))

            def run(ename, eng):
                waited = {}
                for o in ops:
                    if o["eng"] != ename: continue
                    acc = []
                    expand(o, acc, set())
                    best = {}
                    for k, v in acc:
                        if v is not None and v > best.get(k, 0): best[k] = v
                    for k, v in best.items():
                        if waited.get(k, 0) >= v: continue
                        waited[k] = v
                        eng.wait_ge(esem[k[1]] if k[0] == "e" else dsem[k[1]], v)
                    if o["fn"] is None: continue
                    ins = o["fn"](eng)
                    if o["dma"] is not None:
                        ins.then_inc(dsem[o["dma"]], 16)
                    elif o["sig"]:
                        ins.then_inc(esem[ename], 1)

            @block.tensor
            def _(e): run("pe", e)

            @block.scalar
            def _(e): run("act", e)

            @block.vector
            def _(e): run("dve", e)

            @block.gpsimd
            def _(e): run("pool", e)

            @block.sync
            def _(e): run("sp", e)


class Bld:
    def __init__(self, nc):
        self.P = Prog(nc)

    def mm(self, out, lhsT, rhs, start, stop, r, w):
        self.P.op("pe", lambda e: e.matmul(out, lhsT=lhsT, rhs=rhs, start=start, stop=stop), reads=r, writes=w)

    def tr(self, out, in_, ident, r, w):
        self.P.op("pe", lambda e: e.transpose(out=out, in_=in_, identity=ident), reads=r, writes=w)

    def act(self, out, in_, func, r, w, scale=None, bias=None, accum=None):
        kw = {}
        if scale is not None: kw["scale"] = scale
        if bias is not None: kw["bias"] = bias
        if accum is not None: kw["accum_out"] = accum
        self.P.op("act", lambda e: e.activation(out=out, in_=in_, func=func, **kw), reads=r, writes=w)

    def tt(self, eng, out, in0, in1, op, r, w):
        self.P.op(eng, lambda e: e.tensor_tensor(out=out, in0=in0, in1=in1, op=op), reads=r, writes=w)

    def ts(self, eng, out, in0, s1, s2, op0, op1, r, w):
        if s2 is None:
            self.P.op(eng, lambda e: e.tensor_scalar(out=out, in0=in0, scalar1=s1, scalar2=None, op0=op0), reads=r, writes=w)
        else:
            self.P.op(eng, lambda e: e.tensor_scalar(out=out, in0=in0, scalar1=s1, scalar2=s2, op0=op0, op1=op1), reads=r, writes=w)

    def stt(self, eng, out, in0, scalar, in1, op0, op1, r, w, accum=None):
        kw = {} if accum is None else {"accum_out": accum}
        self.P.op(eng, lambda e: e.scalar_tensor_tensor(out=out, in0=in0, scalar=scalar, in1=in1, op0=op0, op1=op1, **kw), reads=r, writes=w)

    def cp(self, eng, out, in_, r, w):
        self.P.op(eng, lambda e: e.tensor_copy(out=out, in_=in_), reads=r, writes=w)

    def ms(self, eng, out, val, w):
        self.P.op(eng, lambda e: e.memset(out, val), writes=w)

    def dma(self, q, out, in_, r, w, name):
        self.P.op(q, lambda e: e.dma_start(out=out, in_=in_), reads=r, writes=w, dma=name)


def build(stage="full"):
    nc = bass.Bass("TRN2", target_bir_lowering=False)
    dt = lambda name, shape, kind="ExternalInput", dty=F32: nc.dram_tensor(name, shape, dty, kind=kind).ap()
    xT_d = dt("xT", [128, 8, 4096]); xm_d = dt("xm", [NT, D]); win_d = dt("win", [128, 8, 3080])
    wout_d = dt("wout", [128, 8, 1024]); cst_d = dt("consts", [128, C_TOT])
    if stage not in ("A", "A2"):
        wq_d = dt("wq", [128, 8, 2048]); keys_d = dt("keysT", [128, 16, 128])
        ut_d = dt("UT", [128, 128, 8, 128]); v_d = dt("V", [16384, 1024])
    y_d = dt("y", [NT, D], kind="ExternalOutput")
    x1_d = dt("x1s", [NT, D], kind="Internal") if stage not in ("A", "A2") else y_d
    if stage not in ("A", "A2"):
        ub_d = dt("ubs", [128, 128, 1024], kind="Internal", dty=BF16)
        vb_d = dt("vbs", [16384, 1024], kind="Internal", dty=BF16)

    b = Bld(nc); P = b.P
    with contextlib.ExitStack() as st:
        NW = 41400
        cs = st.enter_context(nc.sbuf_tensor("cs", [128, C_TOT], F32))
        xn2T = st.enter_context(nc.sbuf_tensor("xn2T", [128, 8, NT], BF16))
        cb16 = st.enter_context(nc.sbuf_tensor("cb16", [128, 512], BF16))
        ar = st.enter_context(nc.sbuf_tensor("arena", [128, NW], F32))
        psb = [st.enter_context(nc.psum_tensor("psb%d" % i, [128, 512], F32))[:, :] for i in range(8)]
        identb = cb16[:, 0:128]; onesb = cb16[:, 128:256]; trib = cb16[:, 256:384]; iotab = cb16[:, 384:512]
        ident = cs[:, C_CST:C_CST + 128]; tri = cs[:, C_CST + 128:C_CST + 256]
        onesf = cs[:, C_CST + 256:C_CST + 384]; iota = cs[:, C_CST + 384:C_CST + 512]
        col = lambda c, n=1: cs[:, c:c + n]
        eps_c = col(C_EPS); one_c = col(C_ONE); lns_c = col(C_LNS); flag_c = col(C_FLAG)

        off = [0]
        def carve(words, dty=F32, shape=None):
            a = ar[:, off[0]:off[0] + words]; off[0] += words
            assert off[0] <= NW, off[0]
            if dty != F32: a = a.bitcast(dty)
            if shape is not None:
                names = " ".join("d%d" % i for i in range(len(shape)))
                a = a.rearrange("p (%s) -> p %s" % (names, names), **{"d%d" % i: s for i, s in enumerate(shape)})
            return a

        b.dma("sp", cs[:, :], cst_d[:, :], [], ["cs"], "cs")
        b.cp("dve", identb, ident, ["cs"], ["cb16"])
        b.cp("dve", onesb, onesf, ["cs"], ["cb16"])
        b.cp("dve", trib, tri, ["cs"], ["cb16"])
        b.cp("dve", iotab, iota, ["cs"], ["cb16"])

        Winb = carve(12320, BF16, [8, 3080]); Woutb = carve(4096, BF16, [8, 1024])
        xTs = carve(4096, F32, [8, 512]); raw = carve(4120, F32, [8, 515])
        sqb = carve(512, BF16, [2, 512]); lnv = carve(512); rstdb = carve(512)
        hT = carve(2048, BF16, [8, 512]); accs = carve(1024, F32, [2, 512]); qkT = carve(2048, BF16, [8, 512])
        WsTb = carve(256, BF16, [4, 128])
        sm = carve(128)
        sg = carve(192)
        vext = carve(264, BF16, [4, 132]); sig = carve(256, BF16); u_sb = carve(256, BF16)
        vgs = carve(512); vn32 = carve(512); vnb = carve(256, BF16); ktok = carve(256, BF16)
        PT = carve(256, BF16, [4, 128]); ybf = carve(512, BF16); yT = carve(512, BF16, [8, 128])
        xmt = carve(1024); x1 = carve(1024); xs = carve(512, BF16)
        Gst = carve(528, F32, [4, 132]); Cb = carve(264, BF16, [4, 132])
        offA = off[0]

        gsb = sg[:, 0:32]; e1 = sg[:, 32:48]; nlf = sg[:, 48:64]; t16 = sg[:, 64:80]; EK = sg[:, 80:96]; EB = sg[:, 96:112]
        A_t = [sg[:, 112:128], sg[:, 128:144]]; nhl16 = sg[:, 144:160].bitcast(BF16); nhi = nhl16[:, 0:16]; nlo = nhl16[:, 16:32]
        gsb3 = gsb.rearrange("p (j g) -> p j g", j=4)
        den = sm[:, 36:40]; dab = sm[:, 40:44]
        rr = sm[:, 44:48]; ssq = sm[:, 48:52]; mm4 = sm[:, 52:56]; scl = sm[:, 56:60]; st8 = sm[:, 60:68]
        mean = sm[:, 68:72]; msq = sm[:, 72:76]; var = sm[:, 76:80]; rs4 = sm[:, 80:84]
        ss2 = sm[:, 84:85]; l2 = sm[:, 85:86]; r2 = sm[:, 86:87]
        nhl = sm[:, 88:92].bitcast(BF16)
        psP = [psb[0], psb[1]]; psS = psb[3]; psO = [psb[4], psb[5]]; psU = [psb[6], psb[7]]
        psM = psb[2]
        psT = psM[:, 256:512].bitcast(BF16)
        Prog.BANK.clear()
        Prog.BANK.update({"psP0": "B0", "psP1": "B1", "psMg": "B2", "psMb": "B2", "psMl": "B2", "psT": "B2", "psS": "B3",
                          "psO0": "B4", "psO1": "B5", "psU0": "B6", "psU1": "B7",
                          "pq0": "B0", "pq1": "B1", "psc0": "B2", "psc1": "B3", "psc2": "B4", "psc3": "B5",
                          "pt6a": "B6", "pt6b": "B6", "pt6c": "B6",
                          "psOut0": "B0", "psOut1": "B1", "psOut2": "B2", "psOut3": "B3", "psA0": "B4", "psA1": "B5",
                          "psG0": "B6", "psG1": "B7"})
        pcnt = [0]
        def nextP():
            pcnt[0] += 1
            return psP[pcnt[0] % 2], "psP%d" % (pcnt[0] % 2)

        stg = [xTs.rearrange("p a b -> p (a b)"), raw.rearrange("p a b -> p (a b)")]
        for c in range(8):
            s = stg[c % 2][:, 0:3080]; sk = "stg%d" % (c % 2)
            b.dma("sp", s, win_d[:, c, :], [], [sk], sk)
            eng = "dve"
            b.cp(eng, Winb[:, c, 0:1540], s[:, 0:1540], [sk], ["Winb"])
            b.act(Winb[:, c, 1540:3080], s[:, 1540:3080], AF.Copy, [sk], ["Winb"])
        for hf in range(2):
            s = stg[hf][:, 0:4096]; sk = "stg%d" % hf
            b.dma("sp", s, wout_d[:, 4 * hf:4 * hf + 4, :], [], [sk], sk)
            for c4 in range(4):
                c = 4 * hf + c4
                if hf == 0:
                    b.ts("dve", Woutb[:, c, :], s[:, c4 * 1024:(c4 + 1) * 1024], col(C_NG + c), None, ALU.mult, None, [sk, "cs"], ["Woutb"])
                else:
                    b.cp("dve", Woutb[:, c, :], s[:, c4 * 1024:(c4 + 1) * 1024], [sk], ["Woutb"])
        b.cp("dve", WsTb.rearrange("p a b -> p (a b)"), cs[:, C_WS:C_WS + 512], ["cs"], ["WsTb"])
        for g in range(4):
            b.ms("dve", WsTb[64:128, g, 0:64], 0.0, ["WsTb"])
        b.ms("dve", raw.rearrange("p a b -> p (a b)"), 0.0, ["stg1"] + ["raw%d" % c for c in range(8)])
        b.ms("dve", Gst.rearrange("p a b -> p (a b)"), 0.0, ["G"])
        b.ms("dve", Cb.rearrange("p a b -> p (a b)"), 0.0, ["Cb"])
        b.ms("dve", A_t[1], 1.0, ["A1"])
        b.ms("dve", vext.rearrange("p a b -> p (a b)"), 0.0, ["vext"])

        for blk in range(int(os.environ.get("TRUNC", "8"))):
            main = blk >= 4
            tok0 = blk * 512
            for c in range(8):
                b.dma("sp", xTs[:, c, :], xT_d[:, c, tok0:tok0 + 512], [], ["xTs0", "stg0"] if c == 0 else ["xTs%d" % c], "xTs%d" % c)
            for c in range(8):
                b.act(sqb[:, c % 2, :], xTs[:, c, :], AF.Square, ["xTs%d" % c], ["sqb%d" % (c % 2)])
                b.mm(psS, onesb, sqb[:, c % 2, :], c == 0, c == 7, ["cb16", "sqb%d" % (c % 2)], ["psS"])
            b.act(lnv, psS, AF.Ln, ["psS", "cs"], ["lnv"], scale=1.0 / 1024, bias=eps_c)
            b.act(rstdb, lnv, AF.Exp, ["lnv"], ["rstdb"], scale=-0.5)
            for c in range(8):
                b.stt("dve", hT[:, c, :], xTs[:, c, :], col(C_G1 + c), rstdb, ALU.mult, ALU.mult, ["xTs%d" % c, "cs", "rstdb"], ["hT"])
            par = blk % 2
            for j in range(4):
                for c in range(8):
                    b.mm(psM[:, j * 8:(j + 1) * 8], hT[:, c, j * 128:(j + 1) * 128], Winb[:, c, 2048:2056], c == 0, c == 7, ["hT", "Winb"], ["psMg"])
            b.tt("dve", gsb3, psM[:, 0:32].rearrange("p (j g) -> p j g", j=4), col(C_GB, 8).unsqueeze(1).to_broadcast([128, 4, 8]), ALU.add,
                 ["psMg", "cs"], ["gsb"])
            b.act(e1.rearrange("p (j h) -> p j h", j=4), gsb3[:, :, 4:8], AF.Exp, ["gsb"], ["e1"], scale=-1.0)
            b.act(nlf, e1, AF.Ln, ["e1", "cs"], ["nlf"], scale=1.0, bias=one_c)
            b.cp("dve", nhi, nlf, ["nlf"], ["nhi"])
            b.tt("dve", nlo, nlf, nhi, ALU.subtract, ["nlf", "nhi"], ["nlo"])
            for j in range(4):
                js = slice(j * 4, (j + 1) * 4)
                b.mm(psM[:, 64 + j * 4:68 + j * 4], trib, nhi[:, js], True, False, ["cb16", "nhi"], ["psMb"])
                b.mm(psM[:, 64 + j * 4:68 + j * 4], trib, nlo[:, js], False, True, ["cb16", "nlo"], ["psMb"])
                b.mm(psM[:, 96 + j * 4:100 + j * 4], onesb, nhi[:, js], True, False, ["cb16", "nhi"], ["psMl"])
                b.mm(psM[:, 96 + j * 4:100 + j * 4], onesb, nlo[:, js], False, True, ["cb16", "nlo"], ["psMl"])
            b.tt("dve", t16.rearrange("p (j h) -> p j h", j=4), gsb3[:, :, 0:4], psM[:, 64:80].rearrange("p (j h) -> p j h", j=4), ALU.add,
                 ["gsb", "psMb"], ["t4"])
            b.act(EK, t16, AF.Exp, ["t4", "cs"], ["ek"], scale=1.0, bias=lns_c)
            b.act(EB, psM[:, 64:80], AF.Exp, ["psMb"], ["eb"], scale=-1.0)
            b.act(A_t[par], psM[:, 96:112], AF.Exp, ["psMl"], ["A%d" % par], scale=-1.0)
            if blk == 3:
                b.ts("dve", A_t[1][:, 12:16], A_t[1][:, 12:16], flag_c, None, ALU.mult, None, ["A1", "cs"], ["A1"])
            cc_list = list(range(8)) if blk >= 3 else list(range(4, 8))
            for cc in cc_list:
                ps, pk = nextP()
                for c in range(8):
                    b.mm(ps, Winb[:, c, cc * 128:(cc + 1) * 128], hT[:, c, :], c == 0, c == 7, ["Winb", "hT"], [pk])
                rk = "raw%d" % cc
                if blk == 4:
                    b.ts("dve", raw[:, cc, 0:3], raw[:, cc, 512:515], flag_c, None, ALU.mult, None, [rk, "cs"], [rk])
                elif blk > 0:
                    b.cp("dve", raw[:, cc, 0:3], raw[:, cc, 512:515], [rk], [rk])
                b.act(raw[:, cc, 3:515], ps, AF.Copy, [pk], [rk])
                if cc >= 4 or main:
                    ac = accs[:, cc % 2, :]; ak = "acc%d" % (cc % 2)
                    b.act(ac, raw[:, cc, 3:515], AF.Identity, [rk, "cs"], [ak], scale=col(C_CW + 3 * 8 + cc), bias=col(C_CB + cc))
                    for j in (2, 1, 0):
                        b.stt("dve", ac, raw[:, cc, j:j + 512], col(C_CW + j * 8 + cc), ac, ALU.mult, ALU.add, [rk, "cs", ak], [ak])
                    b.act(qkT[:, cc, :], ac, AF.Silu, [ak], ["qkT%d" % cc])
            for j in range(4):
                tsl = slice(j * 128, (j + 1) * 128)
                cidx = blk * 4 + j
                js = slice(j * 4, (j + 1) * 4)
                ek = EK[:, js]; eb = EB[:, js]
                acur = A_t[par][:, js]; ack = "A%d" % par
                if j > 0:
                    aprev = A_t[par][:, (j - 1) * 4:j * 4]; apk = ack
                else:
                    aprev = A_t[1 - par][:, 12:16]; apk = "A%d" % (1 - par)
                ps, pk = nextP()
                for c in range(8):
                    b.mm(ps, hT[:, c, tsl], Winb[:, c, 1024:1536], c == 0, c == 7, ["hT", "Winb"], [pk])
                for h in range(4):
                    b.act(vext[:, h, 0:128], ps[:, h * 128:(h + 1) * 128], AF.Copy, [pk, "ek"], ["vext"], scale=ek[:, h:h + 1])
                b.cp("dve", vext[:, :, 128], ek, ["ek"], ["vext"])
                for h in range(4):
                    b.tr(psT[:, h * 128:(h + 1) * 128], qkT[:, 4 + h, tsl], identb, ["qkT%d" % (4 + h), "cb16"], ["psT"])
                b.cp("dve", ktok, psT, ["psT"], ["ktok"])
                if main:
                    for h in range(4):
                        b.mm(psS[:, h * 128:(h + 1) * 128], qkT[:, 4 + h, tsl], qkT[:, h, tsl], True, True,
                             ["qkT%d" % (4 + h), "qkT%d" % h], ["psS"])
                    b.tt("dve", PT, psS.rearrange("p (a b) -> p a b", a=4), tri.unsqueeze(1).to_broadcast([128, 4, 128]),
                         ALU.mult, ["psS", "cs"], ["PT"])
                    for h in range(4):
                        o = psO[h // 2][:, (h % 2) * 256:(h % 2) * 256 + 129]; ok = "psO%d" % (h // 2)
                        b.mm(o, PT[:, h, :], vext[:, h, 0:129], True, False, ["PT", "vext"], [ok])
                        b.mm(o, qkT[:, h, tsl], Cb[:, h, 0:129], False, True, ["qkT%d" % h, "Cb"], [ok])
                for h in range(4):
                    u = psU[h // 2][:, (h % 2) * 256:(h % 2) * 256 + 129]; uk = "psU%d" % (h // 2)
                    b.mm(u, ktok[:, h * 128:(h + 1) * 128], vext[:, h, 0:129], True, True, ["ktok", "vext"], [uk])
                for h in range(4):
                    u = psU[h // 2][:, (h % 2) * 256:(h % 2) * 256 + 129]; uk = "psU%d" % (h // 2)
                    b.stt("dve", Gst[:, h, 0:129], Gst[:, h, 0:129], aprev[:, h:h + 1], u, ALU.mult, ALU.add, ["G", apk, uk], ["G"])
                    if cidx >= 15:
                        b.ts("dve", Cb[:, h, 0:129], Gst[:, h, 0:129], acur[:, h:h + 1], None, ALU.mult, None, ["G", ack], ["Cb"])
                if not main:
                    continue
                for p2 in range(2):
                    dv = psO[p2].rearrange("p (h w) -> p h w", h=2)[:, :, 128]
                    b.tt("dve", den[:, 2 * p2:2 * p2 + 2], dv, eb[:, 2 * p2:2 * p2 + 2], ALU.mult, ["psO%d" % p2, "eb"], ["den"])
                b.act(dab, den, AF.Abs, ["den"], ["dab"])
                b.ts("dve", dab, dab, 1.0, None, ALU.max, None, ["dab"], ["dab"])
                b.P.op("dve", lambda e: e.reciprocal(out=dab, in_=dab), reads=["dab"], writes=["dab"])
                b.tt("dve", rr, eb, dab, ALU.mult, ["eb", "dab"], ["rr"])
                b.ms("dve", ssq, 0.0, ["ssq"])
                for h in range(4):
                    o = psO[h // 2][:, (h % 2) * 256:(h % 2) * 256 + 128]
                    b.act(vn32[:, 0:128], o, AF.Square, ["psO%d" % (h // 2), "ssq"], ["vn32", "ssq"], accum=ssq[:, h:h + 1])
                b.tt("dve", mm4, ssq, rr, ALU.mult, ["ssq", "rr"], ["mm4"])
                b.tt("dve", mm4, mm4, rr, ALU.mult, ["mm4", "rr"], ["mm4"])
                b.act(mm4, mm4, AF.Ln, ["mm4", "cs"], ["mm4"], scale=1.0 / 128, bias=eps_c)
                b.act(mm4, mm4, AF.Exp, ["mm4"], ["mm4"], scale=-0.5)
                b.tt("dve", scl, rr, mm4, ALU.mult, ["rr", "mm4"], ["scl"])
                ps, pk = nextP()
                for c in range(8):
                    b.mm(ps, hT[:, c, tsl], Winb[:, c, 1536:2048], c == 0, c == 7, ["hT", "Winb"], [pk])
                b.act(sig, ps, AF.Sigmoid, [pk], ["sig"])
                for h in range(4):
                    o = psO[h // 2][:, (h % 2) * 256:(h % 2) * 256 + 128]
                    b.stt("dve", ybf[:, h * 128:(h + 1) * 128], o, scl[:, h:h + 1], sig[:, h * 128:(h + 1) * 128], ALU.mult, ALU.mult,
                          ["psO%d" % (h // 2), "scl", "sig"], ["ybf"])
                ps, pk = nextP()
                for c in range(8):
                    b.mm(ps, hT[:, c, tsl], Winb[:, c, 2056:2568], c == 0, c == 7, ["hT", "Winb"], [pk])
                b.act(u_sb, ps, AF.Gelu_apprx_tanh, [pk], ["u_sb"])
                ps, pk = nextP()
                for c in range(8):
                    b.mm(ps, hT[:, c, tsl], Winb[:, c, 2568:3080], c == 0, c == 7, ["hT", "Winb"], [pk])
                b.ms("dve", st8, 0.0, ["st8"])
                for g in range(4):
                    b.act(vgs[:, g * 128:(g + 1) * 128], ps[:, g * 128:(g + 1) * 128], AF.Gelu_apprx_tanh, [pk, "st8"], ["vgs", "st8"],
                          accum=st8[:, g:g + 1])
                for g in range(4):
                    b.act(vn32[:, g * 128:(g + 1) * 128], vgs[:, g * 128:(g + 1) * 128], AF.Square, ["vgs", "st8"], ["vn32", "st8"],
                          accum=st8[:, 4 + g:5 + g])
                b.ts("dve", mean, st8[:, 0:4], 1.0 / 128, None, ALU.mult, None, ["st8"], ["mean"])
                b.tt("dve", msq, mean, mean, ALU.mult, ["mean"], ["msq"])
                b.stt("dve", var, st8[:, 4:8], 1.0 / 128, msq, ALU.mult, ALU.subtract, ["st8", "msq"], ["var"])
                b.act(var, var, AF.Ln, ["var", "cs"], ["var"], scale=1.0, bias=eps_c)
                b.act(rs4, var, AF.Exp, ["var"], ["rs4"], scale=-0.5)
                for g in range(4):
                    b.ts("dve", vn32[:, g * 128:(g + 1) * 128], vgs[:, g * 128:(g + 1) * 128], mean[:, g:g + 1], rs4[:, g:g + 1],
                         ALU.subtract, ALU.mult, ["vgs", "mean", "rs4"], ["vn32"])
                b.tt("dve", vn32, vn32, cs[:, C_LNG:C_LNG + 512], ALU.mult, ["vn32", "cs"], ["vn32"])
                b.tt("dve", vnb, vn32, cs[:, C_LNB:C_LNB + 512], ALU.add, ["vn32", "cs"], ["vnb"])
                for g in range(4):
                    b.mm(psS[:, g * 128:(g + 1) * 128], WsTb[:, g, :], vnb[:, g * 128:(g + 1) * 128], True, True, ["WsTb", "vnb"], ["psS"])
                for g in range(4):
                    b.stt("dve", ybf[:, 512 + g * 128:512 + (g + 1) * 128], psS[:, g * 128:(g + 1) * 128], col(C_BS + g),
                          u_sb[:, g * 128:(g + 1) * 128], ALU.add, ALU.mult, ["psS", "cs", "u_sb"], ["ybf"])
                for hf in range(2):
                    for q in range(4):
                        b.tr(psT[:, q * 128:(q + 1) * 128], ybf[:, (hf * 4 + q) * 128:(hf * 4 + q + 1) * 128], identb, ["ybf", "cb16"], ["psT"])
                    b.cp("dve", yT[:, hf * 4:hf * 4 + 4, :].rearrange("p a b -> p (a b)"), psT, ["psT"], ["yT"])
                for hf in range(2):
                    for c in range(8):
                        b.mm(psO[hf], yT[:, c, :], Woutb[:, c, hf * 512:(hf + 1) * 512], c == 0, c == 7, ["yT", "Woutb"], ["psO%d" % hf])
                mt0 = (blk - 4) * 512 + j * 128
                b.dma("sp", xmt, xm_d[mt0:mt0 + 128, :], [], ["xmt"], "xmt")
                for hf in range(2):
                    b.tt("dve", x1[:, hf * 512:(hf + 1) * 512], psO[hf], xmt[:, hf * 512:(hf + 1) * 512], ALU.add, ["psO%d" % hf, "xmt"], ["x1"])
                b.dma("sp", x1_d[mt0:mt0 + 128, :], x1, ["x1"], ["x1d%d" % (mt0 // 128)], "x1st")
                if stage == "A":
                    continue
                b.ms("dve", ss2, 0.0, ["ss2"])
                b.act(xs, x1, AF.Square, ["x1", "ss2"], ["xs", "ss2"], accum=ss2)
                b.act(l2, ss2, AF.Ln, ["ss2", "cs"], ["l2"], scale=1.0 / 1024, bias=eps_c)
                b.act(r2, l2, AF.Exp, ["l2"], ["r2"], scale=-0.5)
                b.act(xs, x1, AF.Copy, ["x1", "r2"], ["xs"], scale=r2)
                for hf in range(2):
                    for q in range(4):
                        b.tr(psT[:, q * 128:(q + 1) * 128], xs[:, (hf * 4 + q) * 128:(hf * 4 + q + 1) * 128], identb, ["xs", "cb16"], ["psT"])
                    for q in range(4):
                        b.ts("dve", xn2T[:, hf * 4 + q, mt0:mt0 + 128], psT[:, q * 128:(q + 1) * 128], col(C_G2 + hf * 4 + q), None,
                             ALU.mult, None, ["psT", "cs"], ["xn2T"])
        fkeys = ["x1d%d" % i for i in range(16)]
        if stage not in ("A", "A2"):
            fkeys = build_peer(nc, b, carve, off, cs, xn2T, identb, ident, iotab, psb, wq_d, keys_d, ut_d, v_d, x1_d, y_d, stage, ub_d, vb_d)
        P.emit(final_keys=fkeys)
    return nc


import os


def build_peer(nc, b, carve, off, cs, xn2T, identb, ident, iota, psb, wq_d, keys_d, ut_d, v_d, x1_d, y_d, stage='full', ub_d=None, vb_d=None):
    P = b.P
    P.fence()
    off[0] = 0
    col = lambda c, n=1: cs[:, c:c + n]
    eps_c = col(C_EPS)
    I1T = carve(2048); I2T = carve(2048); GTs = carve(2048)
    offBC = off[0]
    Wqb = carve(8192, BF16, [8, 2048]); keysTb = carve(1024, BF16, [16, 128]); stg = carve(2048)
    qTb = carve(1024, BF16, [16, 128]); sc2 = carve(4096, F32, [2, 16, 128]); wk = carve(2048, F32, [16, 128])
    m16 = carve(256, F32, [16, 16]); i16 = carve(256, U32, [16, 16]); i16f = carve(256, F32, [16, 16])
    NC = 80; RECTS = [(0, 2, 16, 0), (2, 2, 8, 32), (4, 4, 4, 48), (8, 8, 2, 64)]
    cand = carve(8 * NC, F32, [8, NC]); cidx = carve(8 * NC, F32, [8, NC]); junk = carve(4 * NC, F32, [4, NC])
    ts16 = carve(128, F32, [8, 16]); eidx = carve(128); ex = carve(128, F32, [8, 16]); gate = carve(128, F32, [8, 16])
    ei = carve(128, I32); i1i = carve(128, I32); i2i = carve(128, I32); i1f = carve(128); i2f = carve(128)
    tb3 = carve(192, BF16)
    smb = carve(32); negm = smb[:, 0:8]; Z = smb[:, 8:16]; rz = smb[:, 16:24]
    cin = carve(6144, F32, [3, 2, 1024]); cout = carve(3072, BF16, [3, 2, 1024])
    jobs = {"in": 0, "cv": 0}

    def conv_in():
        j = jobs["in"]
        if j >= 128: return
        jobs["in"] += 1; s3 = j % 3
        b.dma("sp", cin[:, s3, 0, :], ut_d[j].rearrange("p c e -> p (c e)"), [], ["cinu%d" % s3], "cinu%d" % s3)
        b.dma("sp", cin[:, s3, 1, :], v_d[j * 128:(j + 1) * 128, :], [], ["cinv%d" % s3], "cinv%d" % s3)

    def conv_cv():
        j = jobs["cv"]
        if j >= 128: return
        jobs["cv"] += 1; s3 = j % 3
        b.act(cout[:, s3, 0, :], cin[:, s3, 0, :], AF.Copy, ["cinu%d" % s3], ["coutu%d" % s3])
        b.act(cout[:, s3, 1, :], cin[:, s3, 1, :], AF.Copy, ["cinv%d" % s3], ["coutv%d" % s3])
        b.dma("sp", ub_d[j], cout[:, s3, 0, :], ["coutu%d" % s3], ["ubd"], "cou%d" % s3)
        b.dma("sp", vb_d[j * 128:(j + 1) * 128, :], cout[:, s3, 1, :], ["coutv%d" % s3], ["vbd"], "cov%d" % s3)

    conv_in(); conv_in()
    for c in range(8):
        b.dma("sp", stg, wq_d[:, c, :], [], ["stgB"], "stgB")
        b.cp("dve", Wqb[:, c, 0:1024], stg[:, 0:1024], ["stgB"], ["Wqb"])
        b.act(Wqb[:, c, 1024:2048], stg[:, 1024:2048], AF.Copy, ["stgB"], ["Wqb"])
    b.dma("sp", stg, keys_d[:, :, :].rearrange("p a b -> p (a b)"), [], ["stgB"], "stgB")
    b.cp("dve", keysTb.rearrange("p a b -> p (a b)"), stg, ["stgB"], ["keysTb"])
    PB = int(os.environ.get("PB_STOP", "99"))
    if PB <= 0: return ["x1d%d" % q for q in range(16)]
    def part1(i):
        t0 = i * 128; par = i % 2; sc = sc2[:, par]
        for hp in range(16):
            ps = psb[hp % 2]; pk = "pq%d" % (hp % 2)
            for c in range(8):
                b.mm(ps[:, 0:128], Wqb[:, c, hp * 128:(hp + 1) * 128], xn2T[:, c, t0:t0 + 128], c == 0, c == 7, ["Wqb", "xn2T"], [pk])
            b.act(qTb[:, hp, :], ps[:, 0:128], AF.Copy, [pk], ["qTb%d" % hp])
            if hp % 2 == 0:
                conv_in(); conv_cv()
        for hp in range(16):
            b.mm(psb[2 + hp // 4][:, (hp % 4) * 128:(hp % 4 + 1) * 128], qTb[:, hp, :], keysTb[:, hp, :], True, True,
                 ["qTb%d" % hp, "keysTb"], ["psc%d" % (hp // 4)])
        for q4 in range(4):
            b.act(sc[:, q4 * 4:q4 * 4 + 4, :].rearrange("p a b -> p (a b)"), psb[2 + q4], AF.Copy, ["psc%d" % q4], ["sc%d_%d" % (q4, par)])

    def part2(i):
        t0 = i * 128; par = i % 2; sc = sc2[:, par]
        for hp in range(16):
            b.P.op("dve", (lambda hp: lambda e: e.max(out=m16[:, hp, 0:8], in_=sc[:, hp, :]))(hp), reads=["sc%d_%d" % (hp // 4, par)], writes=["m16_%d" % hp])
        for hp in range(16):
            b.P.op("dve", (lambda hp: lambda e: e.max_index(out=i16[:, hp, 0:8], in_max=m16[:, hp, 0:8], in_values=sc[:, hp, :]))(hp),
                   reads=["sc%d_%d" % (hp // 4, par), "m16_%d" % hp], writes=["i16_%d" % hp])
        for hp in range(16):
            b.P.op("dve", (lambda hp: lambda e: e.match_replace(out=wk[:, hp, :], in_to_replace=m16[:, hp, 0:8], in_values=sc[:, hp, :], imm_value=-1e30))(hp),
                   reads=["sc%d_%d" % (hp // 4, par), "m16_%d" % hp], writes=["wk%d" % hp])
        for hp in range(16):
            b.P.op("dve", (lambda hp: lambda e: e.max(out=m16[:, hp, 8:16], in_=wk[:, hp, :]))(hp), reads=["wk%d" % hp], writes=["m16_%d" % hp])
        for hp in range(16):
            b.P.op("dve", (lambda hp: lambda e: e.max_index(out=i16[:, hp, 8:16], in_max=m16[:, hp, 8:16], in_values=wk[:, hp, :]))(hp),
                   reads=["wk%d" % hp, "m16_%d" % hp], writes=["i16_%d" % hp])
        allm = ["m16_%d" % hp for hp in range(16)]; alli = ["i16_%d" % hp for hp in range(16)]
        b.cp("dve", i16f.rearrange("p a b -> p (a b)"), i16.rearrange("p a b -> p (a b)"), alli, ["i16f"])
        m16h = m16.rearrange("p (h two) a -> p h two a", two=2)
        i16h = i16f.rearrange("p (h two) a -> p h two a", two=2)
        allc = ["cand%d" % h for h in range(8)]; allx = ["cidx%d" % h for h in range(8)]
        b.ts("dve", i16h[:, :, 0, :], i16h[:, :, 0, :], 128.0, None, ALU.mult, None, ["i16f"], ["i16f"])
        for (a0, na, nb, o0) in RECTS:
            shp = [128, 8, na, nb]
            b.tt("dve", cand[:, :, o0:o0 + na * nb].rearrange("p h (a c) -> p h a c", a=na),
                 m16h[:, :, 0, a0:a0 + na].unsqueeze(3).to_broadcast(shp), m16h[:, :, 1, 0:nb].unsqueeze(2).to_broadcast(shp),
                 ALU.add, allm, allc)
            b.tt("dve", cidx[:, :, o0:o0 + na * nb].rearrange("p h (a c) -> p h a c", a=na),
                 i16h[:, :, 0, a0:a0 + na].unsqueeze(3).to_broadcast(shp), i16h[:, :, 1, 0:nb].unsqueeze(2).to_broadcast(shp),
                 ALU.add, ["i16f"], allx)
        wk2 = wk.rearrange("p a b -> p (a b)").rearrange("p (a b) -> p a b", a=8)[:, :, 0:NC]
        for h in range(8):
            b.P.op("dve", (lambda h: lambda e: e.max(out=ts16[:, h, 0:8], in_=cand[:, h, :]))(h), reads=["cand%d" % h], writes=["ts16_%d" % h])
        for h in range(8):
            b.P.op("dve", (lambda h: lambda e: e.match_replace(out=wk2[:, h, :], in_to_replace=ts16[:, h, 0:8], in_values=cand[:, h, :], imm_value=-1e30))(h),
                   reads=["cand%d" % h, "ts16_%d" % h] + ["wk%d" % (2 * h), "wk%d" % (2 * h + 1)], writes=["wk%d" % (2 * h), "wk%d" % (2 * h + 1)])
        for h in range(8):
            b.P.op("dve", (lambda h: lambda e: e.max(out=ts16[:, h, 8:16], in_=wk2[:, h, :]))(h), reads=["wk%d" % (2 * h), "wk%d" % (2 * h + 1)], writes=["ts16_%d" % h])
        b.ms("dve", eidx, 0.0, ["eidx"])
        if os.environ.get("TRIV"):
            for q in range(int(os.environ["TRIV"])):
                b.ms("dve", junk[:, q % 4, :], 0.0, ["junk%d" % (q % 4)])
            return ["x1d%d" % q for q in range(16)]
        for j in range(16):
            for h in range(8):
                jk = "junk%d" % ((j * 8 + h) % 4)
                b.stt("dve", junk[:, (j * 8 + h) % 4, :], cand[:, h, :], ts16[:, h, j:j + 1], cidx[:, h, :], ALU.is_equal, ALU.mult,
                      ["cand%d" % h, "ts16_%d" % h, "cidx%d" % h, "eidx"], [jk, "eidx%d" % (h * 16 + j)], accum=(None if os.environ.get("NOACC") else eidx[:, h * 16 + j:h * 16 + j + 1]))
        alle = ["eidx"] + ["eidx%d" % q for q in range(128)]
        allts = ["ts16_%d" % h for h in range(8)]
        b.ts("dve", negm, ts16[:, :, 0], -1.0, None, ALU.mult, None, allts, ["negm"])
        b.ms("dve", Z, 0.0, ["Z"])
        for h in range(8):
            b.act(ex[:, h, :], ts16[:, h, :], AF.Exp, allts + ["negm", "Z"], ["ex", "Z"], scale=1.0, bias=negm[:, h:h + 1], accum=Z[:, h:h + 1])
        b.P.op("dve", lambda e: e.reciprocal(out=rz, in_=Z), reads=["Z"], writes=["rz"])
        b.tt("dve", gate, ex, rz.unsqueeze(2).to_broadcast([128, 8, 16]), ALU.mult, ["ex", "rz"], ["gate"])
        b.cp("dve", ei, eidx, alle, ["ei"])
        b.ts("dve", i1i, ei, 7, None, ALU.arith_shift_right, None, ["ei"], ["i1i"])
        b.ts("dve", i2i, ei, 127, None, ALU.bitwise_and, None, ["ei"], ["i2i"])
        b.cp("dve", tb3[:, 0:128], i1i, ["i1i"], ["tb3a"])
        b.cp("dve", tb3[:, 128:256], i2i, ["i2i"], ["tb3b"])
        b.cp("dve", tb3[:, 256:384], gate.rearrange("p a b -> p (a b)"), ["gate"], ["tb3c"])
        pT6 = psb[6].bitcast(BF16)
        b.tr(pT6[:, 0:128], tb3[:, 0:128], identb, ["tb3a"], ["pt6a"])
        b.tr(pT6[:, 128:256], tb3[:, 128:256], identb, ["tb3b"], ["pt6b"])
        b.tr(pT6[:, 256:384], tb3[:, 256:384], identb, ["tb3c"], ["pt6c"])
        b.act(I1T[:, t0:t0 + 128], pT6[:, 0:128], AF.Copy, ["pt6a"], ["I1T"])
        b.act(I2T[:, t0:t0 + 128], pT6[:, 128:256], AF.Copy, ["pt6b"], ["I2T"])
        b.act(GTs[:, t0:t0 + 128], pT6[:, 256:384], AF.Copy, ["pt6c"], ["GTs"])
    part1(0)
    for i in range(16):
        if i + 1 < 16: part1(i + 1)
        part2(i)
    while jobs["cv"] < 128:
        conv_in(); conv_cv()
    if stage == "AB":
        return ["x1d%d" % i for i in range(16)]
    P.fence()
    off[0] = offBC
    GT = carve(16384, BF16, [128, 256]); ohA = carve(256, BF16, [8, 64]); ohB = carve(512, BF16, [8, 128])
    NSL = 4
    Ub = carve(NSL * 512, BF16, [NSL, 8, 128]); Vb = carve(NSL * 512, BF16, [NSL, 1024])
    Ab = carve(256, BF16, [2, 256]); AG = carve(256, BF16, [2, 256])
    x1t = carve(1024); x2 = carve(1024); ot = carve(1024); smc = carve(16)
    ssf = smc[:, 0:1]; lf_ = smc[:, 1:2]; rf = smc[:, 2:3]
    psOut = psb[0:4]; psA = psb[4:6]; psG = psb[6:8]
    fkeys = []
    GL = [(0, 0, g) for g in range(64)]
    for T in range(8):
        GL += [(T, 1, g) for g in range(64)]
        if T + 1 < 8: GL += [(T + 1, 0, g) for g in range(64)]
    gn = [0]

    def gstep():
        n = gn[0]; gn[0] += 1
        if n + 1 < len(GL):
            T, half, grp = GL[n + 1]; p = (n + 1) % 2; h0 = 64 * half
            for s4 in range(4):
                t = T * 256 + grp * 4 + s4; sl = p * 4 + s4
                b.ts("dve", ohA[:, sl, :], iota[:, h0:h0 + 64], I1T[:, t:t + 1], GTs[:, t:t + 1], ALU.is_equal, ALU.mult,
                     ["cb16", "I1T", "GTs"], ["ohA%d" % sl])
                b.ts("dve", ohB[:, sl, :], iota, I2T[:, t:t + 1], None, ALU.is_equal, None, ["cb16", "I2T"], ["ohB%d" % sl])
        if n < len(GL):
            p = n % 2
            for s4 in range(4):
                sl = p * 4 + s4
                b.mm(psG[p][:, s4 * 64:(s4 + 1) * 64], ohB[:, sl, :], ohA[:, sl, :], True, True, ["ohA%d" % sl, "ohB%d" % sl], ["psG%d" % p])
        if 1 <= n <= len(GL):
            T, half, grp = GL[n - 1]; p = (n - 1) % 2; h0 = 64 * half
            b.act(GT[:, h0:h0 + 64, grp * 4:grp * 4 + 4], psG[p][:, 0:256].rearrange("p (t i) -> p i t", t=4), AF.Copy,
                  ["psG%d" % p], ["GT%d" % half])

    T, half, grp = GL[0]
    for s4 in range(4):
        t = grp * 4 + s4
        b.ts("dve", ohA[:, s4, :], iota[:, 0:64], I1T[:, t:t + 1], GTs[:, t:t + 1], ALU.is_equal, ALU.mult, ["cb16", "I1T", "GTs"], ["ohA%d" % s4])
        b.ts("dve", ohB[:, s4, :], iota, I2T[:, t:t + 1], None, ALU.is_equal, None, ["cb16", "I2T"], ["ohB%d" % s4])
    for _ in range(64):
        gstep()
    for T in range(8):
        tb = T * 256

        def stage1(blk):
            s4 = blk % NSL; s2 = blk % 2
            b.dma("sp", Ub[:, s4, :, :].rearrange("p a b -> p (a b)"), ub_d[blk], ["ubd"], ["Ub%d" % s4], "Ub%d" % s4)
            b.dma("sp", Vb[:, s4, :], vb_d[blk * 128:(blk + 1) * 128, :], ["vbd"], ["Vb%d" % s4], "Vb%d" % s4)
            for c in range(8):
                b.mm(psA[s2][:, 0:256], Ub[:, s4, c, :], xn2T[:, c, tb:tb + 256], c == 0, c == 7, ["Ub%d" % s4, "xn2T"], ["psA%d" % s2])

        def stage2(blk):
            s2 = blk % 2; s4 = blk % NSL
            b.act(Ab[:, s2, :], psA[s2][:, 0:256], AF.Gelu_apprx_tanh, ["psA%d" % s2], ["Ab%d" % s2])
            b.tt("dve", AG[:, s2, :], Ab[:, s2, :], GT[:, blk, :], ALU.mult, ["Ab%d" % s2, "GT%d" % (blk // 64)], ["AG%d" % s2])
            for sub in range(2):
                for hf in range(2):
                    b.mm(psOut[sub * 2 + hf], AG[:, s2, sub * 128:(sub + 1) * 128], Vb[:, s4, hf * 512:(hf + 1) * 512], blk == 0, blk == 127,
                         ["AG%d" % s2, "Vb%d" % s4], ["psOut%d" % (sub * 2 + hf)])

        for blk in range(129):
            if blk < 128: stage1(blk)
            if blk >= 1: stage2(blk - 1)
            if blk < 128:
                gstep()
        for sub in range(2):
            r0 = tb + sub * 128
            b.dma("sp", x1t, x1_d[r0:r0 + 128, :], ["x1d%d" % (r0 // 128)], ["x1t"], "x1t")
            for hf in range(2):
                b.tt("dve", x2[:, hf * 512:(hf + 1) * 512], psOut[sub * 2 + hf], x1t[:, hf * 512:(hf + 1) * 512], ALU.add,
                     ["psOut%d" % (sub * 2 + hf), "x1t"], ["x2"])
            b.ms("dve", ssf, 0.0, ["ssf"])
            b.act(ot, x2, AF.Square, ["x2", "ssf"], ["ot", "ssf"], accum=ssf)
            b.act(lf_, ssf, AF.Ln, ["ssf", "cs"], ["lf_"], scale=1.0 / 1024, bias=eps_c)
            b.act(rf, lf_, AF.Exp, ["lf_"], ["rf"], scale=-0.5)
            b.stt("dve", ot, x2, rf, cs[:, C_FGB:C_FGB + 1024], ALU.mult, ALU.mult, ["x2", "rf", "cs", "ot"], ["ot"])
            k = "yd%d" % (r0 // 128)
            b.dma("sp", y_d[r0:r0 + 128, :], ot, ["ot"], [k], "yst")
            fkeys.append(k)
    return fkeys


def host_inputs(inp):
    f = lambda a: np.ascontiguousarray(a, dtype=np.float32)
    x = inp["x"]
    consts = np.zeros((128, C_TOT), np.float32)
    cw = inp["conv_w"][0]
    consts[:, C_CW:C_CW + 32] = cw.reshape(4, 8, 128).transpose(2, 0, 1).reshape(128, 32)
    consts[:, C_CB:C_CB + 8] = inp["conv_b"][0].reshape(8, 128).T
    consts[:, C_G1:C_G1 + 8] = inp["norm1_g"][0].reshape(8, 128).T
    consts[:, C_G2:C_G2 + 8] = inp["norm2_g"][0].reshape(8, 128).T
    consts[:, C_NG:C_NG + 4] = inp["mlstm_norm_g"][0].reshape(4, 128).T
    consts[:, C_GB:C_GB + 4] = inp["b_igate"][0][None, :]
    consts[:, C_GB + 4:C_GB + 8] = inp["b_fgate"][0][None, :]
    consts[:, C_BS:C_BS + 4] = inp["gmlp_b_s"][0].T
    consts[:, C_EPS] = EPS; consts[:, C_ONE] = 1.0; consts[:, C_LNS] = np.float32(np.log(128.0 ** -0.5))
    consts[:, C_LNG:C_LNG + 512] = inp["gmlp_ln_g"][0][None, :]
    consts[:, C_LNB:C_LNB + 512] = inp["gmlp_ln_b"][0][None, :]
    consts[:, C_FGB:C_FGB + 1024] = inp["final_g"][None, :]
    consts[:, C_CST:C_CST + 128] = np.eye(128)
    consts[:, C_CST + 128:C_CST + 256] = np.triu(np.ones((128, 128)))
    consts[:, C_CST + 256:C_CST + 384] = 1.0
    consts[:, C_CST + 384:C_CST + 512] = np.arange(128)[None, :]
    consts[:, C_WS:C_WS + 512] = inp["gmlp_w_s"][0].transpose(2, 0, 1).reshape(128, 512)
    shared = {
        "win": f(inp["w_in"][0].reshape(8, 128, 3080).transpose(1, 0, 2)),
        "wout": f(inp["w_out"][0].reshape(8, 128, 1024).transpose(1, 0, 2)),
        "wq": f(inp["peer_w_query"][0].reshape(8, 128, 2048).transpose(1, 0, 2)),
        "keysT": f(inp["peer_sub_keys"][0].reshape(16, 128, 128).transpose(2, 0, 1)),
        "UT": f(inp["peer_u"][0].reshape(128, 128, 8, 128).transpose(0, 3, 2, 1)),
        "V": f(inp["peer_v"][0]),
    }
    maps = []
    for core in range(NCORES):
        bi, s = core // 2, core % 2
        xm = x[bi, s * NT:(s + 1) * NT]
        pre = x[bi, 0:NT]
        toks = np.concatenate([pre, xm], axis=0)
        xT = f(toks.T.reshape(8, 128, 4096).transpose(1, 0, 2))
        c = consts.copy(); c[:, C_FLAG] = float(s)
        m = dict(shared); m.update({"xT": xT, "xm": f(xm), "consts": c})
        maps.append(m)
    return maps


_NC_CACHE = {}


def kernel(**inputs):
    stage = "full"
    if stage not in _NC_CACHE:
        _NC_CACHE[stage] = build(stage)
    nc = _NC_CACHE[stage]
    maps = host_inputs({k: np.asarray(v) for k, v in inputs.items()})
    res = run_bass_kernel_spmd(nc, maps, core_ids=list(range(NCORES)))
    out = np.empty((4, 4096, 1024), np.float32)
    for core in range(NCORES):
        bi, s = core // 2, core % 2
        out[bi, s * NT:(s + 1) * NT] = res.results[core]["y"]
    return out
```

```python
import contextlib
import os
import numpy as np
import concourse.bass as bass
import concourse.mybir as mybir
from concourse.bass_utils import run_bass_kernel_spmd

F32 = mybir.dt.float32; BF16 = mybir.dt.bfloat16; U32 = mybir.dt.uint32; I32 = mybir.dt.int32
AF = mybir.ActivationFunctionType; ALU = mybir.AluOpType

NCORES = 8
NT = 2048
D = 1024
EPS = 1e-6
C_CW = 0; C_CB = 32; C_G1 = 40; C_G2 = 48; C_NG = 56; C_GB = 60; C_BS = 68; C_FLAG = 72
C_EPS = 73; C_ONE = 74; C_LNS = 75; C_SMALL = 80
C_LNG = 80; C_LNB = C_LNG + 512; C_FGB = C_LNB + 512; C_CST = C_FGB + 1024; C_WS = C_CST + 512
C_TOT = C_WS + 512


class Prog:
    ENG = ("pe", "act", "dve", "pool", "sp")

    def __init__(self, nc):
        self.nc = nc; self.ops = []; self.lastw = {}; self.readers = {}; self.dma_cnt = {}

    BANK = {}

    def op(self, eng, fn, reads=(), writes=(), dma=None):
        i = len(self.ops)
        reads = list(reads) + [self.BANK[k] for k in reads if k in self.BANK]
        writes = list(writes) + [self.BANK[k] for k in writes if k in self.BANK]
        deps = set()
        for k in list(reads) + list(writes):
            if k in self.lastw: deps.add(self.lastw[k])
        for k in writes:
            lastr = {}
            for r in self.readers.get(k, ()):
                ro = self.ops[r]
                if ro["dma"] is not None: deps.add(r)
                else: lastr[ro["eng"]] = r
            deps.update(lastr.values())
        o = dict(eng=eng, fn=fn, deps=deps, dma=dma, sig=False, dcount=None, sidx=None)
        if dma is not None:
            self.dma_cnt[dma] = self.dma_cnt.get(dma, 0) + 1
            o["dcount"] = self.dma_cnt[dma]
        self.ops.append(o)
        for k in writes:
            self.lastw[k] = i; self.readers[k] = []
        for k in reads:
            self.readers.setdefault(k, []).append(i)
        return i

    def fence(self):
        last = {}
        for i, o in enumerate(self.ops):
            if o["fn"] is None: continue
            if o["dma"] is not None: last[("d", o["dma"])] = i
            else: last[("e", o["eng"])] = i
        for e in self.ENG:
            i = self.op(e, None)
            self.ops[i]["deps"] = set(last.values())

    def emit(self, final_keys=()):
        nc = self.nc; ops = self.ops
        if not os.environ.get("NOFENCE"):
            self.fence()
        self.op("sp", None, reads=list(final_keys), writes=["__final"])
        pos = {e: 0 for e in self.ENG}
        for o in ops:
            if o["fn"] is not None:
                pos[o["eng"]] += 1
            o["epos"] = pos[o["eng"]]

        def needs(o, od):
            if od["dma"] is not None: return True
            if od["eng"] == o["eng"] and o["fn"] is not None and o["dma"] is None:
                if o["eng"] == "pe": return False
                return o["epos"] - od["epos"] < int(os.environ.get("NEEDS_DIST", "4"))
            return True
        self.needs = needs
        for o in ops:
            for d in o["deps"]:
                od = ops[d]
                if od["dma"] is None and needs(o, od):
                    od["sig"] = True
        cnt = {e: 0 for e in self.ENG}
        for o in ops:
            if o["dma"] is None and o["sig"] and o["fn"] is not None:
                cnt[o["eng"]] += 1; o["sidx"] = cnt[o["eng"]]
        self.stats = dict(nops=len(ops), sig=cnt, dma=dict(self.dma_cnt))
        with contextlib.ExitStack() as st:
            esem = {e: st.enter_context(nc.semaphore("s_" + e)) for e in self.ENG}
            dsem = {k: st.enter_context(nc.semaphore("d_" + str(k))) for k in self.dma_cnt}
            block = st.enter_context(nc.Block())

            def expand(o, acc, seen):
                for d in o["deps"]:
                    if d in seen: continue
                    seen.add(d)
                    od = ops[d]
                    if od["fn"] is None:
                        expand(od, acc, seen)
                    elif od["dma"] is not None:
                        acc.append((("d", od["dma"]), 16 * od["dcount"]))
                    elif self.needs(o, od):
                        acc.append((("e", od["eng"]), od["sidx"]))

            def run(ename, eng):
                waited = {}
                for o in ops:
                    if o["eng"] != ename: continue
                    acc = []
                    expand(o, acc, set())
                    best = {}
                    for k, v in acc:
                        if v is not None and v > best.get(k, 0): best[k] = v
                    for k, v in best.items():
                        if waited.get(k, 0) >= v: continue
                        waited[k] = v
                        eng.wait_ge(esem[k[1]] if k[0] == "e" else dsem[k[1]], v)
                    if o["fn"] is None: continue
                    ins = o["fn"](eng)
                    if o["dma"] is not None:
                        ins.then_inc(dsem[o["dma"]], 16)
                    elif o["sig"]:
                        ins.then_inc(esem[ename], 1)

            @block.tensor
            def _(e): run("pe", e)

            @block.scalar
            def _(e): run("act", e)

            @block.vector
            def _(e): run("dve", e)

            @block.gpsimd
            def _(e): run("pool", e)

            @block.sync
            def _(e): run("sp", e)


class Bld:
    def __init__(self, nc):
        self.P = Prog(nc)

    def mm(self, out, lhsT, rhs, start, stop, r, w):
        self.P.op("pe", lambda e: e.matmul(out, lhsT=lhsT, rhs=rhs, start=start, stop=stop), reads=r, writes=w)

    def tr(self, out, in_, ident, r, w):
        self.P.op("pe", lambda e: e.transpose(out=out, in_=in_, identity=ident), reads=r, writes=w)

    def act(self, out, in_, func, r, w, scale=None, bias=None, accum=None):
        kw = {}
        if scale is not None: kw["scale"] = scale
        if bias is not None: kw["bias"] = bias
        if accum is not None: kw["accum_out"] = accum
        self.P.op("act", lambda e: e.activation(out=out, in_=in_, func=func, **kw), reads=r, writes=w)

    def tt(self, eng, out, in0, in1, op, r, w):
        self.P.op(eng, lambda e: e.tensor_tensor(out=out, in0=in0, in1=in1, op=op), reads=r, writes=w)

    def ts(self, eng, out, in0, s1, s2, op0, op1, r, w):
        if s2 is None:
            self.P.op(eng, lambda e: e.tensor_scalar(out=out, in0=in0, scalar1=s1, scalar2=None, op0=op0), reads=r, writes=w)
        else:
            self.P.op(eng, lambda e: e.tensor_scalar(out=out, in0=in0, scalar1=s1, scalar2=s2, op0=op0, op1=op1), reads=r, writes=w)

    def stt(self, eng, out, in0, scalar, in1, op0, op1, r, w, accum=None):
        kw = {} if accum is None else {"accum_out": accum}
        self.P.op(eng, lambda e: e.scalar_tensor_tensor(out=out, in0=in0, scalar=scalar, in1=in1, op0=op0, op1=op1, **kw), reads=r, writes=w)

    def cp(self, eng, out, in_, r, w):
        self.P.op(eng, lambda e: e.tensor_copy(out=out, in_=in_), reads=r, writes=w)

    def ms(self, eng, out, val, w):
        self.P.op(eng, lambda e: e.memset(out, val), writes=w)

    def dma(self, q, out, in_, r, w, name):
        self.P.op(q, lambda e: e.dma_start(out=out, in_=in_), reads=r, writes=w, dma=name)


def build(stage="full"):
    nc = bass.Bass("TRN2", target_bir_lowering=False)
    dt = lambda name, shape, kind="ExternalInput", dty=F32: nc.dram_tensor(name, shape, dty, kind=kind).ap()
    xT_d = dt("xT", [128, 8, 4096]); xm_d = dt("xm", [NT, D]); win_d = dt("win", [128, 8, 3080])
    wout_d = dt("wout", [128, 8, 1024]); cst_d = dt("consts", [128, C_TOT])
    if stage not in ("A", "A2"):
        wq_d = dt("wq", [128, 8, 2048]); keys_d = dt("keysT", [128, 16, 128])
        ut_d = dt("UT", [128, 128, 8, 128]); v_d = dt("V", [16384, 1024])
    y_d = dt("y", [NT, D], kind="ExternalOutput")
    x1_d = dt("x1s", [NT, D], kind="Internal") if stage not in ("A", "A2") else y_d
    if stage not in ("A", "A2"):
        ub_d = dt("ubs", [128, 128, 1024], kind="Internal", dty=BF16)
        vb_d = dt("vbs", [16384, 1024], kind="Internal", dty=BF16)

    b = Bld(nc); P = b.P
    with contextlib.ExitStack() as st:
        NW = 41400
        cs = st.enter_context(nc.sbuf_tensor("cs", [128, C_TOT], F32))
        xn2T = st.enter_context(nc.sbuf_tensor("xn2T", [128, 8, NT], BF16))
        cb16 = st.enter_context(nc.sbuf_tensor("cb16", [128, 512], BF16))
        ar = st.enter_context(nc.sbuf_tensor("arena", [128, NW], F32))
        psb = [st.enter_context(nc.psum_tensor("psb%d" % i, [128, 512], F32))[:, :] for i in range(8)]
        identb = cb16[:, 0:128]; onesb = cb16[:, 128:256]; trib = cb16[:, 256:384]; iotab = cb16[:, 384:512]
        ident = cs[:, C_CST:C_CST + 128]; tri = cs[:, C_CST + 128:C_CST + 256]
        onesf = cs[:, C_CST + 256:C_CST + 384]; iota = cs[:, C_CST + 384:C_CST + 512]
        col = lambda c, n=1: cs[:, c:c + n]
        eps_c = col(C_EPS); one_c = col(C_ONE); lns_c = col(C_LNS); flag_c = col(C_FLAG)

        off = [0]
        def carve(words, dty=F32, shape=None):
            a = ar[:, off[0]:off[0] + words]; off[0] += words
            assert off[0] <= NW, off[0]
            if dty != F32: a = a.bitcast(dty)
            if shape is not None:
                names = " ".join("d%d" % i for i in range(len(shape)))
                a = a.rearrange("p (%s) -> p %s" % (names, names), **{"d%d" % i: s for i, s in enumerate(shape)})
            return a

        b.dma("sp", cs[:, :], cst_d[:, :], [], ["cs"], "cs")
        b.cp("dve", identb, ident, ["cs"], ["cb16"])
        b.cp("dve", onesb, onesf, ["cs"], ["cb16"])
        b.cp("dve", trib, tri, ["cs"], ["cb16"])
        b.cp("dve", iotab, iota, ["cs"], ["cb16"])

        Winb = carve(12320, BF16, [8, 3080]); Woutb = carve(4096, BF16, [8, 1024])
        xTs = carve(4096, F32, [8, 512]); raw = carve(4120, F32, [8, 515])
        sqb = carve(512, BF16, [2, 512]); lnv = carve(512); rstdb = carve(512)
        hT = carve(2048, BF16, [8, 512]); accs = carve(1024, F32, [2, 512]); qkT = carve(2048, BF16, [8, 512])
        WsTb = carve(256, BF16, [4, 128])
        sm = carve(128)
        sg = carve(192)
        vext = carve(264, BF16, [4, 132]); sig = carve(256, BF16); u_sb = carve(256, BF16)
        vgs = carve(512); vn32 = carve(512); vnb = carve(256, BF16); ktok = carve(256, BF16)
        PT = carve(256, BF16, [4, 128]); ybf = carve(512, BF16); yT = carve(512, BF16, [8, 128])
        xmt = carve(1024); x1 = carve(1024); xs = carve(512, BF16)
        Gst = carve(528, F32, [4, 132]); Cb = carve(264, BF16, [4, 132])
        offA = off[0]

        gsb = sg[:, 0:32]; e1 = sg[:, 32:48]; nlf = sg[:, 48:64]; t16 = sg[:, 64:80]; EK = sg[:, 80:96]; EB = sg[:, 96:112]
        A_t = [sg[:, 112:128], sg[:, 128:144]]; nhl16 = sg[:, 144:160].bitcast(BF16); nhi = nhl16[:, 0:16]; nlo = nhl16[:, 16:32]
        gsb3 = gsb.rearrange("p (j g) -> p j g", j=4)
        den = sm[:, 36:40]; dab = sm[:, 40:44]
        rr = sm[:, 44:48]; ssq = sm[:, 48:52]; mm4 = sm[:, 52:56]; scl = sm[:, 56:60]; st8 = sm[:, 60:68]
        mean = sm[:, 68:72]; msq = sm[:, 72:76]; var = sm[:, 76:80]; rs4 = sm[:, 80:84]
        ss2 = sm[:, 84:85]; l2 = sm[:, 85:86]; r2 = sm[:, 86:87]
        nhl = sm[:, 88:92].bitcast(BF16)
        psP = [psb[0], psb[1]]; psS = psb[3]; psO = [psb[4], psb[5]]; psU = [psb[6], psb[7]]
        psM = psb[2]
        psT = psM[:, 256:512].bitcast(BF16)
        Prog.BANK.clear()
        Prog.BANK.update({"psP0": "B0", "psP1": "B1", "psMg": "B2", "psMb": "B2", "psMl": "B2", "psT": "B2", "psS": "B3",
                          "psO0": "B4", "psO1": "B5", "psU0": "B6", "psU1": "B7",
                          "pq0": "B0", "pq1": "B1", "psc0": "B2", "psc1": "B3", "psc2": "B4", "psc3": "B5",
                          "pt6a": "B6", "pt6b": "B6", "pt6c": "B6",
                          "psOut0": "B0", "psOut1": "B1", "psOut2": "B2", "psOut3": "B3", "psA0": "B4", "psA1": "B5",
                          "psG0": "B6", "psG1": "B7"})
        pcnt = [0]
        def nextP():
            pcnt[0] += 1
            return psP[pcnt[0] % 2], "psP%d" % (pcnt[0] % 2)

        stg = [xTs.rearrange("p a b -> p (a b)"), raw.rearrange("p a b -> p (a b)")]
        for c in range(8):
            s = stg[c % 2][:, 0:3080]; sk = "stg%d" % (c % 2)
            b.dma("sp", s, win_d[:, c, :], [], [sk], sk)
            eng = "dve"
            b.cp(eng, Winb[:, c, 0:1540], s[:, 0:1540], [sk], ["Winb"])
            b.act(Winb[:, c, 1540:3080], s[:, 1540:3080], AF.Copy, [sk], ["Winb"])
        for hf in range(2):
            s = stg[hf][:, 0:4096]; sk = "stg%d" % hf
            b.dma("sp", s, wout_d[:, 4 * hf:4 * hf + 4, :], [], [sk], sk)
            for c4 in range(4):
                c = 4 * hf + c4
                if hf == 0:
                    b.ts("dve", Woutb[:, c, :], s[:, c4 * 1024:(c4 + 1) * 1024], col(C_NG + c), None, ALU.mult, None, [sk, "cs"], ["Woutb"])
                else:
                    b.cp("dve", Woutb[:, c, :], s[:, c4 * 1024:(c4 + 1) * 1024], [sk], ["Woutb"])
        b.cp("dve", WsTb.rearrange("p a b -> p (a b)"), cs[:, C_WS:C_WS + 512], ["cs"], ["WsTb"])
        for g in range(4):
            b.ms("dve", WsTb[64:128, g, 0:64], 0.0, ["WsTb"])
        b.ms("dve", raw.rearrange("p a b -> p (a b)"), 0.0, ["stg1"] + ["raw%d" % c for c in range(8)])
        b.ms("dve", Gst.rearrange("p a b -> p (a b)"), 0.0, ["G"])
        b.ms("dve", Cb.rearrange("p a b -> p (a b)"), 0.0, ["Cb"])
        b.ms("dve", A_t[1], 1.0, ["A1"])
        b.ms("dve", vext.rearrange("p a b -> p (a b)"), 0.0, ["vext"])

        for blk in range(int(os.environ.get("TRUNC", "8"))):
            main = blk >= 4
            tok0 = blk * 512
            for c in range(8):
                b.dma("sp", xTs[:, c, :], xT_d[:, c, tok0:tok0 + 512], [], ["xTs0", "stg0"] if c == 0 else ["xTs%d" % c], "xTs%d" % c)
            for c in range(8):
                b.act(sqb[:, c % 2, :], xTs[:, c, :], AF.Square, ["xTs%d" % c], ["sqb%d" % (c % 2)])
                b.mm(psS, onesb, sqb[:, c % 2, :], c == 0, c == 7, ["cb16", "sqb%d" % (c % 2)], ["psS"])
            b.act(lnv, psS, AF.Ln, ["psS", "cs"], ["lnv"], scale=1.0 / 1024, bias=eps_c)
            b.act(rstdb, lnv, AF.Exp, ["lnv"], ["rstdb"], scale=-0.5)
            for c in range(8):
                b.stt("dve", hT[:, c, :], xTs[:, c, :], col(C_G1 + c), rstdb, ALU.mult, ALU.mult, ["xTs%d" % c, "cs", "rstdb"], ["hT"])
            par = blk % 2
            for j in range(4):
                for c in range(8):
                    b.mm(psM[:, j * 8:(j + 1) * 8], hT[:, c, j * 128:(j + 1) * 128], Winb[:, c, 2048:2056], c == 0, c == 7, ["hT", "Winb"], ["psMg"])
            b.tt("dve", gsb3, psM[:, 0:32].rearrange("p (j g) -> p j g", j=4), col(C_GB, 8).unsqueeze(1).to_broadcast([128, 4, 8]), ALU.add,
                 ["psMg", "cs"], ["gsb"])
            b.act(e1.rearrange("p (j h) -> p j h", j=4), gsb3[:, :, 4:8], AF.Exp, ["gsb"], ["e1"], scale=-1.0)
            b.act(nlf, e1, AF.Ln, ["e1", "cs"], ["nlf"], scale=1.0, bias=one_c)
            b.cp("dve", nhi, nlf, ["nlf"], ["nhi"])
            b.tt("dve", nlo, nlf, nhi, ALU.subtract, ["nlf", "nhi"], ["nlo"])
            for j in range(4):
                js = slice(j * 4, (j + 1) * 4)
                b.mm(psM[:, 64 + j * 4:68 + j * 4], trib, nhi[:, js], True, False, ["cb16", "nhi"], ["psMb"])
                b.mm(psM[:, 64 + j * 4:68 + j * 4], trib, nlo[:, js], False, True, ["cb16", "nlo"], ["psMb"])
                b.mm(psM[:, 96 + j * 4:100 + j * 4], onesb, nhi[:, js], True, False, ["cb16", "nhi"], ["psMl"])
                b.mm(psM[:, 96 + j * 4:100 + j * 4], onesb, nlo[:, js], False, True, ["cb16", "nlo"], ["psMl"])
            b.tt("dve", t16.rearrange("p (j h) -> p j h", j=4), gsb3[:, :, 0:4], psM[:, 64:80].rearrange("p (j h) -> p j h", j=4), ALU.add,
                 ["gsb", "psMb"], ["t4"])
            b.act(EK, t16, AF.Exp, ["t4", "cs"], ["ek"], scale=1.0, bias=lns_c)
            b.act(EB, psM[:, 64:80], AF.Exp, ["psMb"], ["eb"], scale=-1.0)
            b.act(A_t[par], psM[:, 96:112], AF.Exp, ["psMl"], ["A%d" % par], scale=-1.0)
            if blk == 3:
                b.ts("dve", A_t[1][:, 12:16], A_t[1][:, 12:16], flag_c, None, ALU.mult, None, ["A1", "cs"], ["A1"])
            cc_list = list(range(8)) if blk >= 3 else list(range(4, 8))
            for cc in cc_list:
                ps, pk = nextP()
                for c in range(8):
                    b.mm(ps, Winb[:, c, cc * 128:(cc + 1) * 128], hT[:, c, :], c == 0, c == 7, ["Winb", "hT"], [pk])
                rk = "raw%d" % cc
                if blk == 4:
                    b.ts("dve", raw[:, cc, 0:3], raw[:, cc, 512:515], flag_c, None, ALU.mult, None, [rk, "cs"], [rk])
                elif blk > 0:
                    b.cp("dve", raw[:, cc, 0:3], raw[:, cc, 512:515], [rk], [rk])
                b.act(raw[:, cc, 3:515], ps, AF.Copy, [pk], [rk])
                if cc >= 4 or main:
                    ac = accs[:, cc % 2, :]; ak = "acc%d" % (cc % 2)
                    b.act(ac, raw[:, cc, 3:515], AF.Identity, [rk, "cs"], [ak], scale=col(C_CW + 3 * 8 + cc), bias=col(C_CB + cc))
                    for j in (2, 1, 0):
                        b.stt("dve", ac, raw[:, cc, j:j + 512], col(C_CW + j * 8 + cc), ac, ALU.mult, ALU.add, [rk, "cs", ak], [ak])
                    b.act(qkT[:, cc, :], ac, AF.Silu, [ak], ["qkT%d" % cc])
            for j in range(4):
                tsl = slice(j * 128, (j + 1) * 128)
                cidx = blk * 4 + j
                js = slice(j * 4, (j + 1) * 4)
                ek = EK[:, js]; eb = EB[:, js]
                acur = A_t[par][:, js]; ack = "A%d" % par
                if j > 0:
                    aprev = A_t[par][:, (j - 1) * 4:j * 4]; apk = ack
                else:
                    aprev = A_t[1 - par][:, 12:16]; apk = "A%d" % (1 - par)
                ps, pk = nextP()
                for c in range(8):
                    b.mm(ps, hT[:, c, tsl], Winb[:, c, 1024:1536], c == 0, c == 7, ["hT", "Winb"], [pk])
                for h in range(4):
                    b.act(vext[:, h, 0:128], ps[:, h * 128:(h + 1) * 128], AF.Copy, [pk, "ek"], ["vext"], scale=ek[:, h:h + 1])
                b.cp("dve", vext[:, :, 128], ek, ["ek"], ["vext"])
                for h in range(4):
                    b.tr(psT[:, h * 128:(h + 1) * 128], qkT[:, 4 + h, tsl], identb, ["qkT%d" % (4 + h), "cb16"], ["psT"])
                b.cp("dve", ktok, psT, ["psT"], ["ktok"])
                if main:
                    for h in range(4):
                        b.mm(psS[:, h * 128:(h + 1) * 128], qkT[:, 4 + h, tsl], qkT[:, h, tsl], True, True,
                             ["qkT%d" % (4 + h), "qkT%d" % h], ["psS"])
                    b.tt("dve", PT, psS.rearrange("p (a b) -> p a b", a=4), tri.unsqueeze(1).to_broadcast([128, 4, 128]),
                         ALU.mult, ["psS", "cs"], ["PT"])
                    for h in range(4):
                        o = psO[h // 2][:, (h % 2) * 256:(h % 2) * 256 + 129]; ok = "psO%d" % (h // 2)
                        b.mm(o, PT[:, h, :], vext[:, h, 0:129], True, False, ["PT", "vext"], [ok])
                        b.mm(o, qkT[:, h, tsl], Cb[:, h, 0:129], False, True, ["qkT%d" % h, "Cb"], [ok])
                for h in range(4):
                    u = psU[h // 2][:, (h % 2) * 256:(h % 2) * 256 + 129]; uk = "psU%d" % (h // 2)
                    b.mm(u, ktok[:, h * 128:(h + 1) * 128], vext[:, h, 0:129], True, True, ["ktok", "vext"], [uk])
                for h in range(4):
                    u = psU[h // 2][:, (h % 2) * 256:(h % 2) * 256 + 129]; uk = "psU%d" % (h // 2)
                    b.stt("dve", Gst[:, h, 0:129], Gst[:, h, 0:129], aprev[:, h:h + 1], u, ALU.mult, ALU.add, ["G", apk, uk], ["G"])
                    if cidx >= 15:
                        b.ts("dve", Cb[:, h, 0:129], Gst[:, h, 0:129], acur[:, h:h + 1], None, ALU.mult, None, ["G", ack], ["Cb"])
                if not main:
                    continue
                ps, pk = nextP()
                for c in range(8):
                    b.mm(ps, hT[:, c, tsl], Winb[:, c, 1536:2048], c == 0, c == 7, ["hT", "Winb"], [pk])
                b.act(sig, ps, AF.Sigmoid, [pk], ["sig"])
                ps, pk = nextP()
                for c in range(8):
                    b.mm(ps, hT[:, c, tsl], Winb[:, c, 2056:2568], c == 0, c == 7, ["hT", "Winb"], [pk])
                b.act(u_sb, ps, AF.Gelu_apprx_tanh, [pk], ["u_sb"])
                ps, pk = nextP()
                for c in range(8):
                    b.mm(ps, hT[:, c, tsl], Winb[:, c, 2568:3080], c == 0, c == 7, ["hT", "Winb"], [pk])
                b.ms("dve", st8, 0.0, ["st8"])
                for g in range(4):
                    b.act(vgs[:, g * 128:(g + 1) * 128], ps[:, g * 128:(g + 1) * 128], AF.Gelu_apprx_tanh, [pk, "st8"], ["vgs", "st8"],
                          accum=st8[:, g:g + 1])
                for g in range(4):
                    b.act(vn32[:, g * 128:(g + 1) * 128], vgs[:, g * 128:(g + 1) * 128], AF.Square, ["vgs", "st8"], ["vn32", "st8"],
                          accum=st8[:, 4 + g:5 + g])
                b.ts("dve", mean, st8[:, 0:4], 1.0 / 128, None, ALU.mult, None, ["st8"], ["mean"])
                b.tt("dve", msq, mean, mean, ALU.mult, ["mean"], ["msq"])
                b.stt("dve", var, st8[:, 4:8], 1.0 / 128, msq, ALU.mult, ALU.subtract, ["st8", "msq"], ["var"])
                b.act(var, var, AF.Ln, ["var", "cs"], ["var"], scale=1.0, bias=eps_c)
                b.act(rs4, var, AF.Exp, ["var"], ["rs4"], scale=-0.5)
                for g in range(4):
                    b.ts("dve", vn32[:, g * 128:(g + 1) * 128], vgs[:, g * 128:(g + 1) * 128], mean[:, g:g + 1], rs4[:, g:g + 1],
                         ALU.subtract, ALU.mult, ["vgs", "mean", "rs4"], ["vn32"])
                b.tt("dve", vn32, vn32, cs[:, C_LNG:C_LNG + 512], ALU.mult, ["vn32", "cs"], ["vn32"])
                b.tt("dve", vnb, vn32, cs[:, C_LNB:C_LNB + 512], ALU.add, ["vn32", "cs"], ["vnb"])
                for g in range(4):
                    b.mm(psS[:, g * 128:(g + 1) * 128], WsTb[:, g, :], vnb[:, g * 128:(g + 1) * 128], True, True, ["WsTb", "vnb"], ["psS"])
                for g in range(4):
                    b.stt("dve", ybf[:, 512 + g * 128:512 + (g + 1) * 128], psS[:, g * 128:(g + 1) * 128], col(C_BS + g),
                          u_sb[:, g * 128:(g + 1) * 128], ALU.add, ALU.mult, ["psS", "cs", "u_sb"], ["ybf"])
                for p2 in range(2):
                    dv = psO[p2].rearrange("p (h w) -> p h w", h=2)[:, :, 128]
                    b.tt("dve", den[:, 2 * p2:2 * p2 + 2], dv, eb[:, 2 * p2:2 * p2 + 2], ALU.mult, ["psO%d" % p2, "eb"], ["den"])
                b.act(dab, den, AF.Abs, ["den"], ["dab"])
                b.ts("dve", dab, dab, 1.0, None, ALU.max, None, ["dab"], ["dab"])
                b.P.op("dve", lambda e: e.reciprocal(out=dab, in_=dab), reads=["dab"], writes=["dab"])
                b.tt("dve", rr, eb, dab, ALU.mult, ["eb", "dab"], ["rr"])
                b.ms("dve", ssq, 0.0, ["ssq"])
                for h in range(4):
                    o = psO[h // 2][:, (h % 2) * 256:(h % 2) * 256 + 128]
                    b.act(vn32[:, 0:128], o, AF.Square, ["psO%d" % (h // 2), "ssq"], ["vn32", "ssq"], accum=ssq[:, h:h + 1])
                b.tt("dve", mm4, ssq, rr, ALU.mult, ["ssq", "rr"], ["mm4"])
                b.tt("dve", mm4, mm4, rr, ALU.mult, ["mm4", "rr"], ["mm4"])
                b.act(mm4, mm4, AF.Ln, ["mm4", "cs"], ["mm4"], scale=1.0 / 128, bias=eps_c)
                b.act(mm4, mm4, AF.Exp, ["mm4"], ["mm4"], scale=-0.5)
                b.tt("dve", scl, rr, mm4, ALU.mult, ["rr", "mm4"], ["scl"])
                for h in range(4):
                    o = psO[h // 2][:, (h % 2) * 256:(h % 2) * 256 + 128]
                    b.stt("dve", ybf[:, h * 128:(h + 1) * 128], o, scl[:, h:h + 1], sig[:, h * 128:(h + 1) * 128], ALU.mult, ALU.mult,
                          ["psO%d" % (h // 2), "scl", "sig"], ["ybf"])
                for hf in range(2):
                    for q in range(4):
                        b.tr(psT[:, q * 128:(q + 1) * 128], ybf[:, (hf * 4 + q) * 128:(hf * 4 + q + 1) * 128], identb, ["ybf", "cb16"], ["psT"])
                    b.cp("dve", yT[:, hf * 4:hf * 4 + 4, :].rearrange("p a b -> p (a b)"), psT, ["psT"], ["yT"])
                for hf in range(2):
                    for c in range(8):
                        b.mm(psO[hf], yT[:, c, :], Woutb[:, c, hf * 512:(hf + 1) * 512], c == 0, c == 7, ["yT", "Woutb"], ["psO%d" % hf])
                mt0 = (blk - 4) * 512 + j * 128
                b.dma("sp", xmt, xm_d[mt0:mt0 + 128, :], [], ["xmt"], "xmt")
                for hf in range(2):
                    b.tt("dve", x1[:, hf * 512:(hf + 1) * 512], psO[hf], xmt[:, hf * 512:(hf + 1) * 512], ALU.add, ["psO%d" % hf, "xmt"], ["x1"])
                b.dma("sp", x1_d[mt0:mt0 + 128, :], x1, ["x1"], ["x1d%d" % (mt0 // 128)], "x1st")
                if stage == "A":
                    continue
                b.ms("dve", ss2, 0.0, ["ss2"])
                b.act(xs, x1, AF.Square, ["x1", "ss2"], ["xs", "ss2"], accum=ss2)
                b.act(l2, ss2, AF.Ln, ["ss2", "cs"], ["l2"], scale=1.0 / 1024, bias=eps_c)
                b.act(r2, l2, AF.Exp, ["l2"], ["r2"], scale=-0.5)
                b.act(xs, x1, AF.Copy, ["x1", "r2"], ["xs"], scale=r2)
                for hf in range(2):
                    for q in range(4):
                        b.tr(psT[:, q * 128:(q + 1) * 128], xs[:, (hf * 4 + q) * 128:(hf * 4 + q + 1) * 128], identb, ["xs", "cb16"], ["psT"])
                    for q in range(4):
                        b.ts("dve", xn2T[:, hf * 4 + q, mt0:mt0 + 128], psT[:, q * 128:(q + 1) * 128], col(C_G2 + hf * 4 + q), None,
                             ALU.mult, None, ["psT", "cs"], ["xn2T"])
        fkeys = ["x1d%d" % i for i in range(16)]
        if stage not in ("A", "A2"):
            fkeys = build_peer(nc, b, carve, off, cs, xn2T, identb, ident, iotab, psb, wq_d, keys_d, ut_d, v_d, x1_d, y_d, stage, ub_d, vb_d)
        P.emit(final_keys=fkeys)
    return nc


import os


def build_peer(nc, b, carve, off, cs, xn2T, identb, ident, iota, psb, wq_d, keys_d, ut_d, v_d, x1_d, y_d, stage='full', ub_d=None, vb_d=None):
    P = b.P
    P.fence()
    off[0] = 0
    col = lambda c, n=1: cs[:, c:c + n]
    eps_c = col(C_EPS)
    I1T = carve(2048); I2T = carve(2048); GTs = carve(2048)
    offBC = off[0]
    Wqb = carve(8192, BF16, [8, 2048]); keysTb = carve(1024, BF16, [16, 128]); stg = carve(2048)
    qTb = carve(1024, BF16, [16, 128]); sc2 = carve(4096, F32, [2, 16, 128]); wk = carve(2048, F32, [16, 128])
    m16 = carve(256, F32, [16, 16]); i16 = carve(256, U32, [16, 16]); i16f = carve(256, F32, [16, 16])
    NC = 80; RECTS = [(0, 2, 16, 0), (2, 2, 8, 32), (4, 4, 4, 48), (8, 8, 2, 64)]
    cand = carve(8 * NC, F32, [8, NC]); cidx = carve(8 * NC, F32, [8, NC]); junk = carve(4 * NC, F32, [4, NC])
    ts16 = carve(128, F32, [8, 16]); eidx = carve(128); ex = carve(128, F32, [8, 16]); gate = carve(128, F32, [8, 16])
    ei = carve(128, I32); i1i = carve(128, I32); i2i = carve(128, I32); i1f = carve(128); i2f = carve(128)
    tb3 = carve(192, BF16)
    smb = carve(32); negm = smb[:, 0:8]; Z = smb[:, 8:16]; rz = smb[:, 16:24]
    cin = carve(6144, F32, [3, 2, 1024]); cout = carve(3072, BF16, [3, 2, 1024])
    jobs = {"in": 0, "cv": 0}

    def conv_in():
        j = jobs["in"]
        if j >= 128: return
        jobs["in"] += 1; s3 = j % 3
        b.dma("sp", cin[:, s3, 0, :], ut_d[j].rearrange("p c e -> p (c e)"), [], ["cinu%d" % s3], "cinu%d" % s3)
        b.dma("sp", cin[:, s3, 1, :], v_d[j * 128:(j + 1) * 128, :], [], ["cinv%d" % s3], "cinv%d" % s3)

    def conv_cv():
        j = jobs["cv"]
        if j >= 128: return
        jobs["cv"] += 1; s3 = j % 3
        b.act(cout[:, s3, 0, :], cin[:, s3, 0, :], AF.Copy, ["cinu%d" % s3], ["coutu%d" % s3])
        b.act(cout[:, s3, 1, :], cin[:, s3, 1, :], AF.Copy, ["cinv%d" % s3], ["coutv%d" % s3])
        b.dma("sp", ub_d[j], cout[:, s3, 0, :], ["coutu%d" % s3], ["ubd"], "cou%d" % s3)
        b.dma("sp", vb_d[j * 128:(j + 1) * 128, :], cout[:, s3, 1, :], ["coutv%d" % s3], ["vbd"], "cov%d" % s3)

    conv_in(); conv_in()
    for c in range(8):
        b.dma("sp", stg, wq_d[:, c, :], [], ["stgB"], "stgB")
        b.cp("dve", Wqb[:, c, 0:1024], stg[:, 0:1024], ["stgB"], ["Wqb"])
        b.act(Wqb[:, c, 1024:2048], stg[:, 1024:2048], AF.Copy, ["stgB"], ["Wqb"])
    b.dma("sp", stg, keys_d[:, :, :].rearrange("p a b -> p (a b)"), [], ["stgB"], "stgB")
    b.cp("dve", keysTb.rearrange("p a b -> p (a b)"), stg, ["stgB"], ["keysTb"])
    PB = int(os.environ.get("PB_STOP", "99"))
    if PB <= 0: return ["x1d%d" % q for q in range(16)]
    def part1(i):
        t0 = i * 128; par = i % 2; sc = sc2[:, par]
        for hp in range(16):
            ps = psb[hp % 2]; pk = "pq%d" % (hp % 2)
            for c in range(8):
                b.mm(ps[:, 0:128], Wqb[:, c, hp * 128:(hp + 1) * 128], xn2T[:, c, t0:t0 + 128], c == 0, c == 7, ["Wqb", "xn2T"], [pk])
            b.act(qTb[:, hp, :], ps[:, 0:128], AF.Copy, [pk], ["qTb%d" % hp])
            if hp % 2 == 0:
                conv_in(); conv_cv()
        for hp in range(16):
            b.mm(psb[2 + hp // 4][:, (hp % 4) * 128:(hp % 4 + 1) * 128], qTb[:, hp, :], keysTb[:, hp, :], True, True,
                 ["qTb%d" % hp, "keysTb"], ["psc%d" % (hp // 4)])
        for q4 in range(4):
            b.act(sc[:, q4 * 4:q4 * 4 + 4, :].rearrange("p a b -> p (a b)"), psb[2 + q4], AF.Copy, ["psc%d" % q4], ["sc%d_%d" % (q4, par)])

    def part2(i):
        t0 = i * 128; par = i % 2; sc = sc2[:, par]
        for hp in range(16):
            b.P.op("dve", (lambda hp: lambda e: e.max(out=m16[:, hp, 0:8], in_=sc[:, hp, :]))(hp), reads=["sc%d_%d" % (hp // 4, par)], writes=["m16_%d" % hp])
        for hp in range(16):
            b.P.op("dve", (lambda hp: lambda e: e.max_index(out=i16[:, hp, 0:8], in_max=m16[:, hp, 0:8], in_values=sc[:, hp, :]))(hp),
                   reads=["sc%d_%d" % (hp // 4, par), "m16_%d" % hp], writes=["i16_%d" % hp])
        for hp in range(16):
            b.P.op("dve", (lambda hp: lambda e: e.match_replace(out=wk[:, hp, :], in_to_replace=m16[:, hp, 0:8], in_values=sc[:, hp, :], imm_value=-1e30))(hp),
                   reads=["sc%d_%d" % (hp // 4, par), "m16_%d" % hp], writes=["wk%d" % hp])
        for hp in range(16):
            b.P.op("dve", (lambda hp: lambda e: e.max(out=m16[:, hp, 8:16], in_=wk[:, hp, :]))(hp), reads=["wk%d" % hp], writes=["m16_%d" % hp])
        for hp in range(16):
            b.P.op("dve", (lambda hp: lambda e: e.max_index(out=i16[:, hp, 8:16], in_max=m16[:, hp, 8:16], in_values=wk[:, hp, :]))(hp),
                   reads=["wk%d" % hp, "m16_%d" % hp], writes=["i16_%d" % hp])
        allm = ["m16_%d" % hp for hp in range(16)]; alli = ["i16_%d" % hp for hp in range(16)]
        b.cp("dve", i16f.rearrange("p a b -> p (a b)"), i16.rearrange("p a b -> p (a b)"), alli, ["i16f"])
        m16h = m16.rearrange("p (h two) a -> p h two a", two=2)
        i16h = i16f.rearrange("p (h two) a -> p h two a", two=2)
        allc = ["cand%d" % h for h in range(8)]; allx = ["cidx%d" % h for h in range(8)]
        b.ts("dve", i16h[:, :, 0, :], i16h[:, :, 0, :], 128.0, None, ALU.mult, None, ["i16f"], ["i16f"])
        for (a0, na, nb, o0) in RECTS:
            shp = [128, 8, na, nb]
            b.tt("dve", cand[:, :, o0:o0 + na * nb].rearrange("p h (a c) -> p h a c", a=na),
                 m16h[:, :, 0, a0:a0 + na].unsqueeze(3).to_broadcast(shp), m16h[:, :, 1, 0:nb].unsqueeze(2).to_broadcast(shp),
                 ALU.add, allm, allc)
            b.tt("dve", cidx[:, :, o0:o0 + na * nb].rearrange("p h (a c) -> p h a c", a=na),
                 i16h[:, :, 0, a0:a0 + na].unsqueeze(3).to_broadcast(shp), i16h[:, :, 1, 0:nb].unsqueeze(2).to_broadcast(shp),
                 ALU.add, ["i16f"], allx)
        wk2 = wk.rearrange("p a b -> p (a b)").rearrange("p (a b) -> p a b", a=8)[:, :, 0:NC]
        for h in range(8):
            b.P.op("dve", (lambda h: lambda e: e.max(out=ts16[:, h, 0:8], in_=cand[:, h, :]))(h), reads=["cand%d" % h], writes=["ts16_%d" % h])
        for h in range(8):
            b.P.op("dve", (lambda h: lambda e: e.match_replace(out=wk2[:, h, :], in_to_replace=ts16[:, h, 0:8], in_values=cand[:, h, :], imm_value=-1e30))(h),
                   reads=["cand%d" % h, "ts16_%d" % h] + ["wk%d" % (2 * h), "wk%d" % (2 * h + 1)], writes=["wk%d" % (2 * h), "wk%d" % (2 * h + 1)])
        for h in range(8):
            b.P.op("dve", (lambda h: lambda e: e.max(out=ts16[:, h, 8:16], in_=wk2[:, h, :]))(h), reads=["wk%d" % (2 * h), "wk%d" % (2 * h + 1)], writes=["ts16_%d" % h])
        b.ms("dve", eidx, 0.0, ["eidx"])
        if os.environ.get("TRIV"):
            for q in range(int(os.environ["TRIV"])):
                b.ms("dve", junk[:, q % 4, :], 0.0, ["junk%d" % (q % 4)])
            return ["x1d%d" % q for q in range(16)]
        for j in range(16):
            for h in range(8):
                jk = "junk%d" % ((j * 8 + h) % 4)
                b.stt("dve", junk[:, (j * 8 + h) % 4, :], cand[:, h, :], ts16[:, h, j:j + 1], cidx[:, h, :], ALU.is_equal, ALU.mult,
                      ["cand%d" % h, "ts16_%d" % h, "cidx%d" % h, "eidx"], [jk, "eidx%d" % (h * 16 + j)], accum=(None if os.environ.get("NOACC") else eidx[:, h * 16 + j:h * 16 + j + 1]))
        alle = ["eidx"] + ["eidx%d" % q for q in range(128)]
        allts = ["ts16_%d" % h for h in range(8)]
        b.ts("dve", negm, ts16[:, :, 0], -1.0, None, ALU.mult, None, allts, ["negm"])
        b.ms("dve", Z, 0.0, ["Z"])
        for h in range(8):
            b.act(ex[:, h, :], ts16[:, h, :], AF.Exp, allts + ["negm", "Z"], ["ex", "Z"], scale=1.0, bias=negm[:, h:h + 1], accum=Z[:, h:h + 1])
        b.P.op("dve", lambda e: e.reciprocal(out=rz, in_=Z), reads=["Z"], writes=["rz"])
        b.tt("dve", gate, ex, rz.unsqueeze(2).to_broadcast([128, 8, 16]), ALU.mult, ["ex", "rz"], ["gate"])
        b.cp("dve", ei, eidx, alle, ["ei"])
        b.ts("dve", i1i, ei, 7, None, ALU.arith_shift_right, None, ["ei"], ["i1i"])
        b.ts("dve", i2i, ei, 127, None, ALU.bitwise_and, None, ["ei"], ["i2i"])
        b.cp("dve", tb3[:, 0:128], i1i, ["i1i"], ["tb3a"])
        b.cp("dve", tb3[:, 128:256], i2i, ["i2i"], ["tb3b"])
        b.cp("dve", tb3[:, 256:384], gate.rearrange("p a b -> p (a b)"), ["gate"], ["tb3c"])
        pT6 = psb[6].bitcast(BF16)
        b.tr(pT6[:, 0:128], tb3[:, 0:128], identb, ["tb3a"], ["pt6a"])
        b.tr(pT6[:, 128:256], tb3[:, 128:256], identb, ["tb3b"], ["pt6b"])
        b.tr(pT6[:, 256:384], tb3[:, 256:384], identb, ["tb3c"], ["pt6c"])
        b.act(I1T[:, t0:t0 + 128], pT6[:, 0:128], AF.Copy, ["pt6a"], ["I1T"])
        b.act(I2T[:, t0:t0 + 128], pT6[:, 128:256], AF.Copy, ["pt6b"], ["I2T"])
        b.act(GTs[:, t0:t0 + 128], pT6[:, 256:384], AF.Copy, ["pt6c"], ["GTs"])
    part1(0)
    for i in range(16):
        if i + 1 < 16: part1(i + 1)
        part2(i)
    while jobs["cv"] < 128:
        conv_in(); conv_cv()
    if stage == "AB":
        return ["x1d%d" % i for i in range(16)]
    P.fence()
    off[0] = offBC
    GT = carve(16384, BF16, [128, 256]); ohA = carve(256, BF16, [8, 64]); ohB = carve(512, BF16, [8, 128])
    NSL = 4
    Ub = carve(NSL * 512, BF16, [NSL, 8, 128]); Vb = carve(NSL * 512, BF16, [NSL, 1024])
    Ab = carve(256, BF16, [2, 256]); AG = carve(256, BF16, [2, 256])
    x1t = carve(1024); x2 = carve(1024); ot = carve(1024); smc = carve(16)
    ssf = smc[:, 0:1]; lf_ = smc[:, 1:2]; rf = smc[:, 2:3]
    psOut = psb[0:4]; psA = psb[4:6]; psG = psb[6:8]
    fkeys = []
    GL = [(0, 0, g) for g in range(64)]
    for T in range(8):
        GL += [(T, 1, g) for g in range(64)]
        if T + 1 < 8: GL += [(T + 1, 0, g) for g in range(64)]
    gn = [0]

    def gstep():
        n = gn[0]; gn[0] += 1
        if n + 1 < len(GL):
            T, half, grp = GL[n + 1]; p = (n + 1) % 2; h0 = 64 * half
            for s4 in range(4):
                t = T * 256 + grp * 4 + s4; sl = p * 4 + s4
                b.ts("dve", ohA[:, sl, :], iota[:, h0:h0 + 64], I1T[:, t:t + 1], GTs[:, t:t + 1], ALU.is_equal, ALU.mult,
                     ["cb16", "I1T", "GTs"], ["ohA%d" % sl])
                b.ts("dve", ohB[:, sl, :], iota, I2T[:, t:t + 1], None, ALU.is_equal, None, ["cb16", "I2T"], ["ohB%d" % sl])
        if n < len(GL):
            p = n % 2
            for s4 in range(4):
                sl = p * 4 + s4
                b.mm(psG[p][:, s4 * 64:(s4 + 1) * 64], ohB[:, sl, :], ohA[:, sl, :], True, True, ["ohA%d" % sl, "ohB%d" % sl], ["psG%d" % p])
        if 1 <= n <= len(GL):
            T, half, grp = GL[n - 1]; p = (n - 1) % 2; h0 = 64 * half
            b.act(GT[:, h0:h0 + 64, grp * 4:grp * 4 + 4], psG[p][:, 0:256].rearrange("p (t i) -> p i t", t=4), AF.Copy,
                  ["psG%d" % p], ["GT%d" % half])

    T, half, grp = GL[0]
    for s4 in range(4):
        t = grp * 4 + s4
        b.ts("dve", ohA[:, s4, :], iota[:, 0:64], I1T[:, t:t + 1], GTs[:, t:t + 1], ALU.is_equal, ALU.mult, ["cb16", "I1T", "GTs"], ["ohA%d" % s4])
        b.ts("dve", ohB[:, s4, :], iota, I2T[:, t:t + 1], None, ALU.is_equal, None, ["cb16", "I2T"], ["ohB%d" % s4])
    for _ in range(64):
        gstep()
    for T in range(8):
        tb = T * 256

        def stage1(blk):
            s4 = blk % NSL; s2 = blk % 2
            b.dma("sp", Ub[:, s4, :, :].rearrange("p a b -> p (a b)"), ub_d[blk], ["ubd"], ["Ub%d" % s4], "Ub%d" % s4)
            b.dma("sp", Vb[:, s4, :], vb_d[blk * 128:(blk + 1) * 128, :], ["vbd"], ["Vb%d" % s4], "Vb%d" % s4)
            for c in range(8):
                b.mm(psA[s2][:, 0:256], Ub[:, s4, c, :], xn2T[:, c, tb:tb + 256], c == 0, c == 7, ["Ub%d" % s4, "xn2T"], ["psA%d" % s2])

        def stage2(blk):
            s2 = blk % 2; s4 = blk % NSL
            b.act(Ab[:, s2, :], psA[s2][:, 0:256], AF.Gelu_apprx_tanh, ["psA%d" % s2], ["Ab%d" % s2])
            b.tt("dve", AG[:, s2, :], Ab[:, s2, :], GT[:, blk, :], ALU.mult, ["Ab%d" % s2, "GT%d" % (blk // 64)], ["AG%d" % s2])
            for sub in range(2):
                for hf in range(2):
                    b.mm(psOut[sub * 2 + hf], AG[:, s2, sub * 128:(sub + 1) * 128], Vb[:, s4, hf * 512:(hf + 1) * 512], blk == 0, blk == 127,
                         ["AG%d" % s2, "Vb%d" % s4], ["psOut%d" % (sub * 2 + hf)])

        for blk in range(129):
            if blk < 128: stage1(blk)
            if blk >= 1: stage2(blk - 1)
            if blk < 128:
                gstep()
        for sub in range(2):
            r0 = tb + sub * 128
            b.dma("sp", x1t, x1_d[r0:r0 + 128, :], ["x1d%d" % (r0 // 128)], ["x1t"], "x1t")
            for hf in range(2):
                b.tt("dve", x2[:, hf * 512:(hf + 1) * 512], psOut[sub * 2 + hf], x1t[:, hf * 512:(hf + 1) * 512], ALU.add,
                     ["psOut%d" % (sub * 2 + hf), "x1t"], ["x2"])
            b.ms("dve", ssf, 0.0, ["ssf"])
            b.act(ot, x2, AF.Square, ["x2", "ssf"], ["ot", "ssf"], accum=ssf)
            b.act(lf_, ssf, AF.Ln, ["ssf", "cs"], ["lf_"], scale=1.0 / 1024, bias=eps_c)
            b.act(rf, lf_, AF.Exp, ["lf_"], ["rf"], scale=-0.5)
            b.stt("dve", ot, x2, rf, cs[:, C_FGB:C_FGB + 1024], ALU.mult, ALU.mult, ["x2", "rf", "cs", "ot"], ["ot"])
            k = "yd%d" % (r0 // 128)
            b.dma("sp", y_d[r0:r0 + 128, :], ot, ["ot"], [k], "yst")
            fkeys.append(k)
    return fkeys


def host_inputs(inp):
    f = lambda a: np.ascontiguousarray(a, dtype=np.float32)
    x = inp["x"]
    consts = np.zeros((128, C_TOT), np.float32)
    cw = inp["conv_w"][0]
    consts[:, C_CW:C_CW + 32] = cw.reshape(4, 8, 128).transpose(2, 0, 1).reshape(128, 32)
    consts[:, C_CB:C_CB + 8] = inp["conv_b"][0].reshape(8, 128).T
    consts[:, C_G1:C_G1 + 8] = inp["norm1_g"][0].reshape(8, 128).T
    consts[:, C_G2:C_G2 + 8] = inp["norm2_g"][0].reshape(8, 128).T
    consts[:, C_NG:C_NG + 4] = inp["mlstm_norm_g"][0].reshape(4, 128).T
    consts[:, C_GB:C_GB + 4] = inp["b_igate"][0][None, :]
    consts[:, C_GB + 4:C_GB + 8] = inp["b_fgate"][0][None, :]
    consts[:, C_BS:C_BS + 4] = inp["gmlp_b_s"][0].T
    consts[:, C_EPS] = EPS; consts[:, C_ONE] = 1.0; consts[:, C_LNS] = np.float32(np.log(128.0 ** -0.5))
    consts[:, C_LNG:C_LNG + 512] = inp["gmlp_ln_g"][0][None, :]
    consts[:, C_LNB:C_LNB + 512] = inp["gmlp_ln_b"][0][None, :]
    consts[:, C_FGB:C_FGB + 1024] = inp["final_g"][None, :]
    consts[:, C_CST:C_CST + 128] = np.eye(128)
    consts[:, C_CST + 128:C_CST + 256] = np.triu(np.ones((128, 128)))
    consts[:, C_CST + 256:C_CST + 384] = 1.0
    consts[:, C_CST + 384:C_CST + 512] = np.arange(128)[None, :]
    consts[:, C_WS:C_WS + 512] = inp["gmlp_w_s"][0].transpose(2, 0, 1).reshape(128, 512)
    shared = {
        "win": f(inp["w_in"][0].reshape(8, 128, 3080).transpose(1, 0, 2)),
        "wout": f(inp["w_out"][0].reshape(8, 128, 1024).transpose(1, 0, 2)),
        "wq": f(inp["peer_w_query"][0].reshape(8, 128, 2048).transpose(1, 0, 2)),
        "keysT": f(inp["peer_sub_keys"][0].reshape(16, 128, 128).transpose(2, 0, 1)),
        "UT": f(inp["peer_u"][0].reshape(128, 128, 8, 128).transpose(0, 3, 2, 1)),
        "V": f(inp["peer_v"][0]),
    }
    maps = []
    for core in range(NCORES):
        bi, s = core // 2, core % 2
        xm = x[bi, s * NT:(s + 1) * NT]
        pre = x[bi, 0:NT]
        toks = np.concatenate([pre, xm], axis=0)
        xT = f(toks.T.reshape(8, 128, 4096).transpose(1, 0, 2))
        c = consts.copy(); c[:, C_FLAG] = float(s)
        m = dict(shared); m.update({"xT": xT, "xm": f(xm), "consts": c})
        maps.append(m)
    return maps


_NC_CACHE = {}


def kernel(**inputs):
    stage = "full"
    if stage not in _NC_CACHE:
        _NC_CACHE[stage] = build(stage)
    nc = _NC_CACHE[stage]
    maps = host_inputs({k: np.asarray(v) for k, v in inputs.items()})
    res = run_bass_kernel_spmd(nc, maps, core_ids=list(range(NCORES)))
    out = np.empty((4, 4096, 1024), np.float32)
    for core in range(NCORES):
        bi, s = core // 2, core % 2
        out[bi, s * NT:(s + 1) * NT] = res.results[core]["y"]
    return out
```

```python
import contextlib
import os
import numpy as np
import concourse.bass as bass
import concourse.mybir as mybir
from concourse.bass_utils import run_bass_kernel_spmd

F32 = mybir.dt.float32; BF16 = mybir.dt.bfloat16; U32 = mybir.dt.uint32; I32 = mybir.dt.int32
AF = mybir.ActivationFunctionType; ALU = mybir.AluOpType

NCORES = 8
NT = 2048
D = 1024
EPS = 1e-6
C_CW = 0; C_CB = 32; C_G1 = 40; C_G2 = 48; C_NG = 56; C_GB = 60; C_BS = 68; C_FLAG = 72
C_EPS = 73; C_ONE = 74; C_LNS = 75; C_SMALL = 80
C_LNG = 80; C_LNB = C_LNG + 512; C_FGB = C_LNB + 512; C_CST = C_FGB + 1024; C_WS = C_CST + 512
C_TOT = C_WS + 512


class Prog:
    ENG = ("pe", "act", "dve", "pool", "sp")

    def __init__(self, nc):
        self.nc = nc; self.ops = []; self.lastw = {}; self.readers = {}; self.dma_cnt = {}

    BANK = {}

    def op(self, eng, fn, reads=(), writes=(), dma=None):
        i = len(self.ops)
        reads = list(reads) + [self.BANK[k] for k in reads if k in self.BANK]
        writes = list(writes) + [self.BANK[k] for k in writes if k in self.BANK]
        deps = set()
        for k in list(reads) + list(writes):
            if k in self.lastw: deps.add(self.lastw[k])
        for k in writes:
            lastr = {}
            for r in self.readers.get(k, ()):
                ro = self.ops[r]
                if ro["dma"] is not None: deps.add(r)
                else: lastr[ro["eng"]] = r
            deps.update(lastr.values())
        o = dict(eng=eng, fn=fn, deps=deps, dma=dma, sig=False, dcount=None, sidx=None)
        if dma is not None:
            self.dma_cnt[dma] = self.dma_cnt.get(dma, 0) + 1
            o["dcount"] = self.dma_cnt[dma]
        self.ops.append(o)
        for k in writes:
            self.lastw[k] = i; self.readers[k] = []
        for k in reads:
            self.readers.setdefault(k, []).append(i)
        return i

    def fence(self):
        last = {}
        for i, o in enumerate(self.ops):
            if o["fn"] is None: continue
            if o["dma"] is not None: last[("d", o["dma"])] = i
            else: last[("e", o["eng"])] = i
        for e in self.ENG:
            i = self.op(e, None)
            self.ops[i]["deps"] = set(last.values())

    def emit(self, final_keys=()):
        nc = self.nc; ops = self.ops
        if not os.environ.get("NOFENCE"):
            self.fence()
        self.op("sp", None, reads=list(final_keys), writes=["__final"])
        pos = {e: 0 for e in self.ENG}
        for o in ops:
            if o["fn"] is not None:
                pos[o["eng"]] += 1
            o["epos"] = pos[o["eng"]]

        def needs(o, od):
            if od["dma"] is not None: return True
            if od["eng"] == o["eng"] and o["fn"] is not None and o["dma"] is None:
                if o["eng"] == "pe": return False
                return o["epos"] - od["epos"] < int(os.environ.get("NEEDS_DIST", "4"))
            return True
        self.needs = needs
        for o in ops:
            for d in o["deps"]:
                od = ops[d]
                if od["dma"] is None and needs(o, od):
                    od["sig"] = True
        cnt = {e: 0 for e in self.ENG}
        for o in ops:
            if o["dma"] is None and o["sig"] and o["fn"] is not None:
                cnt[o["eng"]] += 1; o["sidx"] = cnt[o["eng"]]
        self.stats = dict(nops=len(ops), sig=cnt, dma=dict(self.dma_cnt))
        with contextlib.ExitStack() as st:
            esem = {e: st.enter_context(nc.semaphore("s_" + e)) for e in self.ENG}
            dsem = {k: st.enter_context(nc.semaphore("d_" + str(k))) for k in self.dma_cnt}
            block = st.enter_context(nc.Block())

            def expand(o, acc, seen):
                for d in o["deps"]:
                    if d in seen: continue
                    seen.add(d)
                    od = ops[d]
                    if od["fn"] is None:
                        expand(od, acc, seen)
                    elif od["dma"] is not None:
                        acc.append((("d", od["dma"]), 16 * od["dcount"]))
                    elif self.needs(o, od):
                        acc.append((("e", od["eng"]), od["sidx"]))

            def run(ename, eng):
                waited = {}
                for o in ops:
                    if o["eng"] != ename: continue
                    acc = []
                    expand(o, acc, set())
                    best = {}
                    for k, v in acc:
                        if v is not None and v > best.get(k, 0): best[k] = v
                    for k, v in best.items():
                        if waited.get(k, 0) >= v: continue
                        waited[k] = v
                        eng.wait_ge(esem[k[1]] if k[0] == "e" else dsem[k[1]], v)
                    if o["fn"] is None: continue
                    ins = o["fn"](eng)
                    if o["dma"] is not None:
                        ins.then_inc(dsem[o["dma"]], 16)
                    elif o["sig"]:
                        ins.then_inc(esem[ename], 1)

            @block.tensor
            def _(e): run("pe", e)

            @block.scalar
            def _(e): run("act", e)

            @block.vector
            def _(e): run("dve", e)

            @block.gpsimd
            def _(e): run("pool", e)

            @block.sync
            def _(e): run("sp", e)


class Bld:
    def __init__(self, nc):
        self.P = Prog(nc)

    def mm(self, out, lhsT, rhs, start, stop, r, w):
        self.P.op("pe", lambda e: e.matmul(out, lhsT=lhsT, rhs=rhs, start=start, stop=stop), reads=r, writes=w)

    def tr(self, out, in_, ident, r, w):
        self.P.op("pe", lambda e: e.transpose(out=out, in_=in_, identity=ident), reads=r, writes=w)

    def act(self, out, in_, func, r, w, scale=None, bias=None, accum=None):
        kw = {}
        if scale is not None: kw["scale"] = scale
        if bias is not None: kw["bias"] = bias
        if accum is not None: kw["accum_out"] = accum
        self.P.op("act", lambda e: e.activation(out=out, in_=in_, func=func, **kw), reads=r, writes=w)

    def tt(self, eng, out, in0, in1, op, r, w):
        self.P.op(eng, lambda e: e.tensor_tensor(out=out, in0=in0, in1=in1, op=op), reads=r, writes=w)

    def ts(self, eng, out, in0, s1, s2, op0, op1, r, w):
        if s2 is None:
            self.P.op(eng, lambda e: e.tensor_scalar(out=out, in0=in0, scalar1=s1, scalar2=None, op0=op0), reads=r, writes=w)
        else:
            self.P.op(eng, lambda e: e.tensor_scalar(out=out, in0=in0, scalar1=s1, scalar2=s2, op0=op0, op1=op1), reads=r, writes=w)

    def stt(self, eng, out, in0, scalar, in1, op0, op1, r, w, accum=None):
        kw = {} if accum is None else {"accum_out": accum}
        self.P.op(eng, lambda e: e.scalar_tensor_tensor(out=out, in0=in0, scalar=scalar, in1=in1, op0=op0, op1=op1, **kw), reads=r, writes=w)

    def cp(self, eng, out, in_, r, w):
        self.P.op(eng, lambda e: e.tensor_copy(out=out, in_=in_), reads=r, writes=w)

    def ms(self, eng, out, val, w):
        self.P.op(eng, lambda e: e.memset(out, val), writes=w)

    def dma(self, q, out, in_, r, w, name):
        self.P.op(q, lambda e: e.dma_start(out=out, in_=in_), reads=r, writes=w, dma=name)


def build(stage="full"):
    nc = bass.Bass("TRN2", target_bir_lowering=False)
    dt = lambda name, shape, kind="ExternalInput", dty=F32: nc.dram_tensor(name, shape, dty, kind=kind).ap()
    xT_d = dt("xT", [128, 8, 4096]); xm_d = dt("xm", [NT, D]); win_d = dt("win", [128, 8, 3080])
    wout_d = dt("wout", [128, 8, 1024]); cst_d = dt("consts", [128, C_TOT])
    if stage not in ("A", "A2"):
        wq_d = dt("wq", [128, 8, 2048]); keys_d = dt("keysT", [128, 16, 128])
        ut_d = dt("UT", [128, 128, 8, 128]); v_d = dt("V", [16384, 1024])
    y_d = dt("y", [NT, D], kind="ExternalOutput")
    x1_d = dt("x1s", [NT, D], kind="Internal") if stage not in ("A", "A2") else y_d
    if stage not in ("A", "A2"):
        ub_d = dt("ubs", [128, 128, 1024], kind="Internal", dty=BF16)
        vb_d = dt("vbs", [16384, 1024], kind="Internal", dty=BF16)

    b = Bld(nc); P = b.P
    with contextlib.ExitStack() as st:
        NW = 41400
        cs = st.enter_context(nc.sbuf_tensor("cs", [128, C_TOT], F32))
        xn2T = st.enter_context(nc.sbuf_tensor("xn2T", [128, 8, NT], BF16))
        cb16 = st.enter_context(nc.sbuf_tensor("cb16", [128, 512], BF16))
        ar = st.enter_context(nc.sbuf_tensor("arena", [128, NW], F32))
        psb = [st.enter_context(nc.psum_tensor("psb%d" % i, [128, 512], F32))[:, :] for i in range(8)]
        identb = cb16[:, 0:128]; onesb = cb16[:, 128:256]; trib = cb16[:, 256:384]; iotab = cb16[:, 384:512]
        ident = cs[:, C_CST:C_CST + 128]; tri = cs[:, C_CST + 128:C_CST + 256]
        onesf = cs[:, C_CST + 256:C_CST + 384]; iota = cs[:, C_CST + 384:C_CST + 512]
        col = lambda c, n=1: cs[:, c:c + n]
        eps_c = col(C_EPS); one_c = col(C_ONE); lns_c = col(C_LNS); flag_c = col(C_FLAG)

        off = [0]
        def carve(words, dty=F32, shape=None):
            a = ar[:, off[0]:off[0] + words]; off[0] += words
            assert off[0] <= NW, off[0]
            if dty != F32: a = a.bitcast(dty)
            if shape is not None:
                names = " ".join("d%d" % i for i in range(len(shape)))
                a = a.rearrange("p (%s) -> p %s" % (names, names), **{"d%d" % i: s for i, s in enumerate(shape)})
            return a

        b.dma("sp", cs[:, :], cst_d[:, :], [], ["cs"], "cs")
        b.cp("dve", identb, ident, ["cs"], ["cb16"])
        b.cp("dve", onesb, onesf, ["cs"], ["cb16"])
        b.cp("dve", trib, tri, ["cs"], ["cb16"])
        b.cp("dve", iotab, iota, ["cs"], ["cb16"])

        Winb = carve(12320, BF16, [8, 3080]); Woutb = carve(4096, BF16, [8, 1024])
        xTs = carve(4096, F32, [8, 512]); raw = carve(4120, F32, [8, 515])
        sqb = carve(512, BF16, [2, 512]); lnv = carve(512); rstdb = carve(512)
        hT = carve(2048, BF16, [8, 512]); accs = carve(1024, F32, [2, 512]); qkT = carve(2048, BF16, [8, 512])
        WsTb = carve(256, BF16, [4, 128])
        sm = carve(128)
        sg = carve(192)
        vext = carve(264, BF16, [4, 132]); sig = carve(256, BF16); u_sb = carve(256, BF16)
        vgs = carve(512); vn32 = carve(512); vnb = carve(256, BF16); ktok = carve(256, BF16)
        PT = carve(256, BF16, [4, 128]); ybf = carve(512, BF16); yT = carve(512, BF16, [8, 128])
        xmt = carve(1024); x1 = carve(1024); xs = carve(512, BF16)
        Gst = carve(528, F32, [4, 132]); Cb = carve(264, BF16, [4, 132])
        offA = off[0]

        gsb = sg[:, 0:32]; e1 = sg[:, 32:48]; nlf = sg[:, 48:64]; t16 = sg[:, 64:80]; EK = sg[:, 80:96]; EB = sg[:, 96:112]
        A_t = [sg[:, 112:128], sg[:, 128:144]]; nhl16 = sg[:, 144:160].bitcast(BF16); nhi = nhl16[:, 0:16]; nlo = nhl16[:, 16:32]
        gsb3 = gsb.rearrange("p (j g) -> p j g", j=4)
        den = sm[:, 36:40]; dab = sm[:, 40:44]
        rr = sm[:, 44:48]; ssq = sm[:, 48:52]; mm4 = sm[:, 52:56]; scl = sm[:, 56:60]; st8 = sm[:, 60:68]
        mean = sm[:, 68:72]; msq = sm[:, 72:76]; var = sm[:, 76:80]; rs4 = sm[:, 80:84]
        ss2 = sm[:, 84:85]; l2 = sm[:, 85:86]; r2 = sm[:, 86:87]
        nhl = sm[:, 88:92].bitcast(BF16)
        psP = [psb[0], psb[1]]; psS = psb[3]; psO = [psb[4], psb[5]]; psU = [psb[6], psb[7]]
        psM = psb[2]
        psT = psM[:, 256:512].bitcast(BF16)
        Prog.BANK.clear()
        Prog.BANK.update({"psP0": "B0", "psP1": "B1", "psMg": "B2", "psMb": "B2", "psMl": "B2", "psT": "B2", "psS": "B3",
                          "psO0": "B4", "psO1": "B5", "psU0": "B6", "psU1": "B7",
                          "pq0": "B0", "pq1": "B1", "psc0": "B2", "psc1": "B3", "psc2": "B4", "psc3": "B5",
                          "pt6a": "B6", "pt6b": "B6", "pt6c": "B6",
                          "psOut0": "B0", "psOut1": "B1", "psOut2": "B2", "psOut3": "B3", "psA0": "B4", "psA1": "B5",
                          "psG0": "B6", "psG1": "B7"})
        pcnt = [0]
        def nextP():
            pcnt[0] += 1
            return psP[pcnt[0] % 2], "psP%d" % (pcnt[0] % 2)

        stg = [xTs.rearrange("p a b -> p (a b)"), raw.rearrange("p a b -> p (a b)")]
        for c in range(8):
            s = stg[c % 2][:, 0:3080]; sk = "stg%d" % (c % 2)
            b.dma("sp", s, win_d[:, c, :], [], [sk], sk)
            eng = "dve"
            b.cp(eng, Winb[:, c, 0:1540], s[:, 0:1540], [sk], ["Winb"])
            b.act(Winb[:, c, 1540:3080], s[:, 1540:3080], AF.Copy, [sk], ["Winb"])
        for hf in range(2):
            s = stg[hf][:, 0:4096]; sk = "stg%d" % hf
            b.dma("sp", s, wout_d[:, 4 * hf:4 * hf + 4, :], [], [sk], sk)
            for c4 in range(4):
                c = 4 * hf + c4
                if hf == 0:
                    b.ts("dve", Woutb[:, c, :], s[:, c4 * 1024:(c4 + 1) * 1024], col(C_NG + c), None, ALU.mult, None, [sk, "cs"], ["Woutb"])
                else:
                    b.cp("dve", Woutb[:, c, :], s[:, c4 * 1024:(c4 + 1) * 1024], [sk], ["Woutb"])
        b.cp("dve", WsTb.rearrange("p a b -> p (a b)"), cs[:, C_WS:C_WS + 512], ["cs"], ["WsTb"])
        for g in range(4):
            b.ms("dve", WsTb[64:128, g, 0:64], 0.0, ["WsTb"])
        b.ms("dve", raw.rearrange("p a b -> p (a b)"), 0.0, ["stg1"] + ["raw%d" % c for c in range(8)])
        b.ms("dve", Gst.rearrange("p a b -> p (a b)"), 0.0, ["G"])
        b.ms("dve", Cb.rearrange("p a b -> p (a b)"), 0.0, ["Cb"])
        b.ms("dve", A_t[1], 1.0, ["A1"])
        b.ms("dve", vext.rearrange("p a b -> p (a b)"), 0.0, ["vext"])

        for blk in range(int(os.environ.get("TRUNC", "8"))):
            main = blk >= 4
            tok0 = blk * 512
            for c in range(8):
                b.dma("sp", xTs[:, c, :], xT_d[:, c, tok0:tok0 + 512], [], ["xTs0", "stg0"] if c == 0 else ["xTs%d" % c], "xTs%d" % c)
            for c in range(8):
                b.act(sqb[:, c % 2, :], xTs[:, c, :], AF.Square, ["xTs%d" % c], ["sqb%d" % (c % 2)])
                b.mm(psS, onesb, sqb[:, c % 2, :], c == 0, c == 7, ["cb16", "sqb%d" % (c % 2)], ["psS"])
            b.act(lnv, psS, AF.Ln, ["psS", "cs"], ["lnv"], scale=1.0 / 1024, bias=eps_c)
            b.act(rstdb, lnv, AF.Exp, ["lnv"], ["rstdb"], scale=-0.5)
            for c in range(8):
                b.stt("dve", hT[:, c, :], xTs[:, c, :], col(C_G1 + c), rstdb, ALU.mult, ALU.mult, ["xTs%d" % c, "cs", "rstdb"], ["hT"])
            par = blk % 2
            for j in range(4):
                for c in range(8):
                    b.mm(psM[:, j * 8:(j + 1) * 8], hT[:, c, j * 128:(j + 1) * 128], Winb[:, c, 2048:2056], c == 0, c == 7, ["hT", "Winb"], ["psMg"])
            b.tt("dve", gsb3, psM[:, 0:32].rearrange("p (j g) -> p j g", j=4), col(C_GB, 8).unsqueeze(1).to_broadcast([128, 4, 8]), ALU.add,
                 ["psMg", "cs"], ["gsb"])
            b.act(e1.rearrange("p (j h) -> p j h", j=4), gsb3[:, :, 4:8], AF.Exp, ["gsb"], ["e1"], scale=-1.0)
            b.act(nlf, e1, AF.Ln, ["e1", "cs"], ["nlf"], scale=1.0, bias=one_c)
            b.cp("dve", nhi, nlf, ["nlf"], ["nhi"])
            b.tt("dve", nlo, nlf, nhi, ALU.subtract, ["nlf", "nhi"], ["nlo"])
            for j in range(4):
                js = slice(j * 4, (j + 1) * 4)
                b.mm(psM[:, 64 + j * 4:68 + j * 4], trib, nhi[:, js], True, False, ["cb16", "nhi"], ["psMb"])
                b.mm(psM[:, 64 + j * 4:68 + j * 4], trib, nlo[:, js], False, True, ["cb16", "nlo"], ["psMb"])
                b.mm(psM[:, 96 + j * 4:100 + j * 4], onesb, nhi[:, js], True, False, ["cb16", "nhi"], ["psMl"])
                b.mm(psM[:, 96 + j * 4:100 + j * 4], onesb, nlo[:, js], False, True, ["cb16", "nlo"], ["psMl"])
            b.tt("dve", t16.rearrange("p (j h) -> p j h", j=4), gsb3[:, :, 0:4], psM[:, 64:80].rearrange("p (j h) -> p j h", j=4), ALU.add,
                 ["gsb", "psMb"], ["t4"])
            b.act(EK, t16, AF.Exp, ["t4", "cs"], ["ek"], scale=1.0, bias=lns_c)
            b.act(EB, psM[:, 64:80], AF.Exp, ["psMb"], ["eb"], scale=-1.0)
            b.act(A_t[par], psM[:, 96:112], AF.Exp, ["psMl"], ["A%d" % par], scale=-1.0)
            if blk == 3:
                b.ts("dve", A_t[1][:, 12:16], A_t[1][:, 12:16], flag_c, None, ALU.mult, None, ["A1", "cs"], ["A1"])
            cc_list = list(range(8)) if blk >= 3 else list(range(4, 8))
            for cc in cc_list:
                ps, pk = nextP()
                for c in range(8):
                    b.mm(ps, Winb[:, c, cc * 128:(cc + 1) * 128], hT[:, c, :], c == 0, c == 7, ["Winb", "hT"], [pk])
                rk = "raw%d" % cc
                if blk == 4:
                    b.ts("dve", raw[:, cc, 0:3], raw[:, cc, 512:515], flag_c, None, ALU.mult, None, [rk, "cs"], [rk])
                elif blk > 0:
                    b.cp("dve", raw[:, cc, 0:3], raw[:, cc, 512:515], [rk], [rk])
                b.act(raw[:, cc, 3:515], ps, AF.Copy, [pk], [rk])
                if cc >= 4 or main:
                    ac = accs[:, cc % 2, :]; ak = "acc%d" % (cc % 2)
                    b.act(ac, raw[:, cc, 3:515], AF.Identity, [rk, "cs"], [ak], scale=col(C_CW + 3 * 8 + cc), bias=col(C_CB + cc))
                    for j in (2, 1, 0):
                        b.stt("dve", ac, raw[:, cc, j:j + 512], col(C_CW + j * 8 + cc), ac, ALU.mult, ALU.add, [rk, "cs", ak], [ak])
                    b.act(qkT[:, cc, :], ac, AF.Silu, [ak], ["qkT%d" % cc])
            for j in range(4):
                tsl = slice(j * 128, (j + 1) * 128)
                cidx = blk * 4 + j
                js = slice(j * 4, (j + 1) * 4)
                ek = EK[:, js]; eb = EB[:, js]
                acur = A_t[par][:, js]; ack = "A%d" % par
                if j > 0:
                    aprev = A_t[par][:, (j - 1) * 4:j * 4]; apk = ack
                else:
                    aprev = A_t[1 - par][:, 12:16]; apk = "A%d" % (1 - par)
                ps, pk = nextP()
                for c in range(8):
                    b.mm(ps, hT[:, c, tsl], Winb[:, c, 1024:1536], c == 0, c == 7, ["hT", "Winb"], [pk])
                for h in range(4):
                    b.act(vext[:, h, 0:128], ps[:, h * 128:(h + 1) * 128], AF.Copy, [pk, "ek"], ["vext"], scale=ek[:, h:h + 1])
                b.cp("dve", vext[:, :, 128], ek, ["ek"], ["vext"])
                for h in range(4):
                    b.tr(psT[:, h * 128:(h + 1) * 128], qkT[:, 4 + h, tsl], identb, ["qkT%d" % (4 + h), "cb16"], ["psT"])
                b.cp("dve", ktok, psT, ["psT"], ["ktok"])
                if main:
                    for h in range(4):
                        b.mm(psS[:, h * 128:(h + 1) * 128], qkT[:, 4 + h, tsl], qkT[:, h, tsl], True, True,
                             ["qkT%d" % (4 + h), "qkT%d" % h], ["psS"])
                    b.tt("dve", PT, psS.rearrange("p (a b) -> p a b", a=4), tri.unsqueeze(1).to_broadcast([128, 4, 128]),
                         ALU.mult, ["psS", "cs"], ["PT"])
                    for h in range(4):
                        o = psO[h // 2][:, (h % 2) * 256:(h % 2) * 256 + 129]; ok = "psO%d" % (h // 2)
                        b.mm(o, PT[:, h, :], vext[:, h, 0:129], True, False, ["PT", "vext"], [ok])
                        b.mm(o, qkT[:, h, tsl], Cb[:, h, 0:129], False, True, ["qkT%d" % h, "Cb"], [ok])
                for h in range(4):
                    u = psU[h // 2][:, (h % 2) * 256:(h % 2) * 256 + 129]; uk = "psU%d" % (h // 2)
                    b.mm(u, ktok[:, h * 128:(h + 1) * 128], vext[:, h, 0:129], True, True, ["ktok", "vext"], [uk])
                for h in range(4):
                    u = psU[h // 2][:, (h % 2) * 256:(h % 2) * 256 + 129]; uk = "psU%d" % (h // 2)
                    b.stt("dve", Gst[:, h, 0:129], Gst[:, h, 0:129], aprev[:, h:h + 1], u, ALU.mult, ALU.add, ["G", apk, uk], ["G"])
                    if cidx >= 15:
                        b.ts("dve", Cb[:, h, 0:129], Gst[:, h, 0:129], acur[:, h:h + 1], None, ALU.mult, None, ["G", ack], ["Cb"])
                if not main:
                    continue
                ps, pk = nextP()
                for c in range(8):
                    b.mm(ps, hT[:, c, tsl], Winb[:, c, 1536:2048], c == 0, c == 7, ["hT", "Winb"], [pk])
                b.act(sig, ps, AF.Sigmoid, [pk], ["sig"])
                ps, pk = nextP()
                for c in range(8):
                    b.mm(ps, hT[:, c, tsl], Winb[:, c, 2056:2568], c == 0, c == 7, ["hT", "Winb"], [pk])
                b.act(u_sb, ps, AF.Gelu_apprx_tanh, [pk], ["u_sb"])
                ps, pk = nextP()
                for c in range(8):
                    b.mm(ps, hT[:, c, tsl], Winb[:, c, 2568:3080], c == 0, c == 7, ["hT", "Winb"], [pk])
                b.ms("dve", st8, 0.0, ["st8"])
                for g in range(4):
                    b.act(vgs[:, g * 128:(g + 1) * 128], ps[:, g * 128:(g + 1) * 128], AF.Gelu_apprx_tanh, [pk, "st8"], ["vgs", "st8"],
                          accum=st8[:, g:g + 1])
                for g in range(4):
                    b.act(vn32[:, g * 128:(g + 1) * 128], vgs[:, g * 128:(g + 1) * 128], AF.Square, ["vgs", "st8"], ["vn32", "st8"],
                          accum=st8[:, 4 + g:5 + g])
                b.ts("dve", mean, st8[:, 0:4], 1.0 / 128, None, ALU.mult, None, ["st8"], ["mean"])
                b.tt("dve", msq, mean, mean, ALU.mult, ["mean"], ["msq"])
                b.stt("dve", var, st8[:, 4:8], 1.0 / 128, msq, ALU.mult, ALU.subtract, ["st8", "msq"], ["var"])
                b.act(var, var, AF.Ln, ["var", "cs"], ["var"], scale=1.0, bias=eps_c)
                b.act(rs4, var, AF.Exp, ["var"], ["rs4"], scale=-0.5)
                for g in range(4):
                    b.ts("dve", vn32[:, g * 128:(g + 1) * 128], vgs[:, g * 128:(g + 1) * 128], mean[:, g:g + 1], rs4[:, g:g + 1],
                         ALU.subtract, ALU.mult, ["vgs", "mean", "rs4"], ["vn32"])
                b.tt("dve", vn32, vn32, cs[:, C_LNG:C_LNG + 512], ALU.mult, ["vn32", "cs"], ["vn32"])
                b.tt("dve", vnb, vn32, cs[:, C_LNB:C_LNB + 512], ALU.add, ["vn32", "cs"], ["vnb"])
                for g in range(4):
                    b.mm(psS[:, g * 128:(g + 1) * 128], WsTb[:, g, :], vnb[:, g * 128:(g + 1) * 128], True, True, ["WsTb", "vnb"], ["psS"])
                for g in range(4):
                    b.stt("dve", ybf[:, 512 + g * 128:512 + (g + 1) * 128], psS[:, g * 128:(g + 1) * 128], col(C_BS + g),
                          u_sb[:, g * 128:(g + 1) * 128], ALU.add, ALU.mult, ["psS", "cs", "u_sb"], ["ybf"])
                for p2 in range(2):
                    dv = psO[p2].rearrange("p (h w) -> p h w", h=2)[:, :, 128]
                    b.tt("dve", den[:, 2 * p2:2 * p2 + 2], dv, eb[:, 2 * p2:2 * p2 + 2], ALU.mult, ["psO%d" % p2, "eb"], ["den"])
                b.act(dab, den, AF.Abs, ["den"], ["dab"])
                b.ts("dve", dab, dab, 1.0, None, ALU.max, None, ["dab"], ["dab"])
                b.P.op("dve", lambda e: e.reciprocal(out=dab, in_=dab), reads=["dab"], writes=["dab"])
                b.tt("dve", rr, eb, dab, ALU.mult, ["eb", "dab"], ["rr"])
                b.ms("dve", ssq, 0.0, ["ssq"])
                for h in range(4):
                    o = psO[h // 2][:, (h % 2) * 256:(h % 2) * 256 + 128]
                    b.act(vn32[:, 0:128], o, AF.Square, ["psO%d" % (h // 2), "ssq"], ["vn32", "ssq"], accum=ssq[:, h:h + 1])
                b.tt("dve", mm4, ssq, rr, ALU.mult, ["ssq", "rr"], ["mm4"])
                b.tt("dve", mm4, mm4, rr, ALU.mult, ["mm4", "rr"], ["mm4"])
                b.act(mm4, mm4, AF.Ln, ["mm4", "cs"], ["mm4"], scale=1.0 / 128, bias=eps_c)
                b.act(mm4, mm4, AF.Exp, ["mm4"], ["mm4"], scale=-0.5)
                b.tt("dve", scl, rr, mm4, ALU.mult, ["rr", "mm4"], ["scl"])
                for h in range(4):
                    o = psO[h // 2][:, (h % 2) * 256:(h % 2) * 256 + 128]
                    b.stt("dve", ybf[:, h * 128:(h + 1) * 128], o, scl[:, h:h + 1], sig[:, h * 128:(h + 1) * 128], ALU.mult, ALU.mult,
                          ["psO%d" % (h // 2), "scl", "sig"], ["ybf"])
                for hf in range(2):
                    for q in range(4):
                        b.tr(psT[:, q * 128:(q + 1) * 128], ybf[:, (hf * 4 + q) * 128:(hf * 4 + q + 1) * 128], identb, ["ybf", "cb16"], ["psT"])
                    b.cp("dve", yT[:, hf * 4:hf * 4 + 4, :].rearrange("p a b -> p (a b)"), psT, ["psT"], ["yT"])
                for hf in range(2):
                    for c in range(8):
                        b.mm(psO[hf], yT[:, c, :], Woutb[:, c, hf * 512:(hf + 1) * 512], c == 0, c == 7, ["yT", "Woutb"], ["psO%d" % hf])
                mt0 = (blk - 4) * 512 + j * 128
                b.dma("sp", xmt, xm_d[mt0:mt0 + 128, :], [], ["xmt"], "xmt")
                for hf in range(2):
                    b.tt("dve", x1[:, hf * 512:(hf + 1) * 512], psO[hf], xmt[:, hf * 512:(hf + 1) * 512], ALU.add, ["psO%d" % hf, "xmt"], ["x1"])
                b.dma("sp", x1_d[mt0:mt0 + 128, :], x1, ["x1"], ["x1d%d" % (mt0 // 128)], "x1st")
                if stage == "A":
                    continue
                b.ms("dve", ss2, 0.0, ["ss2"])
                b.act(xs, x1, AF.Square, ["x1", "ss2"], ["xs", "ss2"], accum=ss2)
                b.act(l2, ss2, AF.Ln, ["ss2", "cs"], ["l2"], scale=1.0 / 1024, bias=eps_c)
                b.act(r2, l2, AF.Exp, ["l2"], ["r2"], scale=-0.5)
                b.act(xs, x1, AF.Copy, ["x1", "r2"], ["xs"], scale=r2)
                for hf in range(2):
                    for q in range(4):
                        b.tr(psT[:, q * 128:(q + 1) * 128], xs[:, (hf * 4 + q) * 128:(hf * 4 + q + 1) * 128], identb, ["xs", "cb16"], ["psT"])
                    for q in range(4):
                        b.ts("dve", xn2T[:, hf * 4 + q, mt0:mt0 + 128], psT[:, q * 128:(q + 1) * 128], col(C_G2 + hf * 4 + q), None,
                             ALU.mult, None, ["psT", "cs"], ["xn2T"])
        fkeys = ["x1d%d" % i for i in range(16)]
        if stage not in ("A", "A2"):
            fkeys = build_peer(nc, b, carve, off, cs, xn2T, identb, ident, iotab, psb, wq_d, keys_d, ut_d, v_d, x1_d, y_d, stage, ub_d, vb_d)
        P.emit(final_keys=fkeys)
    return nc


import os


def build_peer(nc, b, carve, off, cs, xn2T, identb, ident, iota, psb, wq_d, keys_d, ut_d, v_d, x1_d, y_d, stage='full', ub_d=None, vb_d=None):
    P = b.P
    P.fence()
    off[0] = 0
    col = lambda c, n=1: cs[:, c:c + n]
    eps_c = col(C_EPS)
    I1T = carve(2048); I2T = carve(2048); GTs = carve(2048)
    offBC = off[0]
    Wqb = carve(8192, BF16, [8, 2048]); keysTb = carve(1024, BF16, [16, 128]); stg = carve(2048)
    qTb = carve(1024, BF16, [16, 128]); sc2 = carve(4096, F32, [2, 16, 128]); wk = carve(2048, F32, [16, 128])
    m16 = carve(256, F32, [16, 16]); i16 = carve(256, U32, [16, 16]); i16f = carve(256, F32, [16, 16])
    NC = 80; RECTS = [(0, 2, 16, 0), (2, 2, 8, 32), (4, 4, 4, 48), (8, 8, 2, 64)]
    cand = carve(8 * NC, F32, [8, NC]); cidx = carve(8 * NC, F32, [8, NC]); junk = carve(4 * NC, F32, [4, NC])
    ts16 = carve(128, F32, [8, 16]); eidx = carve(128); ex = carve(128, F32, [8, 16]); gate = carve(128, F32, [8, 16])
    ei = carve(128, I32); i1i = carve(128, I32); i2i = carve(128, I32); i1f = carve(128); i2f = carve(128)
    tb3 = carve(192, BF16)
    smb = carve(32); negm = smb[:, 0:8]; Z = smb[:, 8:16]; rz = smb[:, 16:24]
    cin = carve(6144, F32, [3, 2, 1024]); cout = carve(3072, BF16, [3, 2, 1024])
    jobs = {"in": 0, "cv": 0}

    def conv_in():
        j = jobs["in"]
        if j >= 128: return
        jobs["in"] += 1; s3 = j % 3
        b.dma("sp", cin[:, s3, 0, :], ut_d[j].rearrange("p c e -> p (c e)"), [], ["cinu%d" % s3], "cinu%d" % s3)
        b.dma("sp", cin[:, s3, 1, :], v_d[j * 128:(j + 1) * 128, :], [], ["cinv%d" % s3], "cinv%d" % s3)

    def conv_cv():
        j = jobs["cv"]
        if j >= 128: return
        jobs["cv"] += 1; s3 = j % 3
        b.act(cout[:, s3, 0, :], cin[:, s3, 0, :], AF.Copy, ["cinu%d" % s3], ["coutu%d" % s3])
        b.act(cout[:, s3, 1, :], cin[:, s3, 1, :], AF.Copy, ["cinv%d" % s3], ["coutv%d" % s3])
        b.dma("sp", ub_d[j], cout[:, s3, 0, :], ["coutu%d" % s3], ["ubd"], "cou%d" % s3)
        b.dma("sp", vb_d[j * 128:(j + 1) * 128, :], cout[:, s3, 1, :], ["coutv%d" % s3], ["vbd"], "cov%d" % s3)

    conv_in(); conv_in()
    for c in range(8):
        b.dma("sp", stg, wq_d[:, c, :], [], ["stgB"], "stgB")
        b.cp("dve", Wqb[:, c, 0:1024], stg[:, 0:1024], ["stgB"], ["Wqb"])
        b.act(Wqb[:, c, 1024:2048], stg[:, 1024:2048], AF.Copy, ["stgB"], ["Wqb"])
    b.dma("sp", stg, keys_d[:, :, :].rearrange("p a b -> p (a b)"), [], ["stgB"], "stgB")
    b.cp("dve", keysTb.rearrange("p a b -> p (a b)"), stg, ["stgB"], ["keysTb"])
    PB = int(os.environ.get("PB_STOP", "99"))
    if PB <= 0: return ["x1d%d" % q for q in range(16)]
    def part1(i):
        t0 = i * 128; par = i % 2; sc = sc2[:, par]
        for hp in range(16):
            ps = psb[hp % 2]; pk = "pq%d" % (hp % 2)
            for c in range(8):
                b.mm(ps[:, 0:128], Wqb[:, c, hp * 128:(hp + 1) * 128], xn2T[:, c, t0:t0 + 128], c == 0, c == 7, ["Wqb", "xn2T"], [pk])
            b.act(qTb[:, hp, :], ps[:, 0:128], AF.Copy, [pk], ["qTb%d" % hp])
            if hp % 2 == 0:
                conv_in(); conv_cv()
        for hp in range(16):
            b.mm(psb[2 + hp // 4][:, (hp % 4) * 128:(hp % 4 + 1) * 128], qTb[:, hp, :], keysTb[:, hp, :], True, True,
                 ["qTb%d" % hp, "keysTb"], ["psc%d" % (hp // 4)])
        for q4 in range(4):
            b.act(sc[:, q4 * 4:q4 * 4 + 4, :].rearrange("p a b -> p (a b)"), psb[2 + q4], AF.Copy, ["psc%d" % q4], ["sc%d_%d" % (q4, par)])

    def part2(i):
        t0 = i * 128; par = i % 2; sc = sc2[:, par]
        for hp in range(16):
            b.P.op("dve", (lambda hp: lambda e: e.max(out=m16[:, hp, 0:8], in_=sc[:, hp, :]))(hp), reads=["sc%d_%d" % (hp // 4, par)], writes=["m16_%d" % hp])
        for hp in range(16):
            b.P.op("dve", (lambda hp: lambda e: e.max_index(out=i16[:, hp, 0:8], in_max=m16[:, hp, 0:8], in_values=sc[:, hp, :]))(hp),
                   reads=["sc%d_%d" % (hp // 4, par), "m16_%d" % hp], writes=["i16_%d" % hp])
        for hp in range(16):
            b.P.op("dve", (lambda hp: lambda e: e.match_replace(out=wk[:, hp, :], in_to_replace=m16[:, hp, 0:8], in_values=sc[:, hp, :], imm_value=-1e30))(hp),
                   reads=["sc%d_%d" % (hp // 4, par), "m16_%d" % hp], writes=["wk%d" % hp])
        for hp in range(16):
            b.P.op("dve", (lambda hp: lambda e: e.max(out=m16[:, hp, 8:16], in_=wk[:, hp, :]))(hp), reads=["wk%d" % hp], writes=["m16_%d" % hp])
        for hp in range(16):
            b.P.op("dve", (lambda hp: lambda e: e.max_index(out=i16[:, hp, 8:16], in_max=m16[:, hp, 8:16], in_values=wk[:, hp, :]))(hp),
                   reads=["wk%d" % hp, "m16_%d" % hp], writes=["i16_%d" % hp])
        allm = ["m16_%d" % hp for hp in range(16)]; alli = ["i16_%d" % hp for hp in range(16)]
        b.cp("dve", i16f.rearrange("p a b -> p (a b)"), i16.rearrange("p a b -> p (a b)"), alli, ["i16f"])
        m16h = m16.rearrange("p (h two) a -> p h two a", two=2)
        i16h = i16f.rearrange("p (h two) a -> p h two a", two=2)
        allc = ["cand%d" % h for h in range(8)]; allx = ["cidx%d" % h for h in range(8)]
        b.ts("dve", i16h[:, :, 0, :], i16h[:, :, 0, :], 128.0, None, ALU.mult, None, ["i16f"], ["i16f"])
        for (a0, na, nb, o0) in RECTS:
            shp = [128, 8, na, nb]
            b.tt("dve", cand[:, :, o0:o0 + na * nb].rearrange("p h (a c) -> p h a c", a=na),
                 m16h[:, :, 0, a0:a0 + na].unsqueeze(3).to_broadcast(shp), m16h[:, :, 1, 0:nb].unsqueeze(2).to_broadcast(shp),
                 ALU.add, allm, allc)
            b.tt("dve", cidx[:, :, o0:o0 + na * nb].rearrange("p h (a c) -> p h a c", a=na),
                 i16h[:, :, 0, a0:a0 + na].unsqueeze(3).to_broadcast(shp), i16h[:, :, 1, 0:nb].unsqueeze(2).to_broadcast(shp),
                 ALU.add, ["i16f"], allx)
        wk2 = wk.rearrange("p a b -> p (a b)").rearrange("p (a b) -> p a b", a=8)[:, :, 0:NC]
        for h in range(8):
            b.P.op("dve", (lambda h: lambda e: e.max(out=ts16[:, h, 0:8], in_=cand[:, h, :]))(h), reads=["cand%d" % h], writes=["ts16_%d" % h])
        for h in range(8):
            b.P.op("dve", (lambda h: lambda e: e.match_replace(out=wk2[:, h, :], in_to_replace=ts16[:, h, 0:8], in_values=cand[:, h, :], imm_value=-1e30))(h),
                   reads=["cand%d" % h, "ts16_%d" % h] + ["wk%d" % (2 * h), "wk%d" % (2 * h + 1)], writes=["wk%d" % (2 * h), "wk%d" % (2 * h + 1)])
        for h in range(8):
            b.P.op("dve", (lambda h: lambda e: e.max(out=ts16[:, h, 8:16], in_=wk2[:, h, :]))(h), reads=["wk%d" % (2 * h), "wk%d" % (2 * h + 1)], writes=["ts16_%d" % h])
        b.ms("dve", eidx, 0.0, ["eidx"])
        if os.environ.get("TRIV"):
            for q in range(int(os.environ["TRIV"])):
                b.ms("dve", junk[:, q % 4, :], 0.0, ["junk%d" % (q % 4)])
            return ["x1d%d" % q for q in range(16)]
        for j in range(16):
            for h in range(8):
                jk = "junk%d" % ((j * 8 + h) % 4)
                b.stt("dve", junk[:, (j * 8 + h) % 4, :], cand[:, h, :], ts16[:, h, j:j + 1], cidx[:, h, :], ALU.is_equal, ALU.mult,
                      ["cand%d" % h, "ts16_%d" % h, "cidx%d" % h, "eidx"], [jk, "eidx%d" % (h * 16 + j)], accum=(None if os.environ.get("NOACC") else eidx[:, h * 16 + j:h * 16 + j + 1]))
        alle = ["eidx"] + ["eidx%d" % q for q in range(128)]
        allts = ["ts16_%d" % h for h in range(8)]
        b.ts("dve", negm, ts16[:, :, 0], -1.0, None, ALU.mult, None, allts, ["negm"])
        b.ms("dve", Z, 0.0, ["Z"])
        for h in range(8):
            b.act(ex[:, h, :], ts16[:, h, :], AF.Exp, allts + ["negm", "Z"], ["ex", "Z"], scale=1.0, bias=negm[:, h:h + 1], accum=Z[:, h:h + 1])
        b.P.op("dve", lambda e: e.reciprocal(out=rz, in_=Z), reads=["Z"], writes=["rz"])
        b.tt("dve", gate, ex, rz.unsqueeze(2).to_broadcast([128, 8, 16]), ALU.mult, ["ex", "rz"], ["gate"])
        b.cp("dve", ei, eidx, alle, ["ei"])
        b.ts("dve", i1i, ei, 7, None, ALU.arith_shift_right, None, ["ei"], ["i1i"])
        b.ts("dve", i2i, ei, 127, None, ALU.bitwise_and, None, ["ei"], ["i2i"])
        b.cp("dve", tb3[:, 0:128], i1i, ["i1i"], ["tb3a"])
        b.cp("dve", tb3[:, 128:256], i2i, ["i2i"], ["tb3b"])
        b.cp("dve", tb3[:, 256:384], gate.rearrange("p a b -> p (a b)"), ["gate"], ["tb3c"])
        pT6 = psb[6].bitcast(BF16)
        b.tr(pT6[:, 0:128], tb3[:, 0:128], identb, ["tb3a"], ["pt6a"])
        b.tr(pT6[:, 128:256], tb3[:, 128:256], identb, ["tb3b"], ["pt6b"])
        b.tr(pT6[:, 256:384], tb3[:, 256:384], identb, ["tb3c"], ["pt6c"])
        b.act(I1T[:, t0:t0 + 128], pT6[:, 0:128], AF.Copy, ["pt6a"], ["I1T"])
        b.act(I2T[:, t0:t0 + 128], pT6[:, 128:256], AF.Copy, ["pt6b"], ["I2T"])
        b.act(GTs[:, t0:t0 + 128], pT6[:, 256:384], AF.Copy, ["pt6c"], ["GTs"])
    part1(0)
    for i in range(16):
        if i + 1 < 16: part1(i + 1)
        part2(i)
    while jobs["cv"] < 128:
        conv_in(); conv_cv()
    if stage == "AB":
        return ["x1d%d" % i for i in range(16)]
    P.fence()
    off[0] = offBC
    GT = carve(16384, BF16, [128, 256]); ohA = carve(256, BF16, [8, 64]); ohB = carve(512, BF16, [8, 128])
    NSL = 8
    Ub = carve(NSL * 512, BF16, [NSL, 8, 128]); Vb = carve(NSL * 512, BF16, [NSL, 1024])
    Ab = carve(256, BF16, [2, 256]); AG = carve(256, BF16, [2, 256])
    x1t = [carve(1024), carve(1024)]; x2 = [carve(1024), carve(1024)]; ot = [carve(1024), carve(1024)]; smc = carve(16)
    ssf = [smc[:, 0:1], smc[:, 4:5]]; lf_ = [smc[:, 1:2], smc[:, 5:6]]; rf = [smc[:, 2:3], smc[:, 6:7]]
    psOut = psb[0:4]; psA = psb[4:6]; psG = psb[6:8]
    fkeys = []
    GL = [(0, 0, g) for g in range(64)]
    for T in range(8):
        GL += [(T, 1, g) for g in range(64)]
        if T + 1 < 8: GL += [(T + 1, 0, g) for g in range(64)]
    gn = [0]

    def gstep():
        n = gn[0]; gn[0] += 1
        if n + 1 < len(GL):
            T, half, grp = GL[n + 1]; p = (n + 1) % 2; h0 = 64 * half
            for s4 in range(4):
                t = T * 256 + grp * 4 + s4; sl = p * 4 + s4
                b.ts("dve", ohA[:, sl, :], iota[:, h0:h0 + 64], I1T[:, t:t + 1], GTs[:, t:t + 1], ALU.is_equal, ALU.mult,
                     ["cb16", "I1T", "GTs"], ["ohA%d" % sl])
                b.ts("dve", ohB[:, sl, :], iota, I2T[:, t:t + 1], None, ALU.is_equal, None, ["cb16", "I2T"], ["ohB%d" % sl])
        if n < len(GL):
            p = n % 2
            for s4 in range(4):
                sl = p * 4 + s4
                b.mm(psG[p][:, s4 * 64:(s4 + 1) * 64], ohB[:, sl, :], ohA[:, sl, :], True, True, ["ohA%d" % sl, "ohB%d" % sl], ["psG%d" % p])
        if 1 <= n <= len(GL):
            T, half, grp = GL[n - 1]; p = (n - 1) % 2; h0 = 64 * half
            b.act(GT[:, h0:h0 + 64, grp * 4:grp * 4 + 4], psG[p][:, 0:256].rearrange("p (t i) -> p i t", t=4), AF.Copy,
                  ["psG%d" % p], ["GT%d" % half])

    T, half, grp = GL[0]
    for s4 in range(4):
        t = grp * 4 + s4
        b.ts("dve", ohA[:, s4, :], iota[:, 0:64], I1T[:, t:t + 1], GTs[:, t:t + 1], ALU.is_equal, ALU.mult, ["cb16", "I1T", "GTs"], ["ohA%d" % s4])
        b.ts("dve", ohB[:, s4, :], iota, I2T[:, t:t + 1], None, ALU.is_equal, None, ["cb16", "I2T"], ["ohB%d" % s4])
    for _ in range(64):
        gstep()

    def epi_finish(T):
        for sub in range(2):
            r0 = T * 256 + sub * 128
            b.stt("dve", ot[sub], x2[sub], rf[sub], cs[:, C_FGB:C_FGB + 1024], ALU.mult, ALU.mult,
                  ["x2%d" % sub, "rf%d" % sub, "cs", "ot%d" % sub], ["ot%d" % sub])
            k = "yd%d" % (r0 // 128)
            b.dma("sp", y_d[r0:r0 + 128, :], ot[sub], ["ot%d" % sub], [k], "yst")
            fkeys.append(k)

    for T in range(8):
        tb = T * 256

        def stage1(blk):
            s4 = blk % NSL; s2 = blk % 2
            b.dma("sp", Ub[:, s4, :, :].rearrange("p a b -> p (a b)"), ub_d[blk], ["ubd"], ["Ub%d" % s4], "Ub%d" % s4)
            b.dma("sp", Vb[:, s4, :], vb_d[blk * 128:(blk + 1) * 128, :], ["vbd"], ["Vb%d" % s4], "Vb%d" % s4)
            for c in range(8):
                b.mm(psA[s2][:, 0:256], Ub[:, s4, c, :], xn2T[:, c, tb:tb + 256], c == 0, c == 7, ["Ub%d" % s4, "xn2T"], ["psA%d" % s2])

        def stage2(blk):
            s2 = blk % 2; s4 = blk % NSL
            b.act(Ab[:, s2, :], psA[s2][:, 0:256], AF.Gelu_apprx_tanh, ["psA%d" % s2], ["Ab%d" % s2])
            b.tt("dve", AG[:, s2, :], Ab[:, s2, :], GT[:, blk, :], ALU.mult, ["Ab%d" % s2, "GT%d" % (blk // 64)], ["AG%d" % s2])
            for sub in range(2):
                for hf in range(2):
                    b.mm(psOut[sub * 2 + hf], AG[:, s2, sub * 128:(sub + 1) * 128], Vb[:, s4, hf * 512:(hf + 1) * 512], blk == 0, blk == 127,
                         ["AG%d" % s2, "Vb%d" % s4], ["psOut%d" % (sub * 2 + hf)])

        for blk in range(129):
            if blk < 128: stage1(blk)
            if blk >= 1: stage2(blk - 1)
            if blk < 128:
                gstep()
            if blk == 6 and T > 0:
                epi_finish(T - 1)
            if blk == 96:
                for sub in range(2):
                    r0 = tb + sub * 128
                    b.dma("sp", x1t[sub], x1_d[r0:r0 + 128, :], ["x1d%d" % (r0 // 128)], ["x1t%d" % sub], "x1t%d" % sub)
        for sub in range(2):
            for hf in range(2):
                b.tt("dve", x2[sub][:, hf * 512:(hf + 1) * 512], psOut[sub * 2 + hf], x1t[sub][:, hf * 512:(hf + 1) * 512], ALU.add,
                     ["psOut%d" % (sub * 2 + hf), "x1t%d" % sub], ["x2%d" % sub])
        for sub in range(2):
            b.ms("dve", ssf[sub], 0.0, ["ssf%d" % sub])
            b.act(ot[sub], x2[sub], AF.Square, ["x2%d" % sub, "ssf%d" % sub], ["ot%d" % sub, "ssf%d" % sub], accum=ssf[sub])
            b.act(lf_[sub], ssf[sub], AF.Ln, ["ssf%d" % sub, "cs"], ["lf_%d" % sub], scale=1.0 / 1024, bias=eps_c)
            b.act(rf[sub], lf_[sub], AF.Exp, ["lf_%d" % sub], ["rf%d" % sub], scale=-0.5)
    epi_finish(7)
    return fkeys


def host_inputs(inp):
    f = lambda a: np.ascontiguousarray(a, dtype=np.float32)
    x = inp["x"]
    consts = np.zeros((128, C_TOT), np.float32)
    cw = inp["conv_w"][0]
    consts[:, C_CW:C_CW + 32] = cw.reshape(4, 8, 128).transpose(2, 0, 1).reshape(128, 32)
    consts[:, C_CB:C_CB + 8] = inp["conv_b"][0].reshape(8, 128).T
    consts[:, C_G1:C_G1 + 8] = inp["norm1_g"][0].reshape(8, 128).T
    consts[:, C_G2:C_G2 + 8] = inp["norm2_g"][0].reshape(8, 128).T
    consts[:, C_NG:C_NG + 4] = inp["mlstm_norm_g"][0].reshape(4, 128).T
    consts[:, C_GB:C_GB + 4] = inp["b_igate"][0][None, :]
    consts[:, C_GB + 4:C_GB + 8] = inp["b_fgate"][0][None, :]
    consts[:, C_BS:C_BS + 4] = inp["gmlp_b_s"][0].T
    consts[:, C_EPS] = EPS; consts[:, C_ONE] = 1.0; consts[:, C_LNS] = np.float32(np.log(128.0 ** -0.5))
    consts[:, C_LNG:C_LNG + 512] = inp["gmlp_ln_g"][0][None, :]
    consts[:, C_LNB:C_LNB + 512] = inp["gmlp_ln_b"][0][None, :]
    consts[:, C_FGB:C_FGB + 1024] = inp["final_g"][None, :]
    consts[:, C_CST:C_CST + 128] = np.eye(128)
    consts[:, C_CST + 128:C_CST + 256] = np.triu(np.ones((128, 128)))
    consts[:, C_CST + 256:C_CST + 384] = 1.0
    consts[:, C_CST + 384:C_CST + 512] = np.arange(128)[None, :]
    consts[:, C_WS:C_WS + 512] = inp["gmlp_w_s"][0].transpose(2, 0, 1).reshape(128, 512)
    shared = {
        "win": f(inp["w_in"][0].reshape(8, 128, 3080).transpose(1, 0, 2)),
        "wout": f(inp["w_out"][0].reshape(8, 128, 1024).transpose(1, 0, 2)),
        "wq": f(inp["peer_w_query"][0].reshape(8, 128, 2048).transpose(1, 0, 2)),
        "keysT": f(inp["peer_sub_keys"][0].reshape(16, 128, 128).transpose(2, 0, 1)),
        "UT": f(inp["peer_u"][0].reshape(128, 128, 8, 128).transpose(0, 3, 2, 1)),
        "V": f(inp["peer_v"][0]),
    }
    maps = []
    for core in range(NCORES):
        bi, s = core // 2, core % 2
        xm = x[bi, s * NT:(s + 1) * NT]
        pre = x[bi, 0:NT]
        toks = np.concatenate([pre, xm], axis=0)
        xT = f(toks.T.reshape(8, 128, 4096).transpose(1, 0, 2))
        c = consts.copy(); c[:, C_FLAG] = float(s)
        m = dict(shared); m.update({"xT": xT, "xm": f(xm), "consts": c})
        maps.append(m)
    return maps


_NC_CACHE = {}


def kernel(**inputs):
    stage = "full"
    if stage not in _NC_CACHE:
        _NC_CACHE[stage] = build(stage)
    nc = _NC_CACHE[stage]
    maps = host_inputs({k: np.asarray(v) for k, v in inputs.items()})
    res = run_bass_kernel_spmd(nc, maps, core_ids=list(range(NCORES)))
    out = np.empty((4, 4096, 1024), np.float32)
    for core in range(NCORES):
        bi, s = core // 2, core % 2
        out[bi, s * NT:(s + 1) * NT] = res.results[core]["y"]
    return out
```

```python
import contextlib
import os
import numpy as np
import concourse.bass as bass
import concourse.mybir as mybir
from concourse.bass_utils import run_bass_kernel_spmd

F32 = mybir.dt.float32; BF16 = mybir.dt.bfloat16; U32 = mybir.dt.uint32; I32 = mybir.dt.int32
AF = mybir.ActivationFunctionType; ALU = mybir.AluOpType

NCORES = 8
NT = 2048
D = 1024
EPS = 1e-6
C_CW = 0; C_CB = 32; C_G1 = 40; C_G2 = 48; C_NG = 56; C_GB = 60; C_BS = 68; C_FLAG = 72
C_EPS = 73; C_ONE = 74; C_LNS = 75; C_SMALL = 80
C_LNG = 80; C_LNB = C_LNG + 512; C_FGB = C_LNB + 512; C_CST = C_FGB + 1024; C_WS = C_CST + 512
C_TOT = C_WS + 512


class Prog:
    ENG = ("pe", "act", "dve", "pool", "sp")

    def __init__(self, nc):
        self.nc = nc; self.ops = []; self.lastw = {}; self.readers = {}; self.dma_cnt = {}

    BANK = {}

    def op(self, eng, fn, reads=(), writes=(), dma=None):
        i = len(self.ops)
        reads = list(reads) + [self.BANK[k] for k in reads if k in self.BANK]
        writes = list(writes) + [self.BANK[k] for k in writes if k in self.BANK]
        deps = set()
        for k in list(reads) + list(writes):
            if k in self.lastw: deps.add(self.lastw[k])
        for k in writes:
            lastr = {}
            for r in self.readers.get(k, ()):
                ro = self.ops[r]
                if ro["dma"] is not None: deps.add(r)
                else: lastr[ro["eng"]] = r
            deps.update(lastr.values())
        o = dict(eng=eng, fn=fn, deps=deps, dma=dma, sig=False, dcount=None, sidx=None)
        if dma is not None:
            self.dma_cnt[dma] = self.dma_cnt.get(dma, 0) + 1
            o["dcount"] = self.dma_cnt[dma]
        self.ops.append(o)
        for k in writes:
            self.lastw[k] = i; self.readers[k] = []
        for k in reads:
            self.readers.setdefault(k, []).append(i)
        return i

    def fence(self):
        last = {}
        for i, o in enumerate(self.ops):
            if o["fn"] is None: continue
            if o["dma"] is not None: last[("d", o["dma"])] = i
            else: last[("e", o["eng"])] = i
        for e in self.ENG:
            i = self.op(e, None)
            self.ops[i]["deps"] = set(last.values())

    def emit(self, final_keys=()):
        nc = self.nc; ops = self.ops
        if not os.environ.get("NOFENCE"):
            self.fence()
        self.op("sp", None, reads=list(final_keys), writes=["__final"])
        pos = {e: 0 for e in self.ENG}
        for o in ops:
            if o["fn"] is not None:
                pos[o["eng"]] += 1
            o["epos"] = pos[o["eng"]]

        def needs(o, od):
            if od["dma"] is not None: return True
            if od["eng"] == o["eng"] and o["fn"] is not None and o["dma"] is None:
                if o["eng"] == "pe": return False
                return o["epos"] - od["epos"] < int(os.environ.get("NEEDS_DIST", "4"))
            return True
        self.needs = needs
        for o in ops:
            for d in o["deps"]:
                od = ops[d]
                if od["dma"] is None and needs(o, od):
                    od["sig"] = True
        cnt = {e: 0 for e in self.ENG}
        for o in ops:
            if o["dma"] is None and o["sig"] and o["fn"] is not None:
                cnt[o["eng"]] += 1; o["sidx"] = cnt[o["eng"]]
        self.stats = dict(nops=len(ops), sig=cnt, dma=dict(self.dma_cnt))
        with contextlib.ExitStack() as st:
            esem = {e: st.enter_context(nc.semaphore("s_" + e)) for e in self.ENG}
            dsem = {k: st.enter_context(nc.semaphore("d_" + str(k))) for k in self.dma_cnt}
            block = st.enter_context(nc.Block())

            def expand(o, acc, seen):
                for d in o["deps"]:
                    if d in seen: continue
                    seen.add(d)
                    od = ops[d]
                    if od["fn"] is None:
                        expand(od, acc, seen)
                    elif od["dma"] is not None:
                        acc.append((("d", od["dma"]), 16 * od["dcount"]))
                    elif self.needs(o, od):
                        acc.append((("e", od["eng"]), od["sidx"]))

            def run(ename, eng):
                waited = {}
                for o in ops:
                    if o["eng"] != ename: continue
                    acc = []
                    expand(o, acc, set())
                    best = {}
                    for k, v in acc:
                        if v is not None and v > best.get(k, 0): best[k] = v
                    for k, v in best.items():
                        if waited.get(k, 0) >= v: continue
                        waited[k] = v
                        eng.wait_ge(esem[k[1]] if k[0] == "e" else dsem[k[1]], v)
                    if o["fn"] is None: continue
                    ins = o["fn"](eng)
                    if o["dma"] is not None:
                        ins.then_inc(dsem[o["dma"]], 16)
                    elif o["sig"]:
                        ins.then_inc(esem[ename], 1)

            @block.tensor
            def _(e): run("pe", e)

            @block.scalar
            def _(e): run("act", e)

            @block.vector
            def _(e): run("dve", e)

            @block.gpsimd
            def _(e): run("pool", e)

            @block.sync
            def _(e): run("sp", e)


class Bld:
    def __init__(self, nc):
        self.P = Prog(nc)

    def mm(self, out, lhsT, rhs, start, stop, r, w):
        self.P.op("pe", lambda e: e.matmul(out, lhsT=lhsT, rhs=rhs, start=start, stop=stop), reads=r, writes=w)

    def tr(self, out, in_, ident, r, w):
        self.P.op("pe", lambda e: e.transpose(out=out, in_=in_, identity=ident), reads=r, writes=w)

    def act(self, out, in_, func, r, w, scale=None, bias=None, accum=None):
        kw = {}
        if scale is not None: kw["scale"] = scale
        if bias is not None: kw["bias"] = bias
        if accum is not None: kw["accum_out"] = accum
        self.P.op("act", lambda e: e.activation(out=out, in_=in_, func=func, **kw), reads=r, writes=w)

    def tt(self, eng, out, in0, in1, op, r, w):
        self.P.op(eng, lambda e: e.tensor_tensor(out=out, in0=in0, in1=in1, op=op), reads=r, writes=w)

    def ts(self, eng, out, in0, s1, s2, op0, op1, r, w):
        if s2 is None:
            self.P.op(eng, lambda e: e.tensor_scalar(out=out, in0=in0, scalar1=s1, scalar2=None, op0=op0), reads=r, writes=w)
        else:
            self.P.op(eng, lambda e: e.tensor_scalar(out=out, in0=in0, scalar1=s1, scalar2=s2, op0=op0, op1=op1), reads=r, writes=w)

    def stt(self, eng, out, in0, scalar, in1, op0, op1, r, w, accum=None):
        kw = {} if accum is None else {"accum_out": accum}
        self.P.op(eng, lambda e: e.scalar_tensor_tensor(out=out, in0=in0, scalar=scalar, in1=in1, op0=op0, op1=op1, **kw), reads=r, writes=w)

    def cp(self, eng, out, in_, r, w):
        self.P.op(eng, lambda e: e.tensor_copy(out=out, in_=in_), reads=r, writes=w)

    def ms(self, eng, out, val, w):
        self.P.op(eng, lambda e: e.memset(out, val), writes=w)

    def dma(self, q, out, in_, r, w, name):
        self.P.op(q, lambda e: e.dma_start(out=out, in_=in_), reads=r, writes=w, dma=name)


def build(stage="full"):
    nc = bass.Bass("TRN2", target_bir_lowering=False)
    dt = lambda name, shape, kind="ExternalInput", dty=F32: nc.dram_tensor(name, shape, dty, kind=kind).ap()
    xT_d = dt("xT", [128, 8, 4096]); xm_d = dt("xm", [NT, D]); win_d = dt("win", [128, 8, 3080])
    wout_d = dt("wout", [128, 8, 1024]); cst_d = dt("consts", [128, C_TOT])
    if stage not in ("A", "A2"):
        wq_d = dt("wq", [128, 8, 2048]); keys_d = dt("keysT", [128, 16, 128])
        ut_d = dt("UT", [128, 128, 8, 128]); v_d = dt("V", [16384, 1024])
    y_d = dt("y", [NT, D], kind="ExternalOutput")
    x1_d = dt("x1s", [NT, D], kind="Internal") if stage not in ("A", "A2") else y_d
    if stage not in ("A", "A2"):
        ub_d = dt("ubs", [128, 128, 1024], kind="Internal", dty=BF16)
        vb_d = dt("vbs", [16384, 1024], kind="Internal", dty=BF16)

    b = Bld(nc); P = b.P
    with contextlib.ExitStack() as st:
        NW = 41400
        cs = st.enter_context(nc.sbuf_tensor("cs", [128, C_TOT], F32))
        xn2T = st.enter_context(nc.sbuf_tensor("xn2T", [128, 8, NT], BF16))
        cb16 = st.enter_context(nc.sbuf_tensor("cb16", [128, 512], BF16))
        ar = st.enter_context(nc.sbuf_tensor("arena", [128, NW], F32))
        psb = [st.enter_context(nc.psum_tensor("psb%d" % i, [128, 512], F32))[:, :] for i in range(8)]
        identb = cb16[:, 0:128]; onesb = cb16[:, 128:256]; trib = cb16[:, 256:384]; iotab = cb16[:, 384:512]
        ident = cs[:, C_CST:C_CST + 128]; tri = cs[:, C_CST + 128:C_CST + 256]
        onesf = cs[:, C_CST + 256:C_CST + 384]; iota = cs[:, C_CST + 384:C_CST + 512]
        col = lambda c, n=1: cs[:, c:c + n]
        eps_c = col(C_EPS); one_c = col(C_ONE); lns_c = col(C_LNS); flag_c = col(C_FLAG)

        off = [0]
        def carve(words, dty=F32, shape=None):
            a = ar[:, off[0]:off[0] + words]; off[0] += words
            assert off[0] <= NW, off[0]
            if dty != F32: a = a.bitcast(dty)
            if shape is not None:
                names = " ".join("d%d" % i for i in range(len(shape)))
                a = a.rearrange("p (%s) -> p %s" % (names, names), **{"d%d" % i: s for i, s in enumerate(shape)})
            return a

        b.dma("sp", cs[:, :], cst_d[:, :], [], ["cs"], "cs")
        b.cp("dve", identb, ident, ["cs"], ["cb16"])
        b.cp("dve", onesb, onesf, ["cs"], ["cb16"])
        b.cp("dve", trib, tri, ["cs"], ["cb16"])
        b.cp("dve", iotab, iota, ["cs"], ["cb16"])

        Winb = carve(12320, BF16, [8, 3080]); Woutb = carve(4096, BF16, [8, 1024])
        xTs = carve(4096, F32, [8, 512]); raw = carve(4120, F32, [8, 515])
        sqb = carve(512, BF16, [2, 512]); lnv = carve(512); rstdb = carve(512)
        hT = carve(2048, BF16, [8, 512]); accs = carve(1024, F32, [2, 512]); qkT = carve(2048, BF16, [8, 512])
        WsTb = carve(256, BF16, [4, 128])
        sm = carve(128)
        sg = carve(192)
        vext = carve(264, BF16, [4, 132]); sig = carve(256, BF16); u_sb = carve(256, BF16)
        vgs = carve(512); vn32 = carve(512); vnb = carve(256, BF16); ktok = carve(256, BF16)
        PT = carve(256, BF16, [4, 128]); ybf = carve(512, BF16); yT = carve(512, BF16, [8, 128])
        xmt = carve(1024); x1 = carve(1024); xs = carve(512, BF16)
        Gst = carve(528, F32, [4, 132]); Cb = carve(264, BF16, [4, 132])
        offA = off[0]

        gsb = sg[:, 0:32]; e1 = sg[:, 32:48]; nlf = sg[:, 48:64]; t16 = sg[:, 64:80]; EK = sg[:, 80:96]; EB = sg[:, 96:112]
        A_t = [sg[:, 112:128], sg[:, 128:144]]; nhl16 = sg[:, 144:160].bitcast(BF16); nhi = nhl16[:, 0:16]; nlo = nhl16[:, 16:32]
        gsb3 = gsb.rearrange("p (j g) -> p j g", j=4)
        den = sm[:, 36:40]; dab = sm[:, 40:44]
        rr = sm[:, 44:48]; ssq = sm[:, 48:52]; mm4 = sm[:, 52:56]; scl = sm[:, 56:60]; st8 = sm[:, 60:68]
        mean = sm[:, 68:72]; msq = sm[:, 72:76]; var = sm[:, 76:80]; rs4 = sm[:, 80:84]
        ss2 = sm[:, 84:85]; l2 = sm[:, 85:86]; r2 = sm[:, 86:87]
        nhl = sm[:, 88:92].bitcast(BF16)
        psP = [psb[0], psb[1]]; psS = psb[3]; psO = [psb[4], psb[5]]; psU = [psb[6], psb[7]]
        psM = psb[2]
        psT = psM[:, 256:512].bitcast(BF16)
        Prog.BANK.clear()
        Prog.BANK.update({"psP0": "B0", "psP1": "B1", "psMg": "B2", "psMb": "B2", "psMl": "B2", "psT": "B2", "psS": "B3",
                          "psO0": "B4", "psO1": "B5", "psU0": "B6", "psU1": "B7",
                          "pq0": "B0", "pq1": "B1", "psc0": "B2", "psc1": "B3", "psc2": "B4", "psc3": "B5",
                          "pt6a": "B6", "pt6b": "B6", "pt6c": "B6",
                          "psOut0": "B0", "psOut1": "B1", "psOut2": "B2", "psOut3": "B3", "psA0": "B4", "psA1": "B5",
                          "psG0": "B6", "psG1": "B7"})
        pcnt = [0]
        def nextP():
            pcnt[0] += 1
            return psP[pcnt[0] % 2], "psP%d" % (pcnt[0] % 2)

        stg = [xTs.rearrange("p a b -> p (a b)"), raw.rearrange("p a b -> p (a b)")]
        for c in range(8):
            s = stg[c % 2][:, 0:3080]; sk = "stg%d" % (c % 2)
            b.dma("sp", s, win_d[:, c, :], [], [sk], sk)
            eng = "dve"
            b.cp(eng, Winb[:, c, 0:1540], s[:, 0:1540], [sk], ["Winb"])
            b.act(Winb[:, c, 1540:3080], s[:, 1540:3080], AF.Copy, [sk], ["Winb"])
        for hf in range(2):
            s = stg[hf][:, 0:4096]; sk = "stg%d" % hf
            b.dma("sp", s, wout_d[:, 4 * hf:4 * hf + 4, :], [], [sk], sk)
            for c4 in range(4):
                c = 4 * hf + c4
                if hf == 0:
                    b.ts("dve", Woutb[:, c, :], s[:, c4 * 1024:(c4 + 1) * 1024], col(C_NG + c), None, ALU.mult, None, [sk, "cs"], ["Woutb"])
                else:
                    b.cp("dve", Woutb[:, c, :], s[:, c4 * 1024:(c4 + 1) * 1024], [sk], ["Woutb"])
        b.cp("dve", WsTb.rearrange("p a b -> p (a b)"), cs[:, C_WS:C_WS + 512], ["cs"], ["WsTb"])
        for g in range(4):
            b.ms("dve", WsTb[64:128, g, 0:64], 0.0, ["WsTb"])
        b.ms("dve", raw.rearrange("p a b -> p (a b)"), 0.0, ["stg1"] + ["raw%d" % c for c in range(8)])
        b.ms("dve", Gst.rearrange("p a b -> p (a b)"), 0.0, ["G"])
        b.ms("dve", Cb.rearrange("p a b -> p (a b)"), 0.0, ["Cb"])
        b.ms("dve", A_t[1], 1.0, ["A1"])
        b.ms("dve", vext.rearrange("p a b -> p (a b)"), 0.0, ["vext"])

        for blk in range(int(os.environ.get("TRUNC", "8"))):
            main = blk >= 4
            tok0 = blk * 512
            for c in range(8):
                b.dma("sp", xTs[:, c, :], xT_d[:, c, tok0:tok0 + 512], [], ["xTs0", "stg0"] if c == 0 else ["xTs%d" % c], "xTs%d" % c)
            for c in range(8):
                b.act(sqb[:, c % 2, :], xTs[:, c, :], AF.Square, ["xTs%d" % c], ["sqb%d" % (c % 2)])
                b.mm(psS, onesb, sqb[:, c % 2, :], c == 0, c == 7, ["cb16", "sqb%d" % (c % 2)], ["psS"])
            b.act(lnv, psS, AF.Ln, ["psS", "cs"], ["lnv"], scale=1.0 / 1024, bias=eps_c)
            b.act(rstdb, lnv, AF.Exp, ["lnv"], ["rstdb"], scale=-0.5)
            for c in range(8):
                b.stt("dve", hT[:, c, :], xTs[:, c, :], col(C_G1 + c), rstdb, ALU.mult, ALU.mult, ["xTs%d" % c, "cs", "rstdb"], ["hT"])
            par = blk % 2
            for j in range(4):
                for c in range(8):
                    b.mm(psM[:, j * 8:(j + 1) * 8], hT[:, c, j * 128:(j + 1) * 128], Winb[:, c, 2048:2056], c == 0, c == 7, ["hT", "Winb"], ["psMg"])
            b.tt("dve", gsb3, psM[:, 0:32].rearrange("p (j g) -> p j g", j=4), col(C_GB, 8).unsqueeze(1).to_broadcast([128, 4, 8]), ALU.add,
                 ["psMg", "cs"], ["gsb"])
            b.act(e1.rearrange("p (j h) -> p j h", j=4), gsb3[:, :, 4:8], AF.Exp, ["gsb"], ["e1"], scale=-1.0)
            b.act(nlf, e1, AF.Ln, ["e1", "cs"], ["nlf"], scale=1.0, bias=one_c)
            b.cp("dve", nhi, nlf, ["nlf"], ["nhi"])
            b.tt("dve", nlo, nlf, nhi, ALU.subtract, ["nlf", "nhi"], ["nlo"])
            for j in range(4):
                js = slice(j * 4, (j + 1) * 4)
                b.mm(psM[:, 64 + j * 4:68 + j * 4], trib, nhi[:, js], True, False, ["cb16", "nhi"], ["psMb"])
                b.mm(psM[:, 64 + j * 4:68 + j * 4], trib, nlo[:, js], False, True, ["cb16", "nlo"], ["psMb"])
                b.mm(psM[:, 96 + j * 4:100 + j * 4], onesb, nhi[:, js], True, False, ["cb16", "nhi"], ["psMl"])
                b.mm(psM[:, 96 + j * 4:100 + j * 4], onesb, nlo[:, js], False, True, ["cb16", "nlo"], ["psMl"])
            b.tt("dve", t16.rearrange("p (j h) -> p j h", j=4), gsb3[:, :, 0:4], psM[:, 64:80].rearrange("p (j h) -> p j h", j=4), ALU.add,
                 ["gsb", "psMb"], ["t4"])
            b.act(EK, t16, AF.Exp, ["t4", "cs"], ["ek"], scale=1.0, bias=lns_c)
            b.act(EB, psM[:, 64:80], AF.Exp, ["psMb"], ["eb"], scale=-1.0)
            b.act(A_t[par], psM[:, 96:112], AF.Exp, ["psMl"], ["A%d" % par], scale=-1.0)
            if blk == 3:
                b.ts("dve", A_t[1][:, 12:16], A_t[1][:, 12:16], flag_c, None, ALU.mult, None, ["A1", "cs"], ["A1"])
            cc_list = list(range(8)) if blk >= 3 else list(range(4, 8))
            for cc in cc_list:
                ps, pk = nextP()
                for c in range(8):
                    b.mm(ps, Winb[:, c, cc * 128:(cc + 1) * 128], hT[:, c, :], c == 0, c == 7, ["Winb", "hT"], [pk])
                rk = "raw%d" % cc
                if blk == 4:
                    b.ts("dve", raw[:, cc, 0:3], raw[:, cc, 512:515], flag_c, None, ALU.mult, None, [rk, "cs"], [rk])
                elif blk > 0:
                    b.cp("dve", raw[:, cc, 0:3], raw[:, cc, 512:515], [rk], [rk])
                b.act(raw[:, cc, 3:515], ps, AF.Copy, [pk], [rk])
                if cc >= 4 or main:
                    ac = accs[:, cc % 2, :]; ak = "acc%d" % (cc % 2)
                    b.act(ac, raw[:, cc, 3:515], AF.Identity, [rk, "cs"], [ak], scale=col(C_CW + 3 * 8 + cc), bias=col(C_CB + cc))
                    for j in (2, 1, 0):
                        b.stt("dve", ac, raw[:, cc, j:j + 512], col(C_CW + j * 8 + cc), ac, ALU.mult, ALU.add, [rk, "cs", ak], [ak])
                    b.act(qkT[:, cc, :], ac, AF.Silu, [ak], ["qkT%d" % cc])
            for j in range(4):
                tsl = slice(j * 128, (j + 1) * 128)
                cidx = blk * 4 + j
                js = slice(j * 4, (j + 1) * 4)
                ek = EK[:, js]; eb = EB[:, js]
                acur = A_t[par][:, js]; ack = "A%d" % par
                if j > 0:
                    aprev = A_t[par][:, (j - 1) * 4:j * 4]; apk = ack
                else:
                    aprev = A_t[1 - par][:, 12:16]; apk = "A%d" % (1 - par)
                ps, pk = nextP()
                for c in range(8):
                    b.mm(ps, hT[:, c, tsl], Winb[:, c, 1024:1536], c == 0, c == 7, ["hT", "Winb"], [pk])
                for h in range(4):
                    b.act(vext[:, h, 0:128], ps[:, h * 128:(h + 1) * 128], AF.Copy, [pk, "ek"], ["vext"], scale=ek[:, h:h + 1])
                b.cp("dve", vext[:, :, 128], ek, ["ek"], ["vext"])
                for h in range(4):
                    b.tr(psT[:, h * 128:(h + 1) * 128], qkT[:, 4 + h, tsl], identb, ["qkT%d" % (4 + h), "cb16"], ["psT"])
                b.cp("dve", ktok, psT, ["psT"], ["ktok"])
                if main:
                    for h in range(4):
                        b.mm(psS[:, h * 128:(h + 1) * 128], qkT[:, 4 + h, tsl], qkT[:, h, tsl], True, True,
                             ["qkT%d" % (4 + h), "qkT%d" % h], ["psS"])
                    b.tt("dve", PT, psS.rearrange("p (a b) -> p a b", a=4), tri.unsqueeze(1).to_broadcast([128, 4, 128]),
                         ALU.mult, ["psS", "cs"], ["PT"])
                    for h in range(4):
                        o = psO[h // 2][:, (h % 2) * 256:(h % 2) * 256 + 129]; ok = "psO%d" % (h // 2)
                        b.mm(o, PT[:, h, :], vext[:, h, 0:129], True, False, ["PT", "vext"], [ok])
                        b.mm(o, qkT[:, h, tsl], Cb[:, h, 0:129], False, True, ["qkT%d" % h, "Cb"], [ok])
                for h in range(4):
                    u = psU[h // 2][:, (h % 2) * 256:(h % 2) * 256 + 129]; uk = "psU%d" % (h // 2)
                    b.mm(u, ktok[:, h * 128:(h + 1) * 128], vext[:, h, 0:129], True, True, ["ktok", "vext"], [uk])
                for h in range(4):
                    u = psU[h // 2][:, (h % 2) * 256:(h % 2) * 256 + 129]; uk = "psU%d" % (h // 2)
                    b.stt("dve", Gst[:, h, 0:129], Gst[:, h, 0:129], aprev[:, h:h + 1], u, ALU.mult, ALU.add, ["G", apk, uk], ["G"])
                    if cidx >= 15:
                        b.ts("dve", Cb[:, h, 0:129], Gst[:, h, 0:129], acur[:, h:h + 1], None, ALU.mult, None, ["G", ack], ["Cb"])
                if not main:
                    continue
                ps, pk = nextP()
                for c in range(8):
                    b.mm(ps, hT[:, c, tsl], Winb[:, c, 1536:2048], c == 0, c == 7, ["hT", "Winb"], [pk])
                b.act(sig, ps, AF.Sigmoid, [pk], ["sig"])
                ps, pk = nextP()
                for c in range(8):
                    b.mm(ps, hT[:, c, tsl], Winb[:, c, 2056:2568], c == 0, c == 7, ["hT", "Winb"], [pk])
                b.act(u_sb, ps, AF.Gelu_apprx_tanh, [pk], ["u_sb"])
                ps, pk = nextP()
                for c in range(8):
                    b.mm(ps, hT[:, c, tsl], Winb[:, c, 2568:3080], c == 0, c == 7, ["hT", "Winb"], [pk])
                b.ms("dve", st8, 0.0, ["st8"])
                for g in range(4):
                    b.act(vgs[:, g * 128:(g + 1) * 128], ps[:, g * 128:(g + 1) * 128], AF.Gelu_apprx_tanh, [pk, "st8"], ["vgs", "st8"],
                          accum=st8[:, g:g + 1])
                for g in range(4):
                    b.act(vn32[:, g * 128:(g + 1) * 128], vgs[:, g * 128:(g + 1) * 128], AF.Square, ["vgs", "st8"], ["vn32", "st8"],
                          accum=st8[:, 4 + g:5 + g])
                b.ts("dve", mean, st8[:, 0:4], 1.0 / 128, None, ALU.mult, None, ["st8"], ["mean"])
                b.tt("dve", msq, mean, mean, ALU.mult, ["mean"], ["msq"])
                b.stt("dve", var, st8[:, 4:8], 1.0 / 128, msq, ALU.mult, ALU.subtract, ["st8", "msq"], ["var"])
                b.act(var, var, AF.Ln, ["var", "cs"], ["var"], scale=1.0, bias=eps_c)
                b.act(rs4, var, AF.Exp, ["var"], ["rs4"], scale=-0.5)
                for g in range(4):
                    b.ts("dve", vn32[:, g * 128:(g + 1) * 128], vgs[:, g * 128:(g + 1) * 128], mean[:, g:g + 1], rs4[:, g:g + 1],
                         ALU.subtract, ALU.mult, ["vgs", "mean", "rs4"], ["vn32"])
                b.tt("dve", vn32, vn32, cs[:, C_LNG:C_LNG + 512], ALU.mult, ["vn32", "cs"], ["vn32"])
                b.tt("dve", vnb, vn32, cs[:, C_LNB:C_LNB + 512], ALU.add, ["vn32", "cs"], ["vnb"])
                for g in range(4):
                    b.mm(psS[:, g * 128:(g + 1) * 128], WsTb[:, g, :], vnb[:, g * 128:(g + 1) * 128], True, True, ["WsTb", "vnb"], ["psS"])
                for g in range(4):
                    b.stt("dve", ybf[:, 512 + g * 128:512 + (g + 1) * 128], psS[:, g * 128:(g + 1) * 128], col(C_BS + g),
                          u_sb[:, g * 128:(g + 1) * 128], ALU.add, ALU.mult, ["psS", "cs", "u_sb"], ["ybf"])
                for p2 in range(2):
                    dv = psO[p2].rearrange("p (h w) -> p h w", h=2)[:, :, 128]
                    b.tt("dve", den[:, 2 * p2:2 * p2 + 2], dv, eb[:, 2 * p2:2 * p2 + 2], ALU.mult, ["psO%d" % p2, "eb"], ["den"])
                b.act(dab, den, AF.Abs, ["den"], ["dab"])
                b.ts("dve", dab, dab, 1.0, None, ALU.max, None, ["dab"], ["dab"])
                b.P.op("dve", lambda e: e.reciprocal(out=dab, in_=dab), reads=["dab"], writes=["dab"])
                b.tt("dve", rr, eb, dab, ALU.mult, ["eb", "dab"], ["rr"])
                b.ms("dve", ssq, 0.0, ["ssq"])
                for h in range(4):
                    o = psO[h // 2][:, (h % 2) * 256:(h % 2) * 256 + 128]
                    b.act(vn32[:, 0:128], o, AF.Square, ["psO%d" % (h // 2), "ssq"], ["vn32", "ssq"], accum=ssq[:, h:h + 1])
                b.tt("dve", mm4, ssq, rr, ALU.mult, ["ssq", "rr"], ["mm4"])
                b.tt("dve", mm4, mm4, rr, ALU.mult, ["mm4", "rr"], ["mm4"])
                b.act(mm4, mm4, AF.Ln, ["mm4", "cs"], ["mm4"], scale=1.0 / 128, bias=eps_c)
                b.act(mm4, mm4, AF.Exp, ["mm4"], ["mm4"], scale=-0.5)
                b.tt("dve", scl, rr, mm4, ALU.mult, ["rr", "mm4"], ["scl"])
                for h in range(4):
                    o = psO[h // 2][:, (h % 2) * 256:(h % 2) * 256 + 128]
                    b.stt("dve", ybf[:, h * 128:(h + 1) * 128], o, scl[:, h:h + 1], sig[:, h * 128:(h + 1) * 128], ALU.mult, ALU.mult,
                          ["psO%d" % (h // 2), "scl", "sig"], ["ybf"])
                for hf in range(2):
                    for q in range(4):
                        b.tr(psT[:, q * 128:(q + 1) * 128], ybf[:, (hf * 4 + q) * 128:(hf * 4 + q + 1) * 128], identb, ["ybf", "cb16"], ["psT"])
                    b.cp("dve", yT[:, hf * 4:hf * 4 + 4, :].rearrange("p a b -> p (a b)"), psT, ["psT"], ["yT"])
                for hf in range(2):
                    for c in range(8):
                        b.mm(psO[hf], yT[:, c, :], Woutb[:, c, hf * 512:(hf + 1) * 512], c == 0, c == 7, ["yT", "Woutb"], ["psO%d" % hf])
                mt0 = (blk - 4) * 512 + j * 128
                b.dma("sp", xmt, xm_d[mt0:mt0 + 128, :], [], ["xmt"], "xmt")
                for hf in range(2):
                    b.tt("dve", x1[:, hf * 512:(hf + 1) * 512], psO[hf], xmt[:, hf * 512:(hf + 1) * 512], ALU.add, ["psO%d" % hf, "xmt"], ["x1"])
                b.dma("sp", x1_d[mt0:mt0 + 128, :], x1, ["x1"], ["x1d%d" % (mt0 // 128)], "x1st")
                if stage == "A":
                    continue
                b.ms("dve", ss2, 0.0, ["ss2"])
                b.act(xs, x1, AF.Square, ["x1", "ss2"], ["xs", "ss2"], accum=ss2)
                b.act(l2, ss2, AF.Ln, ["ss2", "cs"], ["l2"], scale=1.0 / 1024, bias=eps_c)
                b.act(r2, l2, AF.Exp, ["l2"], ["r2"], scale=-0.5)
                b.act(xs, x1, AF.Copy, ["x1", "r2"], ["xs"], scale=r2)
                for hf in range(2):
                    for q in range(4):
                        b.tr(psT[:, q * 128:(q + 1) * 128], xs[:, (hf * 4 + q) * 128:(hf * 4 + q + 1) * 128], identb, ["xs", "cb16"], ["psT"])
                    for q in range(4):
                        b.ts("dve", xn2T[:, hf * 4 + q, mt0:mt0 + 128], psT[:, q * 128:(q + 1) * 128], col(C_G2 + hf * 4 + q), None,
                             ALU.mult, None, ["psT", "cs"], ["xn2T"])
        fkeys = ["x1d%d" % i for i in range(16)]
        if stage not in ("A", "A2"):
            fkeys = build_peer(nc, b, carve, off, cs, xn2T, identb, ident, iotab, psb, wq_d, keys_d, ut_d, v_d, x1_d, y_d, stage, ub_d, vb_d)
        P.emit(final_keys=fkeys)
    return nc


import os


def build_peer(nc, b, carve, off, cs, xn2T, identb, ident, iota, psb, wq_d, keys_d, ut_d, v_d, x1_d, y_d, stage='full', ub_d=None, vb_d=None):
    P = b.P
    P.fence()
    off[0] = 0
    col = lambda c, n=1: cs[:, c:c + n]
    eps_c = col(C_EPS)
    I1T = carve(2048); I2T = carve(2048); GTs = carve(2048)
    offBC = off[0]
    Wqb = carve(8192, BF16, [8, 2048]); keysTb = carve(1024, BF16, [16, 128]); stg = carve(2048)
    qTb = carve(1024, BF16, [16, 128]); sc2 = carve(4096, F32, [2, 16, 128]); wk = carve(2048, F32, [16, 128])
    m16 = carve(256, F32, [16, 16]); i16 = carve(256, U32, [16, 16]); i16f = carve(256, F32, [16, 16])
    NC = 80; RECTS = [(0, 2, 16, 0), (2, 2, 8, 32), (4, 4, 4, 48), (8, 8, 2, 64)]
    cand = carve(8 * NC, F32, [8, NC]); cidx = carve(8 * NC, F32, [8, NC]); junk = carve(4 * NC, F32, [4, NC])
    ts16 = carve(128, F32, [8, 16]); eidx = carve(128); ex = carve(128, F32, [8, 16]); gate = carve(128, F32, [8, 16])
    ei = carve(128, I32); i1i = carve(128, I32); i2i = carve(128, I32); i1f = carve(128); i2f = carve(128)
    tb3 = carve(192, BF16)
    smb = carve(32); negm = smb[:, 0:8]; Z = smb[:, 8:16]; rz = smb[:, 16:24]
    cin = carve(6144, F32, [3, 2, 1024]); cout = carve(3072, BF16, [3, 2, 1024])
    jobs = {"in": 0, "cv": 0}

    def conv_in():
        j = jobs["in"]
        if j >= 128: return
        jobs["in"] += 1; s3 = j % 3
        b.dma("sp", cin[:, s3, 0, :], ut_d[j].rearrange("p c e -> p (c e)"), [], ["cinu%d" % s3], "cinu%d" % s3)
        b.dma("sp", cin[:, s3, 1, :], v_d[j * 128:(j + 1) * 128, :], [], ["cinv%d" % s3], "cinv%d" % s3)

    def conv_cv():
        j = jobs["cv"]
        if j >= 128: return
        jobs["cv"] += 1; s3 = j % 3
        b.act(cout[:, s3, 0, :], cin[:, s3, 0, :], AF.Copy, ["cinu%d" % s3], ["coutu%d" % s3])
        b.act(cout[:, s3, 1, :], cin[:, s3, 1, :], AF.Copy, ["cinv%d" % s3], ["coutv%d" % s3])
        b.dma("sp", ub_d[j], cout[:, s3, 0, :], ["coutu%d" % s3], ["ubd"], "cou%d" % s3)
        b.dma("sp", vb_d[j * 128:(j + 1) * 128, :], cout[:, s3, 1, :], ["coutv%d" % s3], ["vbd"], "cov%d" % s3)

    conv_in(); conv_in()
    for c in range(8):
        b.dma("sp", stg, wq_d[:, c, :], [], ["stgB"], "stgB")
        b.cp("dve", Wqb[:, c, 0:1024], stg[:, 0:1024], ["stgB"], ["Wqb"])
        b.act(Wqb[:, c, 1024:2048], stg[:, 1024:2048], AF.Copy, ["stgB"], ["Wqb"])
    b.dma("sp", stg, keys_d[:, :, :].rearrange("p a b -> p (a b)"), [], ["stgB"], "stgB")
    b.cp("dve", keysTb.rearrange("p a b -> p (a b)"), stg, ["stgB"], ["keysTb"])
    PB = int(os.environ.get("PB_STOP", "99"))
    if PB <= 0: return ["x1d%d" % q for q in range(16)]
    def part1(i):
        t0 = i * 128; par = i % 2; sc = sc2[:, par]
        for hp in range(16):
            ps = psb[hp % 2]; pk = "pq%d" % (hp % 2)
            for c in range(8):
                b.mm(ps[:, 0:128], Wqb[:, c, hp * 128:(hp + 1) * 128], xn2T[:, c, t0:t0 + 128], c == 0, c == 7, ["Wqb", "xn2T"], [pk])
            b.act(qTb[:, hp, :], ps[:, 0:128], AF.Copy, [pk], ["qTb%d" % hp])
            if hp % 2 == 0:
                conv_in(); conv_cv()
        for hp in range(16):
            b.mm(psb[2 + hp // 4][:, (hp % 4) * 128:(hp % 4 + 1) * 128], qTb[:, hp, :], keysTb[:, hp, :], True, True,
                 ["qTb%d" % hp, "keysTb"], ["psc%d" % (hp // 4)])
        for q4 in range(4):
            b.act(sc[:, q4 * 4:q4 * 4 + 4, :].rearrange("p a b -> p (a b)"), psb[2 + q4], AF.Copy, ["psc%d" % q4], ["sc%d_%d" % (q4, par)])

    def part2(i):
        t0 = i * 128; par = i % 2; sc = sc2[:, par]
        for hp in range(16):
            b.P.op("dve", (lambda hp: lambda e: e.max(out=m16[:, hp, 0:8], in_=sc[:, hp, :]))(hp), reads=["sc%d_%d" % (hp // 4, par)], writes=["m16_%d" % hp])
        for hp in range(16):
            b.P.op("dve", (lambda hp: lambda e: e.max_index(out=i16[:, hp, 0:8], in_max=m16[:, hp, 0:8], in_values=sc[:, hp, :]))(hp),
                   reads=["sc%d_%d" % (hp // 4, par), "m16_%d" % hp], writes=["i16_%d" % hp])
        for hp in range(16):
            b.P.op("dve", (lambda hp: lambda e: e.match_replace(out=wk[:, hp, :], in_to_replace=m16[:, hp, 0:8], in_values=sc[:, hp, :], imm_value=-1e30))(hp),
                   reads=["sc%d_%d" % (hp // 4, par), "m16_%d" % hp], writes=["wk%d" % hp])
        for hp in range(16):
            b.P.op("dve", (lambda hp: lambda e: e.max(out=m16[:, hp, 8:16], in_=wk[:, hp, :]))(hp), reads=["wk%d" % hp], writes=["m16_%d" % hp])
        for hp in range(16):
            b.P.op("dve", (lambda hp: lambda e: e.max_index(out=i16[:, hp, 8:16], in_max=m16[:, hp, 8:16], in_values=wk[:, hp, :]))(hp),
                   reads=["wk%d" % hp, "m16_%d" % hp], writes=["i16_%d" % hp])
        allm = ["m16_%d" % hp for hp in range(16)]; alli = ["i16_%d" % hp for hp in range(16)]
        b.cp("dve", i16f.rearrange("p a b -> p (a b)"), i16.rearrange("p a b -> p (a b)"), alli, ["i16f"])
        m16h = m16.rearrange("p (h two) a -> p h two a", two=2)
        i16h = i16f.rearrange("p (h two) a -> p h two a", two=2)
        allc = ["cand%d" % h for h in range(8)]; allx = ["cidx%d" % h for h in range(8)]
        b.ts("dve", i16h[:, :, 0, :], i16h[:, :, 0, :], 128.0, None, ALU.mult, None, ["i16f"], ["i16f"])
        for (a0, na, nb, o0) in RECTS:
            shp = [128, 8, na, nb]
            b.tt("dve", cand[:, :, o0:o0 + na * nb].rearrange("p h (a c) -> p h a c", a=na),
                 m16h[:, :, 0, a0:a0 + na].unsqueeze(3).to_broadcast(shp), m16h[:, :, 1, 0:nb].unsqueeze(2).to_broadcast(shp),
                 ALU.add, allm, allc)
            b.tt("dve", cidx[:, :, o0:o0 + na * nb].rearrange("p h (a c) -> p h a c", a=na),
                 i16h[:, :, 0, a0:a0 + na].unsqueeze(3).to_broadcast(shp), i16h[:, :, 1, 0:nb].unsqueeze(2).to_broadcast(shp),
                 ALU.add, ["i16f"], allx)
        wk2 = wk.rearrange("p a b -> p (a b)").rearrange("p (a b) -> p a b", a=8)[:, :, 0:NC]
        for h in range(8):
            b.P.op("dve", (lambda h: lambda e: e.max(out=ts16[:, h, 0:8], in_=cand[:, h, :]))(h), reads=["cand%d" % h], writes=["ts16_%d" % h])
        for h in range(8):
            b.P.op("dve", (lambda h: lambda e: e.match_replace(out=wk2[:, h, :], in_to_replace=ts16[:, h, 0:8], in_values=cand[:, h, :], imm_value=-1e30))(h),
                   reads=["cand%d" % h, "ts16_%d" % h] + ["wk%d" % (2 * h), "wk%d" % (2 * h + 1)], writes=["wk%d" % (2 * h), "wk%d" % (2 * h + 1)])
        for h in range(8):
            b.P.op("dve", (lambda h: lambda e: e.max(out=ts16[:, h, 8:16], in_=wk2[:, h, :]))(h), reads=["wk%d" % (2 * h), "wk%d" % (2 * h + 1)], writes=["ts16_%d" % h])
        b.ms("dve", eidx, 0.0, ["eidx"])
        if os.environ.get("TRIV"):
            for q in range(int(os.environ["TRIV"])):
                b.ms("dve", junk[:, q % 4, :], 0.0, ["junk%d" % (q % 4)])
            return ["x1d%d" % q for q in range(16)]
        for j in range(16):
            for h in range(8):
                jk = "junk%d" % ((j * 8 + h) % 4)
                b.stt("dve", junk[:, (j * 8 + h) % 4, :], cand[:, h, :], ts16[:, h, j:j + 1], cidx[:, h, :], ALU.is_equal, ALU.mult,
                      ["cand%d" % h, "ts16_%d" % h, "cidx%d" % h, "eidx"], [jk, "eidx%d" % (h * 16 + j)], accum=(None if os.environ.get("NOACC") else eidx[:, h * 16 + j:h * 16 + j + 1]))
        alle = ["eidx"] + ["eidx%d" % q for q in range(128)]
        allts = ["ts16_%d" % h for h in range(8)]
        b.ts("dve", negm, ts16[:, :, 0], -1.0, None, ALU.mult, None, allts, ["negm"])
        b.ms("dve", Z, 0.0, ["Z"])
        for h in range(8):
            b.act(ex[:, h, :], ts16[:, h, :], AF.Exp, allts + ["negm", "Z"], ["ex", "Z"], scale=1.0, bias=negm[:, h:h + 1], accum=Z[:, h:h + 1])
        b.P.op("dve", lambda e: e.reciprocal(out=rz, in_=Z), reads=["Z"], writes=["rz"])
        b.tt("dve", gate, ex, rz.unsqueeze(2).to_broadcast([128, 8, 16]), ALU.mult, ["ex", "rz"], ["gate"])
        b.cp("dve", ei, eidx, alle, ["ei"])
        b.ts("dve", i1i, ei, 7, None, ALU.arith_shift_right, None, ["ei"], ["i1i"])
        b.ts("dve", i2i, ei, 127, None, ALU.bitwise_and, None, ["ei"], ["i2i"])
        b.cp("dve", tb3[:, 0:128], i1i, ["i1i"], ["tb3a"])
        b.cp("dve", tb3[:, 128:256], i2i, ["i2i"], ["tb3b"])
        b.cp("dve", tb3[:, 256:384], gate.rearrange("p a b -> p (a b)"), ["gate"], ["tb3c"])
        pT6 = psb[6].bitcast(BF16)
        b.tr(pT6[:, 0:128], tb3[:, 0:128], identb, ["tb3a"], ["pt6a"])
        b.tr(pT6[:, 128:256], tb3[:, 128:256], identb, ["tb3b"], ["pt6b"])
        b.tr(pT6[:, 256:384], tb3[:, 256:384], identb, ["tb3c"], ["pt6c"])
        b.act(I1T[:, t0:t0 + 128], pT6[:, 0:128], AF.Copy, ["pt6a"], ["I1T"])
        b.act(I2T[:, t0:t0 + 128], pT6[:, 128:256], AF.Copy, ["pt6b"], ["I2T"])
        b.act(GTs[:, t0:t0 + 128], pT6[:, 256:384], AF.Copy, ["pt6c"], ["GTs"])
    part1(0)
    for i in range(16):
        if i + 1 < 16: part1(i + 1)
        part2(i)
    while jobs["cv"] < 128:
        conv_in(); conv_cv()
    if stage == "AB":
        return ["x1d%d" % i for i in range(16)]
    P.fence()
    off[0] = offBC
    GT = carve(16384, BF16, [128, 256]); ohA = carve(256, BF16, [8, 64]); ohB = carve(512, BF16, [8, 128])
    NSL = 8
    Ub = carve(NSL * 512, BF16, [NSL, 8, 128]); Vb = carve(NSL * 512, BF16, [NSL, 1024])
    Ab = carve(256, BF16, [2, 256]); AG = carve(256, BF16, [2, 256])
    x1t = [carve(1024), carve(1024)]; x2 = [carve(1024), carve(1024)]; ot = [carve(1024), carve(1024)]; smc = carve(16)
    psOut = psb[0:4]; psA = psb[4:6]; psG = psb[6:8]
    fkeys = []
    GL = [(0, 0, g) for g in range(64)]
    for T in range(8):
        GL += [(T, 1, g) for g in range(64)]
        if T + 1 < 8: GL += [(T + 1, 0, g) for g in range(64)]
    gn = [0]

    def gstep():
        n = gn[0]; gn[0] += 1
        if n + 1 < len(GL):
            T, half, grp = GL[n + 1]; p = (n + 1) % 2; h0 = 64 * half
            for s4 in range(4):
                t = T * 256 + grp * 4 + s4; sl = p * 4 + s4
                b.ts("dve", ohA[:, sl, :], iota[:, h0:h0 + 64], I1T[:, t:t + 1], GTs[:, t:t + 1], ALU.is_equal, ALU.mult,
                     ["cb16", "I1T", "GTs"], ["ohA%d" % sl])
                b.ts("dve", ohB[:, sl, :], iota, I2T[:, t:t + 1], None, ALU.is_equal, None, ["cb16", "I2T"], ["ohB%d" % sl])
        if n < len(GL):
            p = n % 2
            for s4 in range(4):
                sl = p * 4 + s4
                b.mm(psG[p][:, s4 * 64:(s4 + 1) * 64], ohB[:, sl, :], ohA[:, sl, :], True, True, ["ohA%d" % sl, "ohB%d" % sl], ["psG%d" % p])
        if 1 <= n <= len(GL):
            T, half, grp = GL[n - 1]; p = (n - 1) % 2; h0 = 64 * half
            b.act(GT[:, h0:h0 + 64, grp * 4:grp * 4 + 4], psG[p][:, 0:256].rearrange("p (t i) -> p i t", t=4), AF.Copy,
                  ["psG%d" % p], ["GT%d" % half])

    T, half, grp = GL[0]
    for s4 in range(4):
        t = grp * 4 + s4
        b.ts("dve", ohA[:, s4, :], iota[:, 0:64], I1T[:, t:t + 1], GTs[:, t:t + 1], ALU.is_equal, ALU.mult, ["cb16", "I1T", "GTs"], ["ohA%d" % s4])
        b.ts("dve", ohB[:, s4, :], iota, I2T[:, t:t + 1], None, ALU.is_equal, None, ["cb16", "I2T"], ["ohB%d" % s4])
    for _ in range(64):
        gstep()

    ssq2 = smc[:, 0:2]; lf2 = smc[:, 2:4]; rf2 = smc[:, 4:6]

    def epi_sq(T, sub):
        b.ms("dve", ssq2[:, sub:sub + 1], 0.0, ["ssf%d" % sub])
        b.act(ot[sub], x2[sub], AF.Square, ["x2%d" % sub, "ssf%d" % sub], ["ot%d" % sub, "ssf%d" % sub], accum=ssq2[:, sub:sub + 1])

    def epi_ln(T):
        b.act(lf2, ssq2, AF.Ln, ["ssf0", "ssf1", "cs"], ["lf_"], scale=1.0 / 1024, bias=eps_c)
        b.act(rf2, lf2, AF.Exp, ["lf_"], ["rf"], scale=-0.5)

    def epi_fin(T, sub):
        r0 = T * 256 + sub * 128
        b.stt("dve", ot[sub], x2[sub], rf2[:, sub:sub + 1], cs[:, C_FGB:C_FGB + 1024], ALU.mult, ALU.mult,
              ["x2%d" % sub, "rf", "cs", "ot%d" % sub], ["ot%d" % sub])
        k = "yd%d" % (r0 // 128)
        b.dma("pool", y_d[r0:r0 + 128, :], ot[sub], ["ot%d" % sub], [k], "yst")
        fkeys.append(k)

    EPI = {1: lambda T: epi_sq(T, 0), 3: lambda T: epi_sq(T, 1), 5: epi_ln, 8: lambda T: epi_fin(T, 0), 10: lambda T: epi_fin(T, 1)}

    for T in range(8):
        tb = T * 256

        def stage1(blk):
            s4 = blk % NSL; s2 = blk % 2
            b.dma("sp", Ub[:, s4, :, :].rearrange("p a b -> p (a b)"), ub_d[blk], ["ubd"], ["Ub%d" % s4], "Ub%d" % s4)
            b.dma("sp", Vb[:, s4, :], vb_d[blk * 128:(blk + 1) * 128, :], ["vbd"], ["Vb%d" % s4], "Vb%d" % s4)
            for c in range(8):
                b.mm(psA[s2][:, 0:256], Ub[:, s4, c, :], xn2T[:, c, tb:tb + 256], c == 0, c == 7, ["Ub%d" % s4, "xn2T"], ["psA%d" % s2])

        def stage2(blk):
            s2 = blk % 2; s4 = blk % NSL
            b.act(Ab[:, s2, :], psA[s2][:, 0:256], AF.Gelu_apprx_tanh, ["psA%d" % s2], ["Ab%d" % s2])
            b.tt("dve", AG[:, s2, :], Ab[:, s2, :], GT[:, blk, :], ALU.mult, ["Ab%d" % s2, "GT%d" % (blk // 64)], ["AG%d" % s2])
            for sub in range(2):
                for hf in range(2):
                    b.mm(psOut[sub * 2 + hf], AG[:, s2, sub * 128:(sub + 1) * 128], Vb[:, s4, hf * 512:(hf + 1) * 512], blk == 0, blk == 127,
                         ["AG%d" % s2, "Vb%d" % s4], ["psOut%d" % (sub * 2 + hf)])

        for blk in range(129):
            if blk < 128: stage1(blk)
            if blk >= 1: stage2(blk - 1)
            if blk < 128:
                gstep()
            if blk in EPI and T > 0:
                EPI[blk](T - 1)
            if blk == 96:
                for sub in range(2):
                    r0 = tb + sub * 128
                    b.dma("sp", x1t[sub], x1_d[r0:r0 + 128, :], ["x1d%d" % (r0 // 128)], ["x1t%d" % sub], "x1t%d" % sub)
        for sub in range(2):
            for hf in range(2):
                b.tt("dve", x2[sub][:, hf * 512:(hf + 1) * 512], psOut[sub * 2 + hf], x1t[sub][:, hf * 512:(hf + 1) * 512], ALU.add,
                     ["psOut%d" % (sub * 2 + hf), "x1t%d" % sub], ["x2%d" % sub])
    for k in sorted(EPI):
        EPI[k](7)
    return fkeys


def host_inputs(inp):
    f = lambda a: np.ascontiguousarray(a, dtype=np.float32)
    x = inp["x"]
    consts = np.zeros((128, C_TOT), np.float32)
    cw = inp["conv_w"][0]
    consts[:, C_CW:C_CW + 32] = cw.reshape(4, 8, 128).transpose(2, 0, 1).reshape(128, 32)
    consts[:, C_CB:C_CB + 8] = inp["conv_b"][0].reshape(8, 128).T
    consts[:, C_G1:C_G1 + 8] = inp["norm1_g"][0].reshape(8, 128).T
    consts[:, C_G2:C_G2 + 8] = inp["norm2_g"][0].reshape(8, 128).T
    consts[:, C_NG:C_NG + 4] = inp["mlstm_norm_g"][0].reshape(4, 128).T
    consts[:, C_GB:C_GB + 4] = inp["b_igate"][0][None, :]
    consts[:, C_GB + 4:C_GB + 8] = inp["b_fgate"][0][None, :]
    consts[:, C_BS:C_BS + 4] = inp["gmlp_b_s"][0].T
    consts[:, C_EPS] = EPS; consts[:, C_ONE] = 1.0; consts[:, C_LNS] = np.float32(np.log(128.0 ** -0.5))
    consts[:, C_LNG:C_LNG + 512] = inp["gmlp_ln_g"][0][None, :]
    consts[:, C_LNB:C_LNB + 512] = inp["gmlp_ln_b"][0][None, :]
    consts[:, C_FGB:C_FGB + 1024] = inp["final_g"][None, :]
    consts[:, C_CST:C_CST + 128] = np.eye(128)
    consts[:, C_CST + 128:C_CST + 256] = np.triu(np.ones((128, 128)))
    consts[:, C_CST + 256:C_CST + 384] = 1.0
    consts[:, C_CST + 384:C_CST + 512] = np.arange(128)[None, :]
    consts[:, C_WS:C_WS + 512] = inp["gmlp_w_s"][0].transpose(2, 0, 1).reshape(128, 512)
    shared = {
        "win": f(inp["w_in"][0].reshape(8, 128, 3080).transpose(1, 0, 2)),
        "wout": f(inp["w_out"][0].reshape(8, 128, 1024).transpose(1, 0, 2)),
        "wq": f(inp["peer_w_query"][0].reshape(8, 128, 2048).transpose(1, 0, 2)),
        "keysT": f(inp["peer_sub_keys"][0].reshape(16, 128, 128).transpose(2, 0, 1)),
        "UT": f(inp["peer_u"][0].reshape(128, 128, 8, 128).transpose(0, 3, 2, 1)),
        "V": f(inp["peer_v"][0]),
    }
    maps = []
    for core in range(NCORES):
        bi, s = core // 2, core % 2
        xm = x[bi, s * NT:(s + 1) * NT]
        pre = x[bi, 0:NT]
        toks = np.concatenate([pre, xm], axis=0)
        xT = f(toks.T.reshape(8, 128, 4096).transpose(1, 0, 2))
        c = consts.copy(); c[:, C_FLAG] = float(s)
        m = dict(shared); m.update({"xT": xT, "xm": f(xm), "consts": c})
        maps.append(m)
    return maps


_NC_CACHE = {}


def kernel(**inputs):
    stage = "full"
    if stage not in _NC_CACHE:
        _NC_CACHE[stage] = build(stage)
    nc = _NC_CACHE[stage]
    maps = host_inputs({k: np.asarray(v) for k, v in inputs.items()})
    res = run_bass_kernel_spmd(nc, maps, core_ids=list(range(NCORES)))
    out = np.empty((4, 4096, 1024), np.float32)
    for core in range(NCORES):
        bi, s = core // 2, core % 2
        out[bi, s * NT:(s + 1) * NT] = res.results[core]["y"]
    return out
```

```python
import contextlib
import os
import numpy as np
import concourse.bass as bass
import concourse.mybir as mybir
from concourse.bass_utils import run_bass_kernel_spmd

F32 = mybir.dt.float32; BF16 = mybir.dt.bfloat16; U32 = mybir.dt.uint32; I32 = mybir.dt.int32
AF = mybir.ActivationFunctionType; ALU = mybir.AluOpType

NCORES = 8
NT = 2048
D = 1024
EPS = 1e-6
C_CW = 0; C_CB = 32; C_G1 = 40; C_G2 = 48; C_NG = 56; C_GB = 60; C_BS = 68; C_FLAG = 72
C_EPS = 73; C_ONE = 74; C_LNS = 75; C_SMALL = 80
C_LNG = 80; C_LNB = C_LNG + 512; C_FGB = C_LNB + 512; C_CST = C_FGB + 1024; C_WS = C_CST + 512
C_TOT = C_WS + 512


class Prog:
    ENG = ("pe", "act", "dve", "pool", "sp")

    def __init__(self, nc):
        self.nc = nc; self.ops = []; self.lastw = {}; self.readers = {}; self.dma_cnt = {}

    BANK = {}

    def op(self, eng, fn, reads=(), writes=(), dma=None):
        i = len(self.ops)
        reads = list(reads) + [self.BANK[k] for k in reads if k in self.BANK]
        writes = list(writes) + [self.BANK[k] for k in writes if k in self.BANK]
        deps = set()
        for k in list(reads) + list(writes):
            if k in self.lastw: deps.add(self.lastw[k])
        for k in writes:
            lastr = {}
            for r in self.readers.get(k, ()):
                ro = self.ops[r]
                if ro["dma"] is not None: deps.add(r)
                else: lastr[ro["eng"]] = r
            deps.update(lastr.values())
        o = dict(eng=eng, fn=fn, deps=deps, dma=dma, sig=False, dcount=None, sidx=None)
        if dma is not None:
            self.dma_cnt[dma] = self.dma_cnt.get(dma, 0) + 1
            o["dcount"] = self.dma_cnt[dma]
        self.ops.append(o)
        for k in writes:
            self.lastw[k] = i; self.readers[k] = []
        for k in reads:
            self.readers.setdefault(k, []).append(i)
        return i

    def fence(self):
        last = {}
        for i, o in enumerate(self.ops):
            if o["fn"] is None: continue
            if o["dma"] is not None: last[("d", o["dma"])] = i
            else: last[("e", o["eng"])] = i
        for e in self.ENG:
            i = self.op(e, None)
            self.ops[i]["deps"] = set(last.values())

    def emit(self, final_keys=()):
        nc = self.nc; ops = self.ops
        if not os.environ.get("NOFENCE"):
            self.fence()
        self.op("sp", None, reads=list(final_keys), writes=["__final"])
        pos = {e: 0 for e in self.ENG}
        for o in ops:
            if o["fn"] is not None:
                pos[o["eng"]] += 1
            o["epos"] = pos[o["eng"]]

        def needs(o, od):
            if od["dma"] is not None: return True
            if od["eng"] == o["eng"] and o["fn"] is not None and o["dma"] is None:
                if o["eng"] == "pe": return False
                return o["epos"] - od["epos"] < int(os.environ.get("NEEDS_DIST", "4"))
            return True
        self.needs = needs
        for o in ops:
            for d in o["deps"]:
                od = ops[d]
                if od["dma"] is None and needs(o, od):
                    od["sig"] = True
        cnt = {e: 0 for e in self.ENG}
        for o in ops:
            if o["dma"] is None and o["sig"] and o["fn"] is not None:
                cnt[o["eng"]] += 1; o["sidx"] = cnt[o["eng"]]
        self.stats = dict(nops=len(ops), sig=cnt, dma=dict(self.dma_cnt))
        with contextlib.ExitStack() as st:
            esem = {e: st.enter_context(nc.semaphore("s_" + e)) for e in self.ENG}
            dsem = {k: st.enter_context(nc.semaphore("d_" + str(k))) for k in self.dma_cnt}
            block = st.enter_context(nc.Block())

            def expand(o, acc, seen):
                for d in o["deps"]:
                    if d in seen: continue
                    seen.add(d)
                    od = ops[d]
                    if od["fn"] is None:
                        expand(od, acc, seen)
                    elif od["dma"] is not None:
                        acc.append((("d", od["dma"]), 16 * od["dcount"]))
                    elif self.needs(o, od):
                        acc.append((("e", od["eng"]), od["sidx"]))

            def run(ename, eng):
                waited = {}
                for o in ops:
                    if o["eng"] != ename: continue
                    acc = []
                    expand(o, acc, set())
                    best = {}
                    for k, v in acc:
                        if v is not None and v > best.get(k, 0): best[k] = v
                    for k, v in best.items():
                        if waited.get(k, 0) >= v: continue
                        waited[k] = v
                        eng.wait_ge(esem[k[1]] if k[0] == "e" else dsem[k[1]], v)
                    if o["fn"] is None: continue
                    ins = o["fn"](eng)
                    if o["dma"] is not None:
                        ins.then_inc(dsem[o["dma"]], 16)
                    elif o["sig"]:
                        ins.then_inc(esem[ename], 1)

            @block.tensor
            def _(e): run("pe", e)

            @block.scalar
            def _(e): run("act", e)

            @block.vector
            def _(e): run("dve", e)

            @block.gpsimd
            def _(e): run("pool", e)

            @block.sync
            def _(e): run("sp", e)


class Bld:
    def __init__(self, nc):
        self.P = Prog(nc)

    def mm(self, out, lhsT, rhs, start, stop, r, w):
        self.P.op("pe", lambda e: e.matmul(out, lhsT=lhsT, rhs=rhs, start=start, stop=stop), reads=r, writes=w)

    def tr(self, out, in_, ident, r, w):
        self.P.op("pe", lambda e: e.transpose(out=out, in_=in_, identity=ident), reads=r, writes=w)

    def act(self, out, in_, func, r, w, scale=None, bias=None, accum=None):
        kw = {}
        if scale is not None: kw["scale"] = scale
        if bias is not None: kw["bias"] = bias
        if accum is not None: kw["accum_out"] = accum
        self.P.op("act", lambda e: e.activation(out=out, in_=in_, func=func, **kw), reads=r, writes=w)

    def tt(self, eng, out, in0, in1, op, r, w):
        self.P.op(eng, lambda e: e.tensor_tensor(out=out, in0=in0, in1=in1, op=op), reads=r, writes=w)

    def ts(self, eng, out, in0, s1, s2, op0, op1, r, w):
        if s2 is None:
            self.P.op(eng, lambda e: e.tensor_scalar(out=out, in0=in0, scalar1=s1, scalar2=None, op0=op0), reads=r, writes=w)
        else:
            self.P.op(eng, lambda e: e.tensor_scalar(out=out, in0=in0, scalar1=s1, scalar2=s2, op0=op0, op1=op1), reads=r, writes=w)

    def stt(self, eng, out, in0, scalar, in1, op0, op1, r, w, accum=None):
        kw = {} if accum is None else {"accum_out": accum}
        self.P.op(eng, lambda e: e.scalar_tensor_tensor(out=out, in0=in0, scalar=scalar, in1=in1, op0=op0, op1=op1, **kw), reads=r, writes=w)

    def cp(self, eng, out, in_, r, w):
        self.P.op(eng, lambda e: e.tensor_copy(out=out, in_=in_), reads=r, writes=w)

    def ms(self, eng, out, val, w):
        self.P.op(eng, lambda e: e.memset(out, val), writes=w)

    def dma(self, q, out, in_, r, w, name):
        self.P.op(q, lambda e: e.dma_start(out=out, in_=in_), reads=r, writes=w, dma=name)


def build(stage="full"):
    nc = bass.Bass("TRN2", target_bir_lowering=False)
    dt = lambda name, shape, kind="ExternalInput", dty=F32: nc.dram_tensor(name, shape, dty, kind=kind).ap()
    xT_d = dt("xT", [128, 8, 4096]); xm_d = dt("xm", [NT, D]); win_d = dt("win", [128, 8, 3080])
    wout_d = dt("wout", [128, 8, 1024]); cst_d = dt("consts", [128, C_TOT])
    if stage not in ("A", "A2"):
        wq_d = dt("wq", [128, 8, 2048]); keys_d = dt("keysT", [128, 16, 128])
        ut_d = dt("UT", [128, 128, 8, 128]); v_d = dt("V", [16384, 1024])
    y_d = dt("y", [NT, D], kind="ExternalOutput")
    x1_d = dt("x1s", [NT, D], kind="Internal") if stage not in ("A", "A2") else y_d
    if stage not in ("A", "A2"):
        ub_d = dt("ubs", [128, 128, 1024], kind="Internal", dty=BF16)
        vb_d = dt("vbs", [16384, 1024], kind="Internal", dty=BF16)

    b = Bld(nc); P = b.P
    with contextlib.ExitStack() as st:
        NW = 41400
        cs = st.enter_context(nc.sbuf_tensor("cs", [128, C_TOT], F32))
        xn2T = st.enter_context(nc.sbuf_tensor("xn2T", [128, 8, NT], BF16))
        cb16 = st.enter_context(nc.sbuf_tensor("cb16", [128, 512], BF16))
        ar = st.enter_context(nc.sbuf_tensor("arena", [128, NW], F32))
        psb = [st.enter_context(nc.psum_tensor("psb%d" % i, [128, 512], F32))[:, :] for i in range(8)]
        identb = cb16[:, 0:128]; onesb = cb16[:, 128:256]; trib = cb16[:, 256:384]; iotab = cb16[:, 384:512]
        ident = cs[:, C_CST:C_CST + 128]; tri = cs[:, C_CST + 128:C_CST + 256]
        onesf = cs[:, C_CST + 256:C_CST + 384]; iota = cs[:, C_CST + 384:C_CST + 512]
        col = lambda c, n=1: cs[:, c:c + n]
        eps_c = col(C_EPS); one_c = col(C_ONE); lns_c = col(C_LNS); flag_c = col(C_FLAG)

        off = [0]
        def carve(words, dty=F32, shape=None):
            a = ar[:, off[0]:off[0] + words]; off[0] += words
            assert off[0] <= NW, off[0]
            if dty != F32: a = a.bitcast(dty)
            if shape is not None:
                names = " ".join("d%d" % i for i in range(len(shape)))
                a = a.rearrange("p (%s) -> p %s" % (names, names), **{"d%d" % i: s for i, s in enumerate(shape)})
            return a

        b.dma("sp", cs[:, :], cst_d[:, :], [], ["cs"], "cs")
        b.cp("dve", identb, ident, ["cs"], ["cb16"])
        b.cp("dve", onesb, onesf, ["cs"], ["cb16"])
        b.cp("dve", trib, tri, ["cs"], ["cb16"])
        b.cp("dve", iotab, iota, ["cs"], ["cb16"])

        Winb = carve(12320, BF16, [8, 3080]); Woutb = carve(4096, BF16, [8, 1024])
        xTs = carve(4096, F32, [8, 512]); raw = carve(4120, F32, [8, 515])
        sqb = carve(512, BF16, [2, 512]); lnv = carve(512); rstdb = carve(512)
        hT = carve(2048, BF16, [8, 512]); accs = carve(1024, F32, [2, 512]); qkT = carve(2048, BF16, [8, 512])
        WsTb = carve(256, BF16, [4, 128])
        sm = carve(128)
        sg = carve(192)
        vext = carve(264, BF16, [4, 132]); sig = carve(256, BF16); u_sb = carve(256, BF16)
        vgs = carve(512); vn32 = carve(512); vnb = carve(256, BF16); ktok = carve(256, BF16)
        PT = carve(256, BF16, [4, 128]); ybf = carve(512, BF16); yT = carve(512, BF16, [8, 128])
        xmt = carve(1024); x1 = carve(1024); xs = carve(512, BF16)
        Gst = carve(528, F32, [4, 132]); Cb = carve(264, BF16, [4, 132])
        offA = off[0]

        gsb = sg[:, 0:32]; e1 = sg[:, 32:48]; nlf = sg[:, 48:64]; t16 = sg[:, 64:80]; EK = sg[:, 80:96]; EB = sg[:, 96:112]
        A_t = [sg[:, 112:128], sg[:, 128:144]]; nhl16 = sg[:, 144:160].bitcast(BF16); nhi = nhl16[:, 0:16]; nlo = nhl16[:, 16:32]
        gsb3 = gsb.rearrange("p (j g) -> p j g", j=4)
        den = sm[:, 36:40]; dab = sm[:, 40:44]
        rr = sm[:, 44:48]; ssq = sm[:, 48:52]; mm4 = sm[:, 52:56]; scl = sm[:, 56:60]; st8 = sm[:, 60:68]
        mean = sm[:, 68:72]; msq = sm[:, 72:76]; var = sm[:, 76:80]; rs4 = sm[:, 80:84]
        ss2 = sm[:, 84:85]; l2 = sm[:, 85:86]; r2 = sm[:, 86:87]
        nhl = sm[:, 88:92].bitcast(BF16)
        psP = [psb[0], psb[1]]; psS = psb[3]; psO = [psb[4], psb[5]]; psU = [psb[6], psb[7]]
        psM = psb[2]
        psT = psM[:, 256:512].bitcast(BF16)
        Prog.BANK.clear()
        Prog.BANK.update({"psP0": "B0", "psP1": "B1", "psMg": "B2", "psMb": "B2", "psMl": "B2", "psT": "B2", "psS": "B3",
                          "psO0": "B4", "psO1": "B5", "psU0": "B6", "psU1": "B7",
                          "pq0": "B0", "pq1": "B1", "psc0": "B2", "psc1": "B3", "psc2": "B4", "psc3": "B5",
                          "pt6a": "B6", "pt6b": "B6", "pt6c": "B6",
                          "psOut0": "B0", "psOut1": "B1", "psOut2": "B2", "psOut3": "B3", "psA0": "B4", "psA1": "B5",
                          "psG0": "B6", "psG1": "B7"})
        pcnt = [0]
        def nextP():
            pcnt[0] += 1
            return psP[pcnt[0] % 2], "psP%d" % (pcnt[0] % 2)

        stg = [xTs.rearrange("p a b -> p (a b)"), raw.rearrange("p a b -> p (a b)")]
        for c in range(8):
            s = stg[c % 2][:, 0:3080]; sk = "stg%d" % (c % 2)
            b.dma("sp", s, win_d[:, c, :], [], [sk], sk)
            eng = "dve"
            b.cp(eng, Winb[:, c, 0:1540], s[:, 0:1540], [sk], ["Winb"])
            b.act(Winb[:, c, 1540:3080], s[:, 1540:3080], AF.Copy, [sk], ["Winb"])
        for hf in range(2):
            s = stg[hf][:, 0:4096]; sk = "stg%d" % hf
            b.dma("sp", s, wout_d[:, 4 * hf:4 * hf + 4, :], [], [sk], sk)
            for c4 in range(4):
                c = 4 * hf + c4
                if hf == 0:
                    b.ts("dve", Woutb[:, c, :], s[:, c4 * 1024:(c4 + 1) * 1024], col(C_NG + c), None, ALU.mult, None, [sk, "cs"], ["Woutb"])
                else:
                    b.cp("dve", Woutb[:, c, :], s[:, c4 * 1024:(c4 + 1) * 1024], [sk], ["Woutb"])
        b.cp("dve", WsTb.rearrange("p a b -> p (a b)"), cs[:, C_WS:C_WS + 512], ["cs"], ["WsTb"])
        for g in range(4):
            b.ms("dve", WsTb[64:128, g, 0:64], 0.0, ["WsTb"])
        b.ms("dve", raw.rearrange("p a b -> p (a b)"), 0.0, ["stg1"] + ["raw%d" % c for c in range(8)])
        b.ms("dve", Gst.rearrange("p a b -> p (a b)"), 0.0, ["G"])
        b.ms("dve", Cb.rearrange("p a b -> p (a b)"), 0.0, ["Cb"])
        b.ms("dve", A_t[1], 1.0, ["A1"])
        b.ms("dve", vext.rearrange("p a b -> p (a b)"), 0.0, ["vext"])

        for blk in range(int(os.environ.get("TRUNC", "8"))):
            main = blk >= 4
            tok0 = blk * 512
            for c in range(8):
                b.dma("sp", xTs[:, c, :], xT_d[:, c, tok0:tok0 + 512], [], ["xTs0", "stg0"] if c == 0 else ["xTs%d" % c], "xTs%d" % c)
            for c in range(8):
                b.act(sqb[:, c % 2, :], xTs[:, c, :], AF.Square, ["xTs%d" % c], ["sqb%d" % (c % 2)])
                b.mm(psS, onesb, sqb[:, c % 2, :], c == 0, c == 7, ["cb16", "sqb%d" % (c % 2)], ["psS"])
            b.act(lnv, psS, AF.Ln, ["psS", "cs"], ["lnv"], scale=1.0 / 1024, bias=eps_c)
            b.act(rstdb, lnv, AF.Exp, ["lnv"], ["rstdb"], scale=-0.5)
            for c in range(8):
                b.stt("dve", hT[:, c, :], xTs[:, c, :], col(C_G1 + c), rstdb, ALU.mult, ALU.mult, ["xTs%d" % c, "cs", "rstdb"], ["hT"])
            par = blk % 2
            for j in range(4):
                for c in range(8):
                    b.mm(psM[:, j * 8:(j + 1) * 8], hT[:, c, j * 128:(j + 1) * 128], Winb[:, c, 2048:2056], c == 0, c == 7, ["hT", "Winb"], ["psMg"])
            b.tt("dve", gsb3, psM[:, 0:32].rearrange("p (j g) -> p j g", j=4), col(C_GB, 8).unsqueeze(1).to_broadcast([128, 4, 8]), ALU.add,
                 ["psMg", "cs"], ["gsb"])
            b.act(e1.rearrange("p (j h) -> p j h", j=4), gsb3[:, :, 4:8], AF.Exp, ["gsb"], ["e1"], scale=-1.0)
            b.act(nlf, e1, AF.Ln, ["e1", "cs"], ["nlf"], scale=1.0, bias=one_c)
            b.cp("dve", nhi, nlf, ["nlf"], ["nhi"])
            b.tt("dve", nlo, nlf, nhi, ALU.subtract, ["nlf", "nhi"], ["nlo"])
            for j in range(4):
                js = slice(j * 4, (j + 1) * 4)
                b.mm(psM[:, 64 + j * 4:68 + j * 4], trib, nhi[:, js], True, False, ["cb16", "nhi"], ["psMb"])
                b.mm(psM[:, 64 + j * 4:68 + j * 4], trib, nlo[:, js], False, True, ["cb16", "nlo"], ["psMb"])
                b.mm(psM[:, 96 + j * 4:100 + j * 4], onesb, nhi[:, js], True, False, ["cb16", "nhi"], ["psMl"])
                b.mm(psM[:, 96 + j * 4:100 + j * 4], onesb, nlo[:, js], False, True, ["cb16", "nlo"], ["psMl"])
            b.tt("dve", t16.rearrange("p (j h) -> p j h", j=4), gsb3[:, :, 0:4], psM[:, 64:80].rearrange("p (j h) -> p j h", j=4), ALU.add,
                 ["gsb", "psMb"], ["t4"])
            b.act(EK, t16, AF.Exp, ["t4", "cs"], ["ek"], scale=1.0, bias=lns_c)
            b.act(EB, psM[:, 64:80], AF.Exp, ["psMb"], ["eb"], scale=-1.0)
            b.act(A_t[par], psM[:, 96:112], AF.Exp, ["psMl"], ["A%d" % par], scale=-1.0)
            if blk == 3:
                b.ts("dve", A_t[1][:, 12:16], A_t[1][:, 12:16], flag_c, None, ALU.mult, None, ["A1", "cs"], ["A1"])
            cc_list = list(range(8)) if blk >= 3 else list(range(4, 8))
            for cc in cc_list:
                ps, pk = nextP()
                for c in range(8):
                    b.mm(ps, Winb[:, c, cc * 128:(cc + 1) * 128], hT[:, c, :], c == 0, c == 7, ["Winb", "hT"], [pk])
                rk = "raw%d" % cc
                if blk == 4:
                    b.ts("dve", raw[:, cc, 0:3], raw[:, cc, 512:515], flag_c, None, ALU.mult, None, [rk, "cs"], [rk])
                elif blk > 0:
                    b.cp("dve", raw[:, cc, 0:3], raw[:, cc, 512:515], [rk], [rk])
                b.act(raw[:, cc, 3:515], ps, AF.Copy, [pk], [rk])
                if cc >= 4 or main:
                    ac = accs[:, cc % 2, :]; ak = "acc%d" % (cc % 2)
                    b.act(ac, raw[:, cc, 3:515], AF.Identity, [rk, "cs"], [ak], scale=col(C_CW + 3 * 8 + cc), bias=col(C_CB + cc))
                    for j in (2, 1, 0):
                        b.stt("dve", ac, raw[:, cc, j:j + 512], col(C_CW + j * 8 + cc), ac, ALU.mult, ALU.add, [rk, "cs", ak], [ak])
                    b.act(qkT[:, cc, :], ac, AF.Silu, [ak], ["qkT%d" % cc])
            for j in range(4):
                tsl = slice(j * 128, (j + 1) * 128)
                cidx = blk * 4 + j
                js = slice(j * 4, (j + 1) * 4)
                ek = EK[:, js]; eb = EB[:, js]
                acur = A_t[par][:, js]; ack = "A%d" % par
                if j > 0:
                    aprev = A_t[par][:, (j - 1) * 4:j * 4]; apk = ack
                else:
                    aprev = A_t[1 - par][:, 12:16]; apk = "A%d" % (1 - par)
                ps, pk = nextP()
                for c in range(8):
                    b.mm(ps, hT[:, c, tsl], Winb[:, c, 1024:1536], c == 0, c == 7, ["hT", "Winb"], [pk])
                for h in range(4):
                    b.act(vext[:, h, 0:128], ps[:, h * 128:(h + 1) * 128], AF.Copy, [pk, "ek"], ["vext"], scale=ek[:, h:h + 1])
                b.cp("dve", vext[:, :, 128], ek, ["ek"], ["vext"])
                for h in range(4):
                    b.tr(psT[:, h * 128:(h + 1) * 128], qkT[:, 4 + h, tsl], identb, ["qkT%d" % (4 + h), "cb16"], ["psT"])
                b.cp("dve", ktok, psT, ["psT"], ["ktok"])
                if main:
                    for h in range(4):
                        b.mm(psS[:, h * 128:(h + 1) * 128], qkT[:, 4 + h, tsl], qkT[:, h, tsl], True, True,
                             ["qkT%d" % (4 + h), "qkT%d" % h], ["psS"])
                    b.tt("dve", PT, psS.rearrange("p (a b) -> p a b", a=4), tri.unsqueeze(1).to_broadcast([128, 4, 128]),
                         ALU.mult, ["psS", "cs"], ["PT"])
                    for h in range(4):
                        o = psO[h // 2][:, (h % 2) * 256:(h % 2) * 256 + 129]; ok = "psO%d" % (h // 2)
                        b.mm(o, PT[:, h, :], vext[:, h, 0:129], True, False, ["PT", "vext"], [ok])
                        b.mm(o, qkT[:, h, tsl], Cb[:, h, 0:129], False, True, ["qkT%d" % h, "Cb"], [ok])
                for h in range(4):
                    u = psU[h // 2][:, (h % 2) * 256:(h % 2) * 256 + 129]; uk = "psU%d" % (h // 2)
                    b.mm(u, ktok[:, h * 128:(h + 1) * 128], vext[:, h, 0:129], True, True, ["ktok", "vext"], [uk])
                for h in range(4):
                    u = psU[h // 2][:, (h % 2) * 256:(h % 2) * 256 + 129]; uk = "psU%d" % (h // 2)
                    b.stt("dve", Gst[:, h, 0:129], Gst[:, h, 0:129], aprev[:, h:h + 1], u, ALU.mult, ALU.add, ["G", apk, uk], ["G"])
                    if cidx >= 15:
                        b.ts("dve", Cb[:, h, 0:129], Gst[:, h, 0:129], acur[:, h:h + 1], None, ALU.mult, None, ["G", ack], ["Cb"])
                if not main:
                    continue
                ps, pk = nextP()
                for c in range(8):
                    b.mm(ps, hT[:, c, tsl], Winb[:, c, 1536:2048], c == 0, c == 7, ["hT", "Winb"], [pk])
                b.act(sig, ps, AF.Sigmoid, [pk], ["sig"])
                ps, pk = nextP()
                for c in range(8):
                    b.mm(ps, hT[:, c, tsl], Winb[:, c, 2056:2568], c == 0, c == 7, ["hT", "Winb"], [pk])
                b.act(u_sb, ps, AF.Gelu_apprx_tanh, [pk], ["u_sb"])
                ps, pk = nextP()
                for c in range(8):
                    b.mm(ps, hT[:, c, tsl], Winb[:, c, 2568:3080], c == 0, c == 7, ["hT", "Winb"], [pk])
                b.ms("dve", st8, 0.0, ["st8"])
                for g in range(4):
                    b.act(vgs[:, g * 128:(g + 1) * 128], ps[:, g * 128:(g + 1) * 128], AF.Gelu_apprx_tanh, [pk, "st8"], ["vgs", "st8"],
                          accum=st8[:, g:g + 1])
                for g in range(4):
                    b.act(vn32[:, g * 128:(g + 1) * 128], vgs[:, g * 128:(g + 1) * 128], AF.Square, ["vgs", "st8"], ["vn32", "st8"],
                          accum=st8[:, 4 + g:5 + g])
                b.ts("dve", mean, st8[:, 0:4], 1.0 / 128, None, ALU.mult, None, ["st8"], ["mean"])
                b.tt("dve", msq, mean, mean, ALU.mult, ["mean"], ["msq"])
                b.stt("dve", var, st8[:, 4:8], 1.0 / 128, msq, ALU.mult, ALU.subtract, ["st8", "msq"], ["var"])
                b.act(var, var, AF.Ln, ["var", "cs"], ["var"], scale=1.0, bias=eps_c)
                b.act(rs4, var, AF.Exp, ["var"], ["rs4"], scale=-0.5)
                for g in range(4):
                    b.ts("dve", vn32[:, g * 128:(g + 1) * 128], vgs[:, g * 128:(g + 1) * 128], mean[:, g:g + 1], rs4[:, g:g + 1],
                         ALU.subtract, ALU.mult, ["vgs", "mean", "rs4"], ["vn32"])
                b.tt("dve", vn32, vn32, cs[:, C_LNG:C_LNG + 512], ALU.mult, ["vn32", "cs"], ["vn32"])
                b.tt("dve", vnb, vn32, cs[:, C_LNB:C_LNB + 512], ALU.add, ["vn32", "cs"], ["vnb"])
                for g in range(4):
                    b.mm(psS[:, g * 128:(g + 1) * 128], WsTb[:, g, :], vnb[:, g * 128:(g + 1) * 128], True, True, ["WsTb", "vnb"], ["psS"])
                for g in range(4):
                    b.stt("dve", ybf[:, 512 + g * 128:512 + (g + 1) * 128], psS[:, g * 128:(g + 1) * 128], col(C_BS + g),
                          u_sb[:, g * 128:(g + 1) * 128], ALU.add, ALU.mult, ["psS", "cs", "u_sb"], ["ybf"])
                for p2 in range(2):
                    dv = psO[p2].rearrange("p (h w) -> p h w", h=2)[:, :, 128]
                    b.tt("dve", den[:, 2 * p2:2 * p2 + 2], dv, eb[:, 2 * p2:2 * p2 + 2], ALU.mult, ["psO%d" % p2, "eb"], ["den"])
                b.act(dab, den, AF.Abs, ["den"], ["dab"])
                b.ts("dve", dab, dab, 1.0, None, ALU.max, None, ["dab"], ["dab"])
                b.P.op("dve", lambda e: e.reciprocal(out=dab, in_=dab), reads=["dab"], writes=["dab"])
                b.tt("dve", rr, eb, dab, ALU.mult, ["eb", "dab"], ["rr"])
                b.ms("dve", ssq, 0.0, ["ssq"])
                for h in range(4):
                    o = psO[h // 2][:, (h % 2) * 256:(h % 2) * 256 + 128]
                    b.act(vn32[:, 0:128], o, AF.Square, ["psO%d" % (h // 2), "ssq"], ["vn32", "ssq"], accum=ssq[:, h:h + 1])
                b.tt("dve", mm4, ssq, rr, ALU.mult, ["ssq", "rr"], ["mm4"])
                b.tt("dve", mm4, mm4, rr, ALU.mult, ["mm4", "rr"], ["mm4"])
                b.act(mm4, mm4, AF.Ln, ["mm4", "cs"], ["mm4"], scale=1.0 / 128, bias=eps_c)
                b.act(mm4, mm4, AF.Exp, ["mm4"], ["mm4"], scale=-0.5)
                b.tt("dve", scl, rr, mm4, ALU.mult, ["rr", "mm4"], ["scl"])
                for h in range(4):
                    o = psO[h // 2][:, (h % 2) * 256:(h % 2) * 256 + 128]
                    b.stt("dve", ybf[:, h * 128:(h + 1) * 128], o, scl[:, h:h + 1], sig[:, h * 128:(h + 1) * 128], ALU.mult, ALU.mult,
                          ["psO%d" % (h // 2), "scl", "sig"], ["ybf"])
                for hf in range(2):
                    for q in range(4):
                        b.tr(psT[:, q * 128:(q + 1) * 128], ybf[:, (hf * 4 + q) * 128:(hf * 4 + q + 1) * 128], identb, ["ybf", "cb16"], ["psT"])
                    b.cp("dve", yT[:, hf * 4:hf * 4 + 4, :].rearrange("p a b -> p (a b)"), psT, ["psT"], ["yT"])
                for hf in range(2):
                    for c in range(8):
                        b.mm(psO[hf], yT[:, c, :], Woutb[:, c, hf * 512:(hf + 1) * 512], c == 0, c == 7, ["yT", "Woutb"], ["psO%d" % hf])
                mt0 = (blk - 4) * 512 + j * 128
                b.dma("sp", xmt, xm_d[mt0:mt0 + 128, :], [], ["xmt"], "xmt")
                for hf in range(2):
                    b.tt("dve", x1[:, hf * 512:(hf + 1) * 512], psO[hf], xmt[:, hf * 512:(hf + 1) * 512], ALU.add, ["psO%d" % hf, "xmt"], ["x1"])
                b.dma("sp", x1_d[mt0:mt0 + 128, :], x1, ["x1"], ["x1d%d" % (mt0 // 128)], "x1st")
                if stage == "A":
                    continue
                b.ms("dve", ss2, 0.0, ["ss2"])
                b.act(xs, x1, AF.Square, ["x1", "ss2"], ["xs", "ss2"], accum=ss2)
                b.act(l2, ss2, AF.Ln, ["ss2", "cs"], ["l2"], scale=1.0 / 1024, bias=eps_c)
                b.act(r2, l2, AF.Exp, ["l2"], ["r2"], scale=-0.5)
                b.act(xs, x1, AF.Copy, ["x1", "r2"], ["xs"], scale=r2)
                for hf in range(2):
                    for q in range(4):
                        b.tr(psT[:, q * 128:(q + 1) * 128], xs[:, (hf * 4 + q) * 128:(hf * 4 + q + 1) * 128], identb, ["xs", "cb16"], ["psT"])
                    for q in range(4):
                        b.ts("dve", xn2T[:, hf * 4 + q, mt0:mt0 + 128], psT[:, q * 128:(q + 1) * 128], col(C_G2 + hf * 4 + q), None,
                             ALU.mult, None, ["psT", "cs"], ["xn2T"])
        fkeys = ["x1d%d" % i for i in range(16)]
        if stage not in ("A", "A2"):
            fkeys = build_peer(nc, b, carve, off, cs, xn2T, identb, ident, iotab, psb, wq_d, keys_d, ut_d, v_d, x1_d, y_d, stage, ub_d, vb_d)
        P.emit(final_keys=fkeys)
    return nc


import os


def build_peer(nc, b, carve, off, cs, xn2T, identb, ident, iota, psb, wq_d, keys_d, ut_d, v_d, x1_d, y_d, stage='full', ub_d=None, vb_d=None):
    P = b.P
    P.fence()
    off[0] = 0
    col = lambda c, n=1: cs[:, c:c + n]
    eps_c = col(C_EPS)
    I1T = carve(1024, BF16); I2T = carve(1024, BF16); GTs = carve(1024, BF16)
    offBC = off[0]
    Wqb = carve(8192, BF16, [8, 2048]); keysTb = carve(1024, BF16, [16, 128]); stg = carve(2048)
    qTb = carve(1024, BF16, [16, 128]); sc2 = carve(4096, F32, [2, 16, 128]); wk = carve(2048, F32, [16, 128])
    m16 = carve(256, F32, [16, 16]); i16 = carve(256, U32, [16, 16]); i16f = carve(256, F32, [16, 16])
    NC = 80; RECTS = [(0, 2, 16, 0), (2, 2, 8, 32), (4, 4, 4, 48), (8, 8, 2, 64)]
    cand = carve(8 * NC, F32, [8, NC]); cidx = carve(8 * NC, F32, [8, NC]); junk = carve(4 * NC, F32, [4, NC])
    ts16 = carve(128, F32, [8, 16]); eidx = carve(128); ex = carve(128, F32, [8, 16]); gate = carve(128, F32, [8, 16])
    ei = carve(128, I32); i1i = carve(128, I32); i2i = carve(128, I32); i1f = carve(128); i2f = carve(128)
    tb3 = carve(192, BF16)
    smb = carve(32); negm = smb[:, 0:8]; Z = smb[:, 8:16]; rz = smb[:, 16:24]
    cin = carve(6144, F32, [3, 2, 1024]); cout = carve(3072, BF16, [3, 2, 1024])
    jobs = {"in": 0, "cv": 0}

    def conv_in():
        j = jobs["in"]
        if j >= 128: return
        jobs["in"] += 1; s3 = j % 3
        b.dma("sp", cin[:, s3, 0, :], ut_d[j].rearrange("p c e -> p (c e)"), [], ["cinu%d" % s3], "cinu%d" % s3)
        b.dma("sp", cin[:, s3, 1, :], v_d[j * 128:(j + 1) * 128, :], [], ["cinv%d" % s3], "cinv%d" % s3)

    def conv_cv():
        j = jobs["cv"]
        if j >= 128: return
        jobs["cv"] += 1; s3 = j % 3
        b.act(cout[:, s3, 0, :], cin[:, s3, 0, :], AF.Copy, ["cinu%d" % s3], ["coutu%d" % s3])
        b.act(cout[:, s3, 1, :], cin[:, s3, 1, :], AF.Copy, ["cinv%d" % s3], ["coutv%d" % s3])
        b.dma("sp", ub_d[j], cout[:, s3, 0, :], ["coutu%d" % s3], ["ubd"], "cou%d" % s3)
        b.dma("sp", vb_d[j * 128:(j + 1) * 128, :], cout[:, s3, 1, :], ["coutv%d" % s3], ["vbd"], "cov%d" % s3)

    conv_in(); conv_in()
    for c in range(8):
        b.dma("sp", stg, wq_d[:, c, :], [], ["stgB"], "stgB")
        b.cp("dve", Wqb[:, c, 0:1024], stg[:, 0:1024], ["stgB"], ["Wqb"])
        b.act(Wqb[:, c, 1024:2048], stg[:, 1024:2048], AF.Copy, ["stgB"], ["Wqb"])
    b.dma("sp", stg, keys_d[:, :, :].rearrange("p a b -> p (a b)"), [], ["stgB"], "stgB")
    b.cp("dve", keysTb.rearrange("p a b -> p (a b)"), stg, ["stgB"], ["keysTb"])
    PB = int(os.environ.get("PB_STOP", "99"))
    if PB <= 0: return ["x1d%d" % q for q in range(16)]
    def part1(i):
        t0 = i * 128; par = i % 2; sc = sc2[:, par]
        for hp in range(16):
            ps = psb[hp % 2]; pk = "pq%d" % (hp % 2)
            for c in range(8):
                b.mm(ps[:, 0:128], Wqb[:, c, hp * 128:(hp + 1) * 128], xn2T[:, c, t0:t0 + 128], c == 0, c == 7, ["Wqb", "xn2T"], [pk])
            b.act(qTb[:, hp, :], ps[:, 0:128], AF.Copy, [pk], ["qTb%d" % hp])
            if hp % 2 == 0:
                conv_in(); conv_cv()
        for hp in range(16):
            b.mm(psb[2 + hp // 4][:, (hp % 4) * 128:(hp % 4 + 1) * 128], qTb[:, hp, :], keysTb[:, hp, :], True, True,
                 ["qTb%d" % hp, "keysTb"], ["psc%d" % (hp // 4)])
        for q4 in range(4):
            b.act(sc[:, q4 * 4:q4 * 4 + 4, :].rearrange("p a b -> p (a b)"), psb[2 + q4], AF.Copy, ["psc%d" % q4], ["sc%d_%d" % (q4, par)])

    def part2(i):
        t0 = i * 128; par = i % 2; sc = sc2[:, par]
        for hp in range(16):
            b.P.op("dve", (lambda hp: lambda e: e.max(out=m16[:, hp, 0:8], in_=sc[:, hp, :]))(hp), reads=["sc%d_%d" % (hp // 4, par)], writes=["m16_%d" % hp])
        for hp in range(16):
            b.P.op("dve", (lambda hp: lambda e: e.max_index(out=i16[:, hp, 0:8], in_max=m16[:, hp, 0:8], in_values=sc[:, hp, :]))(hp),
                   reads=["sc%d_%d" % (hp // 4, par), "m16_%d" % hp], writes=["i16_%d" % hp])
        for hp in range(16):
            b.P.op("dve", (lambda hp: lambda e: e.match_replace(out=wk[:, hp, :], in_to_replace=m16[:, hp, 0:8], in_values=sc[:, hp, :], imm_value=-1e30))(hp),
                   reads=["sc%d_%d" % (hp // 4, par), "m16_%d" % hp], writes=["wk%d" % hp])
        for hp in range(16):
            b.P.op("dve", (lambda hp: lambda e: e.max(out=m16[:, hp, 8:16], in_=wk[:, hp, :]))(hp), reads=["wk%d" % hp], writes=["m16_%d" % hp])
        for hp in range(16):
            b.P.op("dve", (lambda hp: lambda e: e.max_index(out=i16[:, hp, 8:16], in_max=m16[:, hp, 8:16], in_values=wk[:, hp, :]))(hp),
                   reads=["wk%d" % hp, "m16_%d" % hp], writes=["i16_%d" % hp])
        allm = ["m16_%d" % hp for hp in range(16)]; alli = ["i16_%d" % hp for hp in range(16)]
        b.cp("dve", i16f.rearrange("p a b -> p (a b)"), i16.rearrange("p a b -> p (a b)"), alli, ["i16f"])
        m16h = m16.rearrange("p (h two) a -> p h two a", two=2)
        i16h = i16f.rearrange("p (h two) a -> p h two a", two=2)
        allc = ["cand%d" % h for h in range(8)]; allx = ["cidx%d" % h for h in range(8)]
        b.ts("dve", i16h[:, :, 0, :], i16h[:, :, 0, :], 128.0, None, ALU.mult, None, ["i16f"], ["i16f"])
        for (a0, na, nb, o0) in RECTS:
            shp = [128, 8, na, nb]
            b.tt("dve", cand[:, :, o0:o0 + na * nb].rearrange("p h (a c) -> p h a c", a=na),
                 m16h[:, :, 0, a0:a0 + na].unsqueeze(3).to_broadcast(shp), m16h[:, :, 1, 0:nb].unsqueeze(2).to_broadcast(shp),
                 ALU.add, allm, allc)
            b.tt("dve", cidx[:, :, o0:o0 + na * nb].rearrange("p h (a c) -> p h a c", a=na),
                 i16h[:, :, 0, a0:a0 + na].unsqueeze(3).to_broadcast(shp), i16h[:, :, 1, 0:nb].unsqueeze(2).to_broadcast(shp),
                 ALU.add, ["i16f"], allx)
        wk2 = wk.rearrange("p a b -> p (a b)").rearrange("p (a b) -> p a b", a=8)[:, :, 0:NC]
        for h in range(8):
            b.P.op("dve", (lambda h: lambda e: e.max(out=ts16[:, h, 0:8], in_=cand[:, h, :]))(h), reads=["cand%d" % h], writes=["ts16_%d" % h])
        for h in range(8):
            b.P.op("dve", (lambda h: lambda e: e.match_replace(out=wk2[:, h, :], in_to_replace=ts16[:, h, 0:8], in_values=cand[:, h, :], imm_value=-1e30))(h),
                   reads=["cand%d" % h, "ts16_%d" % h] + ["wk%d" % (2 * h), "wk%d" % (2 * h + 1)], writes=["wk%d" % (2 * h), "wk%d" % (2 * h + 1)])
        for h in range(8):
            b.P.op("dve", (lambda h: lambda e: e.max(out=ts16[:, h, 8:16], in_=wk2[:, h, :]))(h), reads=["wk%d" % (2 * h), "wk%d" % (2 * h + 1)], writes=["ts16_%d" % h])
        b.ms("dve", eidx, 0.0, ["eidx"])
        if os.environ.get("TRIV"):
            for q in range(int(os.environ["TRIV"])):
                b.ms("dve", junk[:, q % 4, :], 0.0, ["junk%d" % (q % 4)])
            return ["x1d%d" % q for q in range(16)]
        for j in range(16):
            for h in range(8):
                jk = "junk%d" % ((j * 8 + h) % 4)
                b.stt("dve", junk[:, (j * 8 + h) % 4, :], cand[:, h, :], ts16[:, h, j:j + 1], cidx[:, h, :], ALU.is_equal, ALU.mult,
                      ["cand%d" % h, "ts16_%d" % h, "cidx%d" % h, "eidx"], [jk, "eidx%d" % (h * 16 + j)], accum=(None if os.environ.get("NOACC") else eidx[:, h * 16 + j:h * 16 + j + 1]))
        alle = ["eidx"] + ["eidx%d" % q for q in range(128)]
        allts = ["ts16_%d" % h for h in range(8)]
        b.ts("dve", negm, ts16[:, :, 0], -1.0, None, ALU.mult, None, allts, ["negm"])
        b.ms("dve", Z, 0.0, ["Z"])
        for h in range(8):
            b.act(ex[:, h, :], ts16[:, h, :], AF.Exp, allts + ["negm", "Z"], ["ex", "Z"], scale=1.0, bias=negm[:, h:h + 1], accum=Z[:, h:h + 1])
        b.P.op("dve", lambda e: e.reciprocal(out=rz, in_=Z), reads=["Z"], writes=["rz"])
        b.tt("dve", gate, ex, rz.unsqueeze(2).to_broadcast([128, 8, 16]), ALU.mult, ["ex", "rz"], ["gate"])
        b.cp("dve", ei, eidx, alle, ["ei"])
        b.ts("dve", i1i, ei, 7, None, ALU.arith_shift_right, None, ["ei"], ["i1i"])
        b.ts("dve", i2i, ei, 127, None, ALU.bitwise_and, None, ["ei"], ["i2i"])
        b.cp("dve", tb3[:, 0:128], i1i, ["i1i"], ["tb3a"])
        b.cp("dve", tb3[:, 128:256], i2i, ["i2i"], ["tb3b"])
        b.cp("dve", tb3[:, 256:384], gate.rearrange("p a b -> p (a b)"), ["gate"], ["tb3c"])
        pT6 = psb[6].bitcast(BF16)
        b.tr(pT6[:, 0:128], tb3[:, 0:128], identb, ["tb3a"], ["pt6a"])
        b.tr(pT6[:, 128:256], tb3[:, 128:256], identb, ["tb3b"], ["pt6b"])
        b.tr(pT6[:, 256:384], tb3[:, 256:384], identb, ["tb3c"], ["pt6c"])
        b.act(I1T[:, t0:t0 + 128], pT6[:, 0:128], AF.Copy, ["pt6a"], ["I1T"])
        b.act(I2T[:, t0:t0 + 128], pT6[:, 128:256], AF.Copy, ["pt6b"], ["I2T"])
        b.act(GTs[:, t0:t0 + 128], pT6[:, 256:384], AF.Copy, ["pt6c"], ["GTs"])
    part1(0)
    for i in range(16):
        if i + 1 < 16: part1(i + 1)
        part2(i)
    while jobs["cv"] < 128:
        conv_in(); conv_cv()
    if stage == "AB":
        return ["x1d%d" % i for i in range(16)]
    P.fence()
    off[0] = offBC
    GT = carve(16384, BF16, [128, 256]); ohA = carve(256, BF16, [8, 64]); ohB = carve(512, BF16, [8, 128])
    NSL = 8
    Ub = carve(NSL * 512, BF16, [NSL, 8, 128]); Vb = carve(NSL * 512, BF16, [NSL, 1024])
    Ab = carve(256, BF16, [2, 256]); AG = carve(256, BF16, [2, 256])
    x1t = [carve(1024), carve(1024)]; x2 = [carve(1024), carve(1024)]; ot = [carve(1024), carve(1024)]; smc = carve(16)
    psOut = psb[0:4]; psA = psb[4:6]; psG = psb[6:8]
    fkeys = []
    GL = [(0, 0, g) for g in range(64)]
    for T in range(8):
        GL += [(T, 1, g) for g in range(64)]
        if T + 1 < 8: GL += [(T + 1, 0, g) for g in range(64)]
    gn = [0]

    def onehots(T, half, grp, p):
        t0 = T * 256 + grp * 4; h0 = 64 * half
        A = ohA[:, p * 4:(p + 1) * 4, :]; Bm = ohB[:, p * 4:(p + 1) * 4, :]
        kA = ["ohA%d" % (p * 4 + q) for q in range(4)]; kB = ["ohB%d" % (p * 4 + q) for q in range(4)]
        b.tt("dve", A, iota[:, h0:h0 + 64].unsqueeze(1).to_broadcast([128, 4, 64]), I1T[:, t0:t0 + 4].unsqueeze(2).to_broadcast([128, 4, 64]),
             ALU.is_equal, ["cb16", "I1T"], kA)
        b.tt("dve", A, A, GTs[:, t0:t0 + 4].unsqueeze(2).to_broadcast([128, 4, 64]), ALU.mult, kA + ["GTs"], kA)
        b.tt("dve", Bm, iota.unsqueeze(1).to_broadcast([128, 4, 128]), I2T[:, t0:t0 + 4].unsqueeze(2).to_broadcast([128, 4, 128]),
             ALU.is_equal, ["cb16", "I2T"], kB)

    def gstep():
        n = gn[0]; gn[0] += 1
        if n + 1 < len(GL):
            T, half, grp = GL[n + 1]
            onehots(T, half, grp, (n + 1) % 2)
        if n < len(GL):
            p = n % 2
            for s4 in range(4):
                sl = p * 4 + s4
                b.mm(psG[p][:, s4 * 64:(s4 + 1) * 64], ohB[:, sl, :], ohA[:, sl, :], True, True, ["ohA%d" % sl, "ohB%d" % sl], ["psG%d" % p])
        if 1 <= n <= len(GL):
            T, half, grp = GL[n - 1]; p = (n - 1) % 2; h0 = 64 * half
            b.act(GT[:, h0:h0 + 64, grp * 4:grp * 4 + 4], psG[p][:, 0:256].rearrange("p (t i) -> p i t", t=4), AF.Copy,
                  ["psG%d" % p], ["GT%d" % half])

    T, half, grp = GL[0]
    onehots(T, half, grp, 0)
    for _ in range(64):
        gstep()

    ssq2 = smc[:, 0:2]; lf2 = smc[:, 2:4]; rf2 = smc[:, 4:6]

    def epi_sq(T, sub):
        b.ms("dve", ssq2[:, sub:sub + 1], 0.0, ["ssf%d" % sub])
        b.act(ot[sub], x2[sub], AF.Square, ["x2%d" % sub, "ssf%d" % sub], ["ot%d" % sub, "ssf%d" % sub], accum=ssq2[:, sub:sub + 1])

    def epi_ln(T):
        b.act(lf2, ssq2, AF.Ln, ["ssf0", "ssf1", "cs"], ["lf_"], scale=1.0 / 1024, bias=eps_c)
        b.act(rf2, lf2, AF.Exp, ["lf_"], ["rf"], scale=-0.5)

    def epi_fin(T, sub):
        r0 = T * 256 + sub * 128
        b.stt("dve", ot[sub], x2[sub], rf2[:, sub:sub + 1], cs[:, C_FGB:C_FGB + 1024], ALU.mult, ALU.mult,
              ["x2%d" % sub, "rf", "cs", "ot%d" % sub], ["ot%d" % sub])
        k = "yd%d" % (r0 // 128)
        b.dma("pool", y_d[r0:r0 + 128, :], ot[sub], ["ot%d" % sub], [k], "yst")
        fkeys.append(k)

    EPI = {1: lambda T: epi_sq(T, 0), 3: lambda T: epi_sq(T, 1), 5: epi_ln, 8: lambda T: epi_fin(T, 0), 10: lambda T: epi_fin(T, 1)}

    for T in range(8):
        tb = T * 256

        def stage1(blk):
            s4 = blk % NSL; s2 = blk % 2
            b.dma("sp", Ub[:, s4, :, :].rearrange("p a b -> p (a b)"), ub_d[blk], ["ubd"], ["Ub%d" % s4], "Ub%d" % s4)
            b.dma("sp", Vb[:, s4, :], vb_d[blk * 128:(blk + 1) * 128, :], ["vbd"], ["Vb%d" % s4], "Vb%d" % s4)
            for c in range(8):
                b.mm(psA[s2][:, 0:256], Ub[:, s4, c, :], xn2T[:, c, tb:tb + 256], c == 0, c == 7, ["Ub%d" % s4, "xn2T"], ["psA%d" % s2])

        def stage2(blk):
            s2 = blk % 2; s4 = blk % NSL
            b.act(Ab[:, s2, :], psA[s2][:, 0:256], AF.Gelu_apprx_tanh, ["psA%d" % s2], ["Ab%d" % s2])
            b.tt("dve", AG[:, s2, :], Ab[:, s2, :], GT[:, blk, :], ALU.mult, ["Ab%d" % s2, "GT%d" % (blk // 64)], ["AG%d" % s2])
            for sub in range(2):
                for hf in range(2):
                    b.mm(psOut[sub * 2 + hf], AG[:, s2, sub * 128:(sub + 1) * 128], Vb[:, s4, hf * 512:(hf + 1) * 512], blk == 0, blk == 127,
                         ["AG%d" % s2, "Vb%d" % s4], ["psOut%d" % (sub * 2 + hf)])

        for blk in range(129):
            if blk < 128: stage1(blk)
            if blk >= 1: stage2(blk - 1)
            if blk < 128:
                gstep()
            if blk in EPI and T > 0:
                EPI[blk](T - 1)
            if blk == 96:
                for sub in range(2):
                    r0 = tb + sub * 128
                    b.dma("sp", x1t[sub], x1_d[r0:r0 + 128, :], ["x1d%d" % (r0 // 128)], ["x1t%d" % sub], "x1t%d" % sub)
        for sub in range(2):
            for hf in range(2):
                b.tt("dve", x2[sub][:, hf * 512:(hf + 1) * 512], psOut[sub * 2 + hf], x1t[sub][:, hf * 512:(hf + 1) * 512], ALU.add,
                     ["psOut%d" % (sub * 2 + hf), "x1t%d" % sub], ["x2%d" % sub])
    for k in sorted(EPI):
        EPI[k](7)
    return fkeys


def host_inputs(inp):
    f = lambda a: np.ascontiguousarray(a, dtype=np.float32)
    x = inp["x"]
    consts = np.zeros((128, C_TOT), np.float32)
    cw = inp["conv_w"][0]
    consts[:, C_CW:C_CW + 32] = cw.reshape(4, 8, 128).transpose(2, 0, 1).reshape(128, 32)
    consts[:, C_CB:C_CB + 8] = inp["conv_b"][0].reshape(8, 128).T
    consts[:, C_G1:C_G1 + 8] = inp["norm1_g"][0].reshape(8, 128).T
    consts[:, C_G2:C_G2 + 8] = inp["norm2_g"][0].reshape(8, 128).T
    consts[:, C_NG:C_NG + 4] = inp["mlstm_norm_g"][0].reshape(4, 128).T
    consts[:, C_GB:C_GB + 4] = inp["b_igate"][0][None, :]
    consts[:, C_GB + 4:C_GB + 8] = inp["b_fgate"][0][None, :]
    consts[:, C_BS:C_BS + 4] = inp["gmlp_b_s"][0].T
    consts[:, C_EPS] = EPS; consts[:, C_ONE] = 1.0; consts[:, C_LNS] = np.float32(np.log(128.0 ** -0.5))
    consts[:, C_LNG:C_LNG + 512] = inp["gmlp_ln_g"][0][None, :]
    consts[:, C_LNB:C_LNB + 512] = inp["gmlp_ln_b"][0][None, :]
    consts[:, C_FGB:C_FGB + 1024] = inp["final_g"][None, :]
    consts[:, C_CST:C_CST + 128] = np.eye(128)
    consts[:, C_CST + 128:C_CST + 256] = np.triu(np.ones((128, 128)))
    consts[:, C_CST + 256:C_CST + 384] = 1.0
    consts[:, C_CST + 384:C_CST + 512] = np.arange(128)[None, :]
    consts[:, C_WS:C_WS + 512] = inp["gmlp_w_s"][0].transpose(2, 0, 1).reshape(128, 512)
    shared = {
        "win": f(inp["w_in"][0].reshape(8, 128, 3080).transpose(1, 0, 2)),
        "wout": f(inp["w_out"][0].reshape(8, 128, 1024).transpose(1, 0, 2)),
        "wq": f(inp["peer_w_query"][0].reshape(8, 128, 2048).transpose(1, 0, 2)),
        "keysT": f(inp["peer_sub_keys"][0].reshape(16, 128, 128).transpose(2, 0, 1)),
        "UT": f(inp["peer_u"][0].reshape(128, 128, 8, 128).transpose(0, 3, 2, 1)),
        "V": f(inp["peer_v"][0]),
    }
    maps = []
    for core in range(NCORES):
        bi, s = core // 2, core % 2
        xm = x[bi, s * NT:(s + 1) * NT]
        pre = x[bi, 0:NT]
        toks = np.concatenate([pre, xm], axis=0)
        xT = f(toks.T.reshape(8, 128, 4096).transpose(1, 0, 2))
        c = consts.copy(); c[:, C_FLAG] = float(s)
        m = dict(shared); m.update({"xT": xT, "xm": f(xm), "consts": c})
        maps.append(m)
    return maps


_NC_CACHE = {}


def kernel(**inputs):
    stage = "full"
    if stage not in _NC_CACHE:
        _NC_CACHE[stage] = build(stage)
    nc = _NC_CACHE[stage]
    maps = host_inputs({k: np.asarray(v) for k, v in inputs.items()})
    res = run_bass_kernel_spmd(nc, maps, core_ids=list(range(NCORES)))
    out = np.empty((4, 4096, 1024), np.float32)
    for core in range(NCORES):
        bi, s = core // 2, core % 2
        out[bi, s * NT:(s + 1) * NT] = res.results[core]["y"]
    return out
```
